# Optimizing a Trainium2 kernel written in Bass

```python
import math
import numpy as np
import jax
import jax.numpy as jnp
from jax import lax

D_MODEL = 1024
BATCH = 4
SEQ = 4096
DEPTH = 4

GRID_W = 64
CTX_LEN = 256
MIX_HALF = D_MODEL // 2
S5_GROUP = 16
S5_GROUPS = MIX_HALF // S5_GROUP
S5_STATE = 64
NA_HEAD_DIM = 64
NA_HEADS = MIX_HALF // NA_HEAD_DIM
NA_WIN_ROWS = 8
NA_WIN_COLS = 16
GLA_HEADS = 4
GLA_DK = MIX_HALF // (2 * GLA_HEADS)
GLA_DV = MIX_HALF // GLA_HEADS
GLA_QK = GLA_HEADS * GLA_DK
GLA_RANK = 16
GLA_TAU = 16.0
GLA_CHUNK = 64
DIFF_HEADS = 4
DIFF_DV = MIX_HALF // DIFF_HEADS
DIFF_DH = DIFF_DV // 2
DIFF_BLOCK = 128
N_GROUPS = 4
EXP_PER_GROUP = 8
N_EXPERTS = N_GROUPS * EXP_PER_GROUP
TOP_K = 2
D_EXPERT = D_MODEL // 4
ROPE_BASE = 10000.0
EPS = 1e-5
EVEN_SPLITS = (MIX_HALF, MIX_HALF, MIX_HALF, MIX_HALF)
ODD_SPLITS = (GLA_QK, GLA_QK, MIX_HALF, MIX_HALF, 2 * GLA_RANK, MIX_HALF, MIX_HALF, MIX_HALF)
EVEN_IN = sum(EVEN_SPLITS)
ODD_IN = sum(ODD_SPLITS)

kernel_name = 'hybrid_s5_natten_gla_diffattn_hmoe_dit'

F32 = jnp.float32


def _cuts(splits):
    return [int(v) for v in np.cumsum(splits)[:-1]]


def layer_norm(x, g, b):
    x32 = x.astype(F32)
    mu = jnp.mean(x32, -1, keepdims=True)
    var = jnp.mean(jnp.square(x32 - mu), -1, keepdims=True)
    return ((x32 - mu) * lax.rsqrt(var + EPS) * g.astype(F32) + b.astype(F32)).astype(x.dtype)


def rms_norm(x, g):
    x32 = x.astype(F32)
    return x32 * lax.rsqrt(jnp.mean(jnp.square(x32), -1, keepdims=True) + EPS) * g.astype(F32)


def s5_discretize(lam_re, lam_im, log_dt, b_re, b_im):
    lam_re, lam_im = lam_re.astype(F32), lam_im.astype(F32)
    dt = jnp.exp(log_dt.astype(F32))[:, None]
    mag = jnp.exp(lam_re * dt)
    a_re = mag * jnp.cos(lam_im * dt)
    a_im = mag * jnp.sin(lam_im * dt)
    den = lam_re * lam_re + lam_im * lam_im
    f_re = ((a_re - 1.0) * lam_re + a_im * lam_im) / den
    f_im = (a_im * lam_re - (a_re - 1.0) * lam_im) / den
    b_re, b_im = b_re.astype(F32), b_im.astype(F32)
    bb_re = f_re[..., None] * b_re - f_im[..., None] * b_im
    bb_im = f_re[..., None] * b_im + f_im[..., None] * b_re
    return a_re, a_im, bb_re, bb_im


def _complex_linear_combine(e1, e2):
    a1r, a1i, b1r, b1i = e1
    a2r, a2i, b2r, b2i = e2
    return (a1r * a2r - a1i * a2i, a1r * a2i + a1i * a2r,
            a2r * b1r - a2i * b1i + b2r, a2r * b1i + a2i * b1r + b2i)


def s5_scan(u, a_re, a_im, bb_re, bb_im, h0, reverse):
    bu_re = jnp.einsum('btgh,gph->btgp', u, bb_re)
    bu_im = jnp.einsum('btgh,gph->btgp', u, bb_im)
    if h0 is not None:
        idx = -1 if reverse else 0
        h0_re, h0_im = h0
        bu_re = bu_re.at[:, idx].add(a_re * h0_re - a_im * h0_im)
        bu_im = bu_im.at[:, idx].add(a_re * h0_im + a_im * h0_re)
    shape = (1, u.shape[1]) + a_re.shape
    elems = (jnp.broadcast_to(a_re, shape), jnp.broadcast_to(a_im, shape), bu_re, bu_im)
    _, _, h_re, h_im = lax.associative_scan(_complex_linear_combine, elems, reverse=reverse, axis=1)
    return h_re, h_im


def s5_readout(h_re, h_im, c_re, c_im):
    return (jnp.einsum('btgp,ghp->btgh', h_re, c_re.astype(F32))
            - jnp.einsum('btgp,ghp->btgh', h_im, c_im.astype(F32)))


def s5_mixer(u_c, u_l, lam_re, lam_im, log_dt, b_re, b_im, c_re, c_im, d_skip, glu_w, glu_b, need_ctx):
    grp = lambda u: u.astype(F32).reshape(u.shape[0], u.shape[1], S5_GROUPS, S5_GROUP)
    uc, ul = grp(u_c), grp(u_l)
    d = d_skip.astype(F32).reshape(S5_GROUPS, S5_GROUP)
    y_l = d * ul
    y_c = d * uc if need_ctx else None
    for direction in range(2):
        rev = direction == 1
        a_re, a_im, bb_re, bb_im = s5_discretize(lam_re[direction], lam_im[direction], log_dt[direction],
                                                 b_re[direction], b_im[direction])
        hc_re, hc_im = s5_scan(uc, a_re, a_im, bb_re, bb_im, None, rev)
        end = 0 if rev else -1
        hl_re, hl_im = s5_scan(ul, a_re, a_im, bb_re, bb_im, (hc_re[:, end], hc_im[:, end]), rev)
        y_l = y_l + s5_readout(hl_re, hl_im, c_re[direction], c_im[direction])
        if need_ctx:
            y_c = y_c + s5_readout(hc_re, hc_im, c_re[direction], c_im[direction])

    def glu(y):
        z = jax.nn.gelu(y.reshape(y.shape[0], y.shape[1], MIX_HALF))
        val, gate = jnp.split(z @ glu_w.astype(F32) + glu_b.astype(F32), 2, axis=-1)
        return (val * jax.nn.sigmoid(gate)).astype(u_l.dtype)

    return (glu(y_c) if need_ctx else None), glu(y_l)


def natten_mixer(q_c, k_c, v_c, q_l, k_l, v_l, rpb, need_ctx):
    b, t, _ = q_l.shape
    rows = t // GRID_W
    wr = min(NA_WIN_ROWS, rows)
    wc = NA_WIN_COLS
    nk = wr * wc
    scale = NA_HEAD_DIM ** -0.5
    heads = lambda z: z.reshape(z.shape[0], z.shape[1], NA_HEADS, NA_HEAD_DIM).transpose(0, 2, 1, 3)
    kc, vc = heads(k_c), heads(v_c)
    grid = lambda z: heads(z).reshape(b, NA_HEADS, rows, GRID_W, NA_HEAD_DIM)
    qg, kg, vg = grid(q_l * scale), grid(k_l), grid(v_l)
    col_start = np.clip(np.arange(GRID_W) - wc // 2, 0, GRID_W - wc)
    col_idx = col_start[:, None] + np.arange(wc)[None, :]
    dc_idx = col_idx - np.arange(GRID_W)[:, None] + (NA_WIN_COLS - 1)

    def row_block(r):
        rs = jnp.clip(r - wr // 2, 0, rows - wr)
        q_row = lax.dynamic_index_in_dim(qg, r, axis=2, keepdims=False)
        k_rows = lax.dynamic_slice_in_dim(kg, rs, wr, axis=2)
        v_rows = lax.dynamic_slice_in_dim(vg, rs, wr, axis=2)
        k_win = k_rows[:, :, :, col_idx].transpose(0, 1, 3, 2, 4, 5).reshape(b, NA_HEADS, GRID_W, nk, NA_HEAD_DIM)
        v_win = v_rows[:, :, :, col_idx].transpose(0, 1, 3, 2, 4, 5).reshape(b, NA_HEADS, GRID_W, nk, NA_HEAD_DIM)
        dr_idx = rs + jnp.arange(wr) - r + (NA_WIN_ROWS - 1)
        bias = rpb[:, dr_idx[:, None, None], dc_idx[None, :, :]]
        bias = bias.transpose(0, 2, 1, 3).reshape(NA_HEADS, GRID_W, nk).astype(F32)
        s_loc = jnp.einsum('bhwd,bhwkd->bhwk', q_row, k_win).astype(F32) + bias
        s_ctx = jnp.einsum('bhwd,bhcd->bhwc', q_row, kc).astype(F32)
        p = jax.nn.softmax(jnp.concatenate([s_loc, s_ctx], axis=-1), axis=-1).astype(v_l.dtype)
        return (jnp.einsum('bhwk,bhwkd->bhwd', p[..., :nk], v_win)
                + jnp.einsum('bhwc,bhcd->bhwd', p[..., nk:], vc))

    o = lax.map(row_block, jnp.arange(rows))
    y_l = o.transpose(1, 0, 3, 2, 4).reshape(b, t, MIX_HALF)
    y_c = None
    if need_ctx:
        qc = heads(q_c * scale)
        p = jax.nn.softmax(jnp.einsum('bhqd,bhkd->bhqk', qc, kc).astype(F32), axis=-1).astype(v_c.dtype)
        y_c = jnp.einsum('bhqk,bhkd->bhqd', p, vc).transpose(0, 2, 1, 3).reshape(b, q_c.shape[1], MIX_HALF)
    return y_c, y_l


def even_mixer(a_c, a_l, w_in, w_out, lam_re, lam_im, log_dt, b_re, b_im, c_re, c_im, d_skip,
               glu_w, glu_b, rpb, need_ctx):
    u_c, q_c, k_c, v_c = jnp.split(a_c @ w_in, _cuts(EVEN_SPLITS), axis=-1)
    u_l, q_l, k_l, v_l = jnp.split(a_l @ w_in, _cuts(EVEN_SPLITS), axis=-1)
    s_c, s_l = s5_mixer(u_c, u_l, lam_re, lam_im, log_dt, b_re, b_im, c_re, c_im, d_skip, glu_w, glu_b, need_ctx)
    n_c, n_l = natten_mixer(q_c, k_c, v_c, q_l, k_l, v_l, rpb, need_ctx)
    y_l = jnp.concatenate([s_l, n_l], axis=-1) @ w_out
    y_c = jnp.concatenate([s_c, n_c], axis=-1) @ w_out if need_ctx else None
    return y_c, y_l


def gla_chunked(q, k, v, g, s0, need_out):
    b, h, t, dk = q.shape
    dv = v.shape[-1]
    n = t // GLA_CHUNK
    blk = lambda z: z.reshape(b, h, n, GLA_CHUNK, z.shape[-1])
    q, k, v, g = blk(q), blk(k), blk(v), blk(g)
    gc = jnp.cumsum(g, axis=3)
    g_end = gc[:, :, :, -1:, :]
    u = jnp.einsum('bhncd,bhncv->bhndv', k * jnp.exp(g_end - gc), v)
    decay = jnp.exp(g_end[:, :, :, 0, :])
    if s0 is None:
        s0 = jnp.zeros((b, h, dk, dv), q.dtype)

    def step(s, inp):
        dec, un = inp
        return dec[..., None] * s + un, s

    s_fin, s_prev = lax.scan(step, s0, (jnp.moveaxis(decay, 2, 0), jnp.moveaxis(u, 2, 0)))
    if not need_out:
        return None, s_fin
    s_prev = jnp.moveaxis(s_prev, 0, 2)
    q_dec = q * jnp.exp(gc)
    att = jnp.einsum('bhnid,bhnjd->bhnij', q_dec, k * jnp.exp(-gc))
    att = jnp.where(jnp.tril(jnp.ones((GLA_CHUNK, GLA_CHUNK), bool)), att, 0.0)
    o = jnp.einsum('bhnij,bhnjv->bhniv', att, v) + jnp.einsum('bhncd,bhndv->bhncv', q_dec, s_prev)
    return o.reshape(b, h, t, dv), s_fin


def gla_mixer(q_c, k_c, v_c, r_c, z_c, q_l, k_l, v_l, r_l, z_l, gate_w, gate_b, norm_g, need_ctx):
    def heads(z, dh):
        return z.astype(F32).reshape(z.shape[0], z.shape[1], GLA_HEADS, dh).transpose(0, 2, 1, 3)

    def gate(z, d):
        zz = z.astype(F32)[..., d * GLA_RANK:(d + 1) * GLA_RANK]
        gl = jax.nn.log_sigmoid(zz @ gate_w[d].astype(F32) + gate_b[d].astype(F32)) / GLA_TAU
        return heads(gl, GLA_DK)

    scale = GLA_DK ** -0.5
    qc, kc, vc = heads(q_c, GLA_DK) * scale, heads(k_c, GLA_DK), heads(v_c, GLA_DV)
    ql, kl, vl = heads(q_l, GLA_DK) * scale, heads(k_l, GLA_DK), heads(v_l, GLA_DV)
    fl = lambda z: jnp.flip(z, axis=2)
    oc_f, sc_f = gla_chunked(qc, kc, vc, gate(z_c, 0), None, need_ctx)
    ol_f, _ = gla_chunked(ql, kl, vl, gate(z_l, 0), sc_f, True)
    oc_b, sc_b = gla_chunked(fl(qc), fl(kc), fl(vc), fl(gate(z_c, 1)), None, need_ctx)
    ol_b, _ = gla_chunked(fl(ql), fl(kl), fl(vl), fl(gate(z_l, 1)), sc_b, True)

    def out(o, r):
        o = rms_norm(o.transpose(0, 2, 1, 3), norm_g.reshape(GLA_HEADS, GLA_DV))
        return (o.reshape(o.shape[0], o.shape[1], MIX_HALF) * jax.nn.silu(r.astype(F32))).astype(r.dtype)

    y_l = out(ol_f + fl(ol_b), r_l)
    y_c = out(oc_f + fl(oc_b), r_c) if need_ctx else None
    return y_c, y_l


def rope_1d(x, pos):
    n = x.shape[-1] // 2
    inv = ROPE_BASE ** (-jnp.arange(n, dtype=F32) / n)
    ang = pos.astype(F32)[:, None] * inv[None, :]
    cos = jnp.cos(ang)[:, None, None, :]
    sin = jnp.sin(ang)[:, None, None, :]
    x1, x2 = x[..., :n], x[..., n:]
    return jnp.concatenate([x1 * cos - x2 * sin, x1 * sin + x2 * cos], axis=-1)


def axial_rope(x, row_pos, col_pos):
    half = x.shape[-1] // 2
    return jnp.concatenate([rope_1d(x[..., :half], row_pos), rope_1d(x[..., half:], col_pos)],
                           axis=-1).astype(x.dtype)


def diff_mixer(q_c, k_c, v_c, q_l, k_l, v_l, lq1, lk1, lq2, lk2, norm_g, lam_init, row_pos, col_pos, need_ctx):
    b, t, _ = q_l.shape
    qk = lambda z: z.reshape(z.shape[0], z.shape[1], DIFF_HEADS, 2, DIFF_DH)
    to_bh = lambda z: z.transpose(0, 2, 3, 1, 4)
    vh = lambda z: z.reshape(z.shape[0], z.shape[1], DIFF_HEADS, DIFF_DV).transpose(0, 2, 1, 3)
    scale = DIFF_DH ** -0.5
    ql = to_bh(axial_rope(qk(q_l * scale), row_pos, col_pos))
    kl = to_bh(axial_rope(qk(k_l), row_pos, col_pos))
    kc, vc = to_bh(qk(k_c)), vh(v_c)
    k_all = jnp.concatenate([kc, kl], axis=3)
    v_all = jnp.concatenate([vc, vh(v_l)], axis=2)
    lam = (jnp.exp(jnp.sum(lq1.astype(F32) * lk1.astype(F32)))
           - jnp.exp(jnp.sum(lq2.astype(F32) * lk2.astype(F32))) + lam_init)

    def attend(qb, kk, vv):
        p = jax.nn.softmax(jnp.einsum('bhmqd,bhmkd->bhmqk', qb, kk).astype(F32), axis=-1)
        w = p[:, :, 0] - lam * p[:, :, 1]
        return jnp.einsum('bhqk,bhkd->bhqd', w.astype(vv.dtype), vv)

    nb = t // DIFF_BLOCK
    q_blocks = ql.reshape(b, DIFF_HEADS, 2, nb, DIFF_BLOCK, DIFF_DH).transpose(3, 0, 1, 2, 4, 5)
    o = lax.map(lambda qb: attend(qb, k_all, v_all), q_blocks)
    o_l = o.transpose(1, 0, 3, 2, 4).reshape(b, t, DIFF_HEADS, DIFF_DV)

    def post(o):
        o32 = rms_norm(o, norm_g) * (1.0 - lam_init)
        return o32.reshape(o.shape[0], o.shape[1], MIX_HALF).astype(q_l.dtype)

    y_l = post(o_l)
    y_c = post(attend(to_bh(qk(q_c * scale)), kc, vc).transpose(0, 2, 1, 3)) if need_ctx else None
    return y_c, y_l


def odd_mixer(a_c, a_l, w_in, w_out, gate_w, gate_b, gla_g, lq1, lk1, lq2, lk2, diff_g, lam_init,
              row_pos, col_pos, need_ctx):
    gq_c, gk_c, gv_c, gr_c, gz_c, dq_c, dk_c, dv_c = jnp.split(a_c @ w_in, _cuts(ODD_SPLITS), axis=-1)
    gq_l, gk_l, gv_l, gr_l, gz_l, dq_l, dk_l, dv_l = jnp.split(a_l @ w_in, _cuts(ODD_SPLITS), axis=-1)
    g_c, g_l = gla_mixer(gq_c, gk_c, gv_c, gr_c, gz_c, gq_l, gk_l, gv_l, gr_l, gz_l, gate_w, gate_b, gla_g, need_ctx)
    d_c, d_l = diff_mixer(dq_c, dk_c, dv_c, dq_l, dk_l, dv_l, lq1, lk1, lq2, lk2, diff_g, lam_init,
                          row_pos, col_pos, need_ctx)
    y_l = jnp.concatenate([g_l, d_l], axis=-1) @ w_out
    y_c = jnp.concatenate([g_c, d_c], axis=-1) @ w_out if need_ctx else None
    return y_c, y_l


def hier_moe(h, w_grp, b_grp, w_exp, b_exp, w_gate, w_up, w_down):
    lg = (h @ w_grp + b_grp).astype(F32)
    g_sel = jnp.argmax(lg, axis=-1)
    p_grp = jnp.max(jax.nn.softmax(lg, axis=-1), axis=-1, keepdims=True)
    le = (h @ w_exp + b_exp).astype(F32)
    le = le.reshape(le.shape[:-1] + (N_GROUPS, EXP_PER_GROUP))
    le_in = jnp.sum(le * jax.nn.one_hot(g_sel, N_GROUPS, dtype=F32)[..., None], axis=-2)
    top_v, top_i = lax.top_k(le_in, TOP_K)
    w_top = jax.nn.softmax(top_v, axis=-1) * p_grp
    eid = g_sel[..., None] * EXP_PER_GROUP + top_i
    combine = jnp.sum(jax.nn.one_hot(eid, N_EXPERTS, dtype=F32) * w_top[..., None], axis=-2).astype(h.dtype)
    y = jnp.zeros_like(h)
    for gi in range(N_GROUPS):
        e0, e1 = gi * EXP_PER_GROUP, (gi + 1) * EXP_PER_GROUP
        hg = jnp.einsum('btd,edf->btef', h, w_gate[e0:e1])
        hu = jnp.einsum('btd,edf->btef', h, w_up[e0:e1])
        act = jax.nn.silu(hg) * hu * combine[..., e0:e1, None]
        y = y + jnp.einsum('btef,efd->btd', act, w_down[e0:e1])
    return y


def setup_inputs(seed: int = 0) -> dict:
    key = jax.random.key(seed)
    keys = iter(jax.random.split(key, 64))

    def nrm(shape, std):
        return jax.random.normal(next(keys), shape, F32) * std

    D = D_MODEL
    ne, no = (DEPTH + 1) // 2, DEPTH // 2
    beta = (8.0 * DEPTH) ** -0.25
    n_idx = jnp.arange(S5_STATE, dtype=F32)
    return {
        'x': nrm((BATCH, SEQ, D), 1.0),
        'c': nrm((BATCH, D), 1.0),
        'ctx': nrm((BATCH, CTX_LEN, D), 1.0),
        'c_ctx': nrm((D,), 1.0),
        'mod_w': nrm((DEPTH, D, 6 * D), D ** -0.5),
        'mod_b': nrm((DEPTH, 6 * D), 0.01),
        'ln_g': 1.0 + nrm((DEPTH, 2, D), 0.01),
        'ln_b': nrm((DEPTH, 2, D), 0.01),
        'even_w_in': nrm((ne, D, EVEN_IN), D ** -0.5),
        'even_w_out': nrm((ne, D, D), beta * D ** -0.5),
        's5_lam_re': -0.5 + nrm((ne, 2, S5_GROUPS, S5_STATE), 0.01),
        's5_lam_im': math.pi * n_idx + nrm((ne, 2, S5_GROUPS, S5_STATE), 0.01),
        's5_log_dt': jax.random.uniform(next(keys), (ne, 2, S5_GROUPS), F32, math.log(1e-3), math.log(1e-1)),
        's5_b_re': nrm((ne, 2, S5_GROUPS, S5_STATE, S5_GROUP), (2 * S5_GROUP) ** -0.5),
        's5_b_im': nrm((ne, 2, S5_GROUPS, S5_STATE, S5_GROUP), (2 * S5_GROUP) ** -0.5),
        's5_c_re': nrm((ne, 2, S5_GROUPS, S5_GROUP, S5_STATE), S5_STATE ** -0.5),
        's5_c_im': nrm((ne, 2, S5_GROUPS, S5_GROUP, S5_STATE), S5_STATE ** -0.5),
        's5_d': nrm((ne, MIX_HALF), 1.0),
        's5_glu_w': nrm((ne, MIX_HALF, 2 * MIX_HALF), MIX_HALF ** -0.5),
        's5_glu_b': nrm((ne, 2 * MIX_HALF), 0.01),
        'na_rpb': nrm((ne, NA_HEADS, 2 * NA_WIN_ROWS - 1, 2 * NA_WIN_COLS - 1), 0.02),
        'odd_w_in': nrm((no, D, ODD_IN), D ** -0.5),
        'odd_w_out': nrm((no, D, D), beta * D ** -0.5),
        'gla_gate_w': nrm((no, 2, GLA_RANK, GLA_QK), GLA_RANK ** -0.5),
        'gla_gate_b': nrm((no, 2, GLA_QK), 0.01),
        'gla_norm_g': 1.0 + nrm((no, MIX_HALF), 0.01),
        'diff_lq1': nrm((no, DIFF_DH), 0.1),
        'diff_lk1': nrm((no, DIFF_DH), 0.1),
        'diff_lq2': nrm((no, DIFF_DH), 0.1),
        'diff_lk2': nrm((no, DIFF_DH), 0.1),
        'diff_norm_g': 1.0 + nrm((no, DIFF_DV), 0.01),
        'moe_w_grp': nrm((DEPTH, D, N_GROUPS), D ** -0.5),
        'moe_b_grp': nrm((DEPTH, N_GROUPS), 0.01),
        'moe_w_exp': nrm((DEPTH, D, N_EXPERTS), D ** -0.5),
        'moe_b_exp': nrm((DEPTH, N_EXPERTS), 0.01),
        'moe_w_gate': nrm((DEPTH, N_EXPERTS, D, D_EXPERT), D ** -0.5),
        'moe_w_up': nrm((DEPTH, N_EXPERTS, D, D_EXPERT), D ** -0.5),
        'moe_w_down': nrm((DEPTH, N_EXPERTS, D_EXPERT, D), beta * D_EXPERT ** -0.5),
    }


def reference(x, c, ctx, c_ctx, mod_w, mod_b, ln_g, ln_b, even_w_in, even_w_out, s5_lam_re, s5_lam_im,
              s5_log_dt, s5_b_re, s5_b_im, s5_c_re, s5_c_im, s5_d, s5_glu_w, s5_glu_b, na_rpb,
              odd_w_in, odd_w_out, gla_gate_w, gla_gate_b, gla_norm_g, diff_lq1, diff_lk1, diff_lq2,
              diff_lk2, diff_norm_g, moe_w_grp, moe_b_grp, moe_w_exp, moe_b_exp, moe_w_gate, moe_w_up,
              moe_w_down):
    t = jnp.arange(x.shape[1])
    row_pos, col_pos = t // GRID_W, t % GRID_W
    alpha = (2.0 * DEPTH) ** 0.25
    s_lat = jax.nn.silu(c)
    s_ctx = jax.nn.silu(c_ctx)
    h_l, h_c = x, ctx
    for i in range(DEPTH):
        need_ctx = i < DEPTH - 1
        m_l = jnp.split(s_lat @ mod_w[i] + mod_b[i], 6, axis=-1)
        sh1_l, sc1_l, g1_l, sh2_l, sc2_l, g2_l = [m[:, None, :] for m in m_l]
        sh1_c, sc1_c, g1_c, sh2_c, sc2_c, g2_c = jnp.split(s_ctx @ mod_w[i] + mod_b[i], 6, axis=-1)
        a_l = h_l * (1.0 + sc1_l) + sh1_l
        a_c = h_c * (1.0 + sc1_c) + sh1_c
        j = i // 2
        if i % 2 == 0:
            y_c, y_l = even_mixer(a_c, a_l, even_w_in[j], even_w_out[j], s5_lam_re[j], s5_lam_im[j],
                                  s5_log_dt[j], s5_b_re[j], s5_b_im[j], s5_c_re[j], s5_c_im[j], s5_d[j],
                                  s5_glu_w[j], s5_glu_b[j], na_rpb[j], need_ctx)
        else:
            lam_init = 0.8 - 0.6 * math.exp(-0.3 * i)
            y_c, y_l = odd_mixer(a_c, a_l, odd_w_in[j], odd_w_out[j], gla_gate_w[j], gla_gate_b[j],
                                 gla_norm_g[j], diff_lq1[j], diff_lk1[j], diff_lq2[j], diff_lk2[j],
                                 diff_norm_g[j], lam_init, row_pos, col_pos, need_ctx)
        h_l = layer_norm(alpha * h_l + g1_l * y_l, ln_g[i, 0], ln_b[i, 0])
        f_l = hier_moe(h_l * (1.0 + sc2_l) + sh2_l, moe_w_grp[i], moe_b_grp[i], moe_w_exp[i], moe_b_exp[i],
                       moe_w_gate[i], moe_w_up[i], moe_w_down[i])
        h_l = layer_norm(alpha * h_l + g2_l * f_l, ln_g[i, 1], ln_b[i, 1])
        if need_ctx:
            h_c = layer_norm(alpha * h_c + g1_c * y_c, ln_g[i, 0], ln_b[i, 0])
            f_c = hier_moe(h_c * (1.0 + sc2_c) + sh2_c, moe_w_grp[i], moe_b_grp[i], moe_w_exp[i], moe_b_exp[i],
                           moe_w_gate[i], moe_w_up[i], moe_w_down[i])
            h_c = layer_norm(alpha * h_c + g2_c * f_c, ln_g[i, 1], ln_b[i, 1])
    return h_l
```

```python
import contextlib
import math
import numpy as np
import concourse.bass as bass
import concourse.mybir as mybir
from concourse.bass_utils import run_bass_kernel_spmd
from concourse.ap import AP

F32 = mybir.dt.float32
BF16 = mybir.dt.bfloat16
I32 = mybir.dt.int32
ALU = mybir.AluOpType
AF = mybir.ActivationFunctionType
AX = mybir.AxisListType

D = 1024
KD = 8
NEXP = 32
FE = 256
EPS = 1e-5
DEPTH = 4
ALPHA = (2.0 * DEPTH) ** 0.25
EPS_LN = EPS / (ALPHA * ALPHA)


class Sched:
    def __init__(self, nc, es, nlanes=4):
        self.nc = nc
        self.eng = {'pe': nc.tensor, 'act': nc.scalar, 'dve': nc.vector, 'pool': nc.gpsimd, 'sp': nc.sync}
        self.semobj = {}
        self.cnt = {}
        for e in self.eng:
            self.semobj[e] = es.enter_context(nc.semaphore("s_" + e))
            self.cnt[e] = 0
        self.seen = {e: {} for e in self.eng}
        self.lanes = {}
        self.lane_rr = {}
        for q in ('sp', 'pool', 'act'):
            self.lanes[q] = []
            self.lane_rr[q] = 0
            for i in range(nlanes):
                key = ('lane', q, i)
                self.semobj[key] = es.enter_context(nc.semaphore("l_%s%d" % (q, i)))
                self.lanes[q].append([key, 0])
        self.lastw = {}
        self.readers = {}
        self.nops = 0

    def _wait(self, e, tok):
        key, val = tok
        if self.seen[e].get(key, 0) >= val:
            return
        self.seen[e][key] = val
        self.eng[e].wait_ge(self.semobj[key], val)

    def _deps(self, r, w):
        d = set()
        for k in list(r) + list(w):
            t = self.lastw.get(k)
            if t is not None:
                d.add(t)
        for k in w:
            for t in self.readers.get(k, ()):
                d.add(t)
        return d

    def _commit(self, r, w, tok):
        for k in w:
            self.lastw[k] = tok
            self.readers[k] = []
        for k in r:
            self.readers.setdefault(k, []).append(tok)

    def op(self, e, fn, r=(), w=()):
        for t in self._deps(r, w):
            if e == 'pe' and t[0] == 'pe':
                continue
            self._wait(e, t)
        ins = fn(self.eng[e])
        self.cnt[e] += 1
        ins.then_inc(self.semobj[e], 1)
        self._commit(r, w, (e, self.cnt[e]))
        self.nops += 1

    def dma(self, q, out, in_, r=(), w=()):
        lanes = self.lanes[q]
        i = self.lane_rr[q]
        self.lane_rr[q] = (i + 1) % len(lanes)
        lane = lanes[i]
        if lane[1] > 0:
            self._wait(q, (lane[0], 16 * lane[1]))
        for t in self._deps(r, w):
            self._wait(q, t)
        self.eng[q].dma_start(out=out, in_=in_).then_inc(self.semobj[lane[0]], 16)
        lane[1] += 1
        self._commit(r, w, (lane[0], 16 * lane[1]))
        self.nops += 1

    def finish(self):
        for q in self.lanes:
            for lane in self.lanes[q]:
                if lane[1] > 0:
                    self.nc.sync.wait_ge(self.semobj[lane[0]], 16 * lane[1])


class Ctx:
    def __init__(self, name="k"):
        self.nc = bass.Bass("TRN2", target_bir_lowering=False)
        self.es0 = contextlib.ExitStack()
        self.es = self.es0
        self.S = Sched(self.nc, self.es0)
        self.n = 0
        self.prefix = ""
        self.alias = {}
        self.fused = False

    def begin(self, prefix, alias):
        self.prefix = prefix
        self.alias = dict(alias)
        self.es = contextlib.ExitStack()

    def end(self):
        phase_barrier(self.S)
        self.es.close()
        self.es = self.es0
        self.prefix = ""
        self.alias = {}

    def din(self, name, shape, dt=F32):
        if name in self.alias:
            return self.alias[name]
        return self.nc.dram_tensor(self.prefix + name, list(shape), dt, kind="ExternalInput").ap()

    def dout(self, name, shape, dt=F32):
        if name in self.alias:
            return self.alias[name]
        return self.nc.dram_tensor(self.prefix + name, list(shape), dt, kind="ExternalOutput").ap()

    def scratch(self, name, shape, dt=F32):
        return self.nc.dram_tensor(name, list(shape), dt, kind="Internal").ap()

    def sb(self, shape, dt=F32, name=None):
        self.n += 1
        return self.es.enter_context(self.nc.sbuf_tensor(name or ("t%d" % self.n), list(shape), dt))

    def ps(self, shape, dt=F32, name=None):
        self.n += 1
        return self.es.enter_context(self.nc.psum_tensor(name or ("p%d" % self.n), list(shape), dt))

    def close(self):
        if self.fused:
            return None
        self.S.finish()
        self.es0.close()
        return self.nc

    def close_all(self):
        self.S.finish()
        self.es0.close()
        return self.nc


def phase_barrier(S):
    for e in S.eng:
        for e2 in S.eng:
            if e2 != e and S.cnt[e2] > 0:
                S._wait(e, (e2, S.cnt[e2]))
        for q in S.lanes:
            for lane in S.lanes[q]:
                if lane[1] > 0:
                    S._wait(e, (lane[0], 16 * lane[1]))
    S.lastw = {}
    S.readers = {}


def mm(S, out, lhsT, rhs, start, stop, r, w):
    S.op('pe', lambda e: e.matmul(out, lhsT, rhs, start=start, stop=stop), r=r, w=w)


def act(S, out, in_, func, r, w, bias=None, scale=None, accum_out=None):
    kw = {}
    if bias is not None:
        kw['bias'] = bias
    if scale is not None:
        kw['scale'] = scale
    if accum_out is not None:
        kw['accum_out'] = accum_out
    S.op('act', lambda e: e.activation(out=out, in_=in_, func=func, **kw), r=r, w=w)


def tt(S, eng, out, in0, in1, op, r, w):
    S.op(eng, lambda e: e.tensor_tensor(out=out, in0=in0, in1=in1, op=op), r=r, w=w)


def ts(S, eng, out, in0, s1, s2, op0, op1, r, w):
    if s2 is None:
        S.op(eng, lambda e: e.tensor_scalar(out=out, in0=in0, scalar1=s1, scalar2=None, op0=op0), r=r, w=w)
    else:
        S.op(eng, lambda e: e.tensor_scalar(out=out, in0=in0, scalar1=s1, scalar2=s2, op0=op0, op1=op1), r=r, w=w)


def stt(S, out, in0, scalar, in1, op0, op1, r, w):
    S.op('dve', lambda e: e.scalar_tensor_tensor(out=out, in0=in0, scalar=scalar, in1=in1, op0=op0, op1=op1), r=r, w=w)


def ln_feature_major(C, x, xk, w, ps1, ps2, ones, tmp_sq, sqk, st, stk, cst):
    S = C.S
    for j in range(KD):
        act(S, tmp_sq[:, j, :w], x[:, j, :w], AF.Square, r=[xk + (j,)], w=[sqk + (j,)])
        mm(S, ps1[:, :w], ones[:, :], x[:, j, :w], j == 0, j == KD - 1, r=[xk + (j,), 'ones'], w=['ps4'])
        mm(S, ps2[:, :w], ones[:, :], tmp_sq[:, j, :w], j == 0, j == KD - 1, r=[sqk + (j,), 'ones'], w=['ps5'])
    mean, m2, var, rstd = st[:, 0, :w], st[:, 1, :w], st[:, 2, :w], st[:, 3, :w]
    act(S, mean, ps1[:, :w], AF.Copy, r=['ps4'], w=[stk + (0,)], scale=1.0 / D)
    act(S, m2, mean, AF.Square, r=[stk + (0,)], w=[stk + (1,)])
    stt(S, var, ps2[:, :w], 1.0 / D, m2, ALU.mult, ALU.subtract, r=['ps5', stk + (1,)], w=[stk + (2,)])
    act(S, var, var, AF.Sqrt, r=[stk + (2,), 'cst'], w=[stk + (2,)], bias=cst[:, 0:1])
    S.op('dve', lambda e: e.reciprocal(out=rstd, in_=var), r=[stk + (2,)], w=[stk + (3,)])
    for j in range(KD):
        tt(S, 'pool', x[:, j, :w], x[:, j, :w], mean, ALU.subtract, r=[xk + (j,), stk + (0,)], w=[xk + (j,)])
        tt(S, 'dve', x[:, j, :w], x[:, j, :w], rstd, ALU.mult, r=[xk + (j,), stk + (3,)], w=[xk + (j,)])
(V_G1L, V_G1C, V_SC2L, V_SC2C, V_SH2L, V_SH2C, V_G2L, V_G2C, V_LNG1, V_LNB1, V_LNG2, V_LNB2,
 V_GLUBV, V_GLUBG) = range(14)
NVB = 14


def build_B(ntok, chunks, groups, even, C=None, colmap=None):
    C = C or Ctx()
    gc = colmap or (lambda c0: c0)
    S = C.S
    nc = C.nc
    hT = C.din("hT", [D, ntok])
    mixT = C.din("mixT", [D, ntok])
    vecs_d = C.din("vecs", [128, NVB * KD])
    wout_d = C.din("w_out", [D, D])
    glw_d = C.din("glu_w", [512, D])
    wr_d = C.din("w_r", [D, 36])
    brbc_d = C.din("b_r_bc", [128, 36])
    wg_d = C.din("w_gate", [NEXP, D, FE])
    wu_d = C.din("w_up", [NEXP, D, FE])
    wd_d = C.din("w_down", [NEXP, FE, D])
    ident_d = C.din("ident", [128, 128])
    sel_d = C.din("sel", [32, NEXP * 128])
    outT = C.dout("outT", [D, ntok])

    gmax = max(sum(chunks[c][1] for c in g) for g in groups)
    WM = max(c[1] for c in chunks)

    vecs = C.sb([128, NVB + 3, KD])
    V_G1A, V_A2, V_G2A = NVB, NVB + 1, NVB + 2
    aux = C.sb([128, 3, KD])
    cst = C.sb([128, 4])
    ones = C.sb([128, 128])
    ident = C.sb([128, 128])
    sel = C.sb([32, NEXP * 128], BF16)
    wr = C.sb([128, KD, 36])
    brbc = C.sb([128, 36])
    NB = 3
    R = C.sb([128, max(KD * D + 4 * D, NB * (2 * KD * FE + 2 * D))], BF16)
    wout = R[:, 0:KD * D].rearrange("p (k n) -> p k n", k=KD)
    gluw = R[:, KD * D:KD * D + 4 * D].rearrange("p (k n) -> p k n", k=4)
    ESZ = 2 * KD * FE + 2 * D

    def wg_v(b):
        return R[:, b * ESZ:b * ESZ + KD * FE].rearrange("p (k f) -> p k f", k=KD)

    def wu_v(b):
        return R[:, b * ESZ + KD * FE:b * ESZ + 2 * KD * FE].rearrange("p (k f) -> p k f", k=KD)

    def wd_v(b):
        return R[:, b * ESZ + 2 * KD * FE:(b + 1) * ESZ].rearrange("p (c d) -> p c d", c=2)

    x2b = C.sb([128, KD, gmax], BF16)
    acc = C.sb([128, KD, gmax])
    combT = C.sb([32, gmax], BF16)
    hch = C.sb([128, KD, WM])
    tq = C.sb([128, KD, WM])
    sqt = C.sb([128, KD, WM])
    mT = C.sb([128, KD, WM], BF16)
    ys = C.sb([128, 4, WM])
    zT = C.sb([128, 4, WM], BF16)
    gtmp = C.sb([128, 2, WM])
    st = C.sb([128, 4, WM])
    sg = [C.sb([128, 2, WM]) for _ in range(2)]
    tmu = [C.sb([128, 2, WM]) for _ in range(2)]
    actb = [C.sb([128, 2, WM], BF16) for _ in range(2)]
    rt = C.sb([128, 160])
    P = [C.ps([128, 512]) for _ in range(8)]
    pk = ['ps%d' % i for i in range(8)]

    S.dma('sp', vecs[:, 0:NVB, :], vecs_d.rearrange("p (v k) -> p v k", v=NVB), w=['vecs'])
    S.dma('sp', ident[:, :], ident_d, w=['ident'])
    S.dma('sp', wr[:, :, :], wr_d.rearrange("(k p) n -> p k n", p=128), w=['wr'])
    S.dma('sp', brbc[:, :], brbc_d, w=['brbc'])
    S.dma('pool', sel[:, :], sel_d, w=['sel'])
    S.op('dve', lambda e: e.memset(ones[:, :], 1.0), w=['ones'])
    S.op('dve', lambda e: e.memset(cst[:, 0:1], EPS_LN), w=['cst'])
    ts(S, 'dve', vecs[:, V_G1A, :], vecs[:, V_G1L, :], 1.0 / ALPHA, None, ALU.mult, None, r=['vecs'], w=['vecs'])
    ts(S, 'dve', vecs[:, V_A2, :], vecs[:, V_SC2L, :], 1.0, None, ALU.add, None, r=['vecs'], w=['vecs'])
    ts(S, 'dve', vecs[:, V_G2A, :], vecs[:, V_G2L, :], 1.0 / ALPHA, None, ALU.mult, None, r=['vecs'], w=['vecs'])
    ts(S, 'dve', aux[:, 0, :], vecs[:, V_G1C, :], 1.0 / ALPHA, None, ALU.mult, None, r=['vecs'], w=['vecs'])
    ts(S, 'dve', aux[:, 1, :], vecs[:, V_SC2C, :], 1.0, None, ALU.add, None, r=['vecs'], w=['vecs'])
    ts(S, 'dve', aux[:, 2, :], vecs[:, V_G2C, :], 1.0 / ALPHA, None, ALU.mult, None, r=['vecs'], w=['vecs'])

    def col(v, j):
        return vecs[:, v, j:j + 1]

    for gi, g in enumerate(groups):
        g0 = chunks[g[0]][0]
        allw = [(nm, b_) for nm in ('wg', 'wu', 'wd') for b_ in range(NB)]
        S.dma('pool', wout, wout_d.rearrange("(k p) n -> p k n", p=128), w=['R'] + allw)
        if even:
            S.dma('pool', gluw, glw_d.rearrange("(k p) n -> p k n", p=128), w=['R2'] + allw)
        rk = ['R', 'R2']
        for ci in g:
            c0, w, isctx = chunks[ci]
            l0 = c0 - g0
            g1a = (lambda j: aux[:, 0, j:j + 1]) if isctx else (lambda j: col(V_G1A, j))
            a2 = (lambda j: aux[:, 1, j:j + 1]) if isctx else (lambda j: col(V_A2, j))
            sh2 = (lambda j: col(V_SH2C, j)) if isctx else (lambda j: col(V_SH2L, j))
            S.dma('sp', hch[:, :, :w], hT[:, gc(c0):gc(c0) + w].rearrange("(k p) t -> p k t", p=128), w=[('hch', j) for j in range(KD)])
            if even:
                S.dma('sp', ys[:, :, :w], mixT[0:512, gc(c0):gc(c0) + w].rearrange("(k p) t -> p k t", p=128), w=[('ys', k) for k in range(4)])
                S.dma('pool', mT[:, 4:8, :w], mixT[512:1024, gc(c0):gc(c0) + w].rearrange("(k p) t -> p k t", p=128), w=[('mT', k) for k in range(4, 8)])
                for k in range(4):
                    x = ys[:, k, :w]
                    t1 = gtmp[:, 0, :w]
                    act(S, t1, x, AF.Square, r=[('ys', k)], w=['gt0'])
                    ts(S, 'dve', t1, t1, 0.044715, 1.0, ALU.mult, ALU.add, r=['gt0'], w=['gt0'])
                    tt(S, 'dve', t1, t1, x, ALU.mult, r=['gt0', ('ys', k)], w=['gt0'])
                    act(S, t1, t1, AF.Sigmoid, r=['gt0'], w=['gt0'], scale=2.0 * math.sqrt(2.0 / math.pi))
                    tt(S, 'pool', zT[:, k, :w], x, t1, ALU.mult, r=['gt0', ('ys', k)], w=[('zT', k)])
                for j in range(4):
                    pv, pg = P[2 * (j % 2)], P[2 * (j % 2) + 1]
                    kv, kg = pk[2 * (j % 2)], pk[2 * (j % 2) + 1]
                    for k in range(4):
                        mm(S, pv[:, :w], gluw[:, k, j * 128:(j + 1) * 128], zT[:, k, :w], k == 0, k == 3, r=[('zT', k)] + rk, w=[kv])
                    for k in range(4):
                        mm(S, pg[:, :w], gluw[:, k, 512 + j * 128:512 + (j + 1) * 128], zT[:, k, :w], k == 0, k == 3, r=[('zT', k)] + rk, w=[kg])
                    t2 = gtmp[:, 1, :w]
                    act(S, t2, pg[:, :w], AF.Sigmoid, r=[kg, 'vecs'], w=['gt1'], bias=col(V_GLUBG, j))
                    stt(S, mT[:, j, :w], pv[:, :w], col(V_GLUBV, j), t2, ALU.add, ALU.mult, r=[kv, 'gt1', 'vecs'], w=[('mT', j)])
            else:
                S.dma('pool', mT[:, :, :w], mixT[:, gc(c0):gc(c0) + w].rearrange("(k p) t -> p k t", p=128), w=[('mT', k) for k in range(KD)])
            for j in range(KD):
                py, ky = P[j % 4], pk[j % 4]
                for k in range(KD):
                    mm(S, py[:, :w], wout[:, k, j * 128:(j + 1) * 128], mT[:, k, :w], k == 0, k == KD - 1, r=[('mT', k)] + rk, w=[ky])
                stt(S, tq[:, j, :w], py[:, :w], g1a(j), hch[:, j, :w], ALU.mult, ALU.add, r=[ky, ('hch', j), 'vecs'], w=[('tq', j)])
            ln_feature_major(C, tq, ('tq',), w, P[4], P[5], ones, sqt, ('sqt',), st, ('st',), cst)
            for j in range(KD):
                a_j = acc[:, j, l0:l0 + w]
                act(S, a_j, tq[:, j, :w], AF.Identity, r=[('tq', j), 'vecs'], w=[('acc', ci, j)], scale=col(V_LNG1, j), bias=col(V_LNB1, j))
                ts(S, 'dve', sqt[:, j, :w], a_j, a2(j), sh2(j), ALU.mult, ALU.add, r=[('acc', ci, j), 'vecs'], w=[('sqt', j)])
                S.op('pool', lambda e, j=j: e.tensor_copy(out=x2b[:, j, l0:l0 + w], in_=sqt[:, j, :w]), r=[('sqt', j)], w=[('x2b', ci, j)])
            for t0 in range(0, w, 128):
                pr, kr = P[6], pk[6]
                for k in range(KD):
                    mm(S, pr[:, 0:36], sqt[:, k, t0:t0 + 128], wr[:, k, :], k == 0, k == KD - 1, r=[('sqt', k), 'wr'], w=[kr])
                lg = rt[:, 0:36]
                tt(S, 'dve', lg, pr[:, 0:36], brbc[:, :], ALU.add, r=[kr, 'brbc'], w=['rt'])
                RK = dict(r=['rt'], w=['rt'])
                gm, ngm, oh, eg, gs, pgp = rt[:, 36:37], rt[:, 37:38], rt[:, 40:44], rt[:, 44:48], rt[:, 38:39], rt[:, 39:40]
                S.op('dve', lambda e: e.tensor_reduce(out=gm, in_=rt[:, 0:4], axis=AX.X, op=ALU.max), **RK)
                ts(S, 'dve', oh, rt[:, 0:4], gm, None, ALU.is_equal, None, **RK)
                ts(S, 'dve', ngm, gm, -1.0, None, ALU.mult, None, **RK)
                act(S, eg, rt[:, 0:4], AF.Exp, bias=ngm, accum_out=gs, **RK)
                S.op('dve', lambda e: e.reciprocal(out=pgp, in_=gs), **RK)
                lein, mk1, le2, mk2, c8 = rt[:, 48:56], rt[:, 56:64], rt[:, 64:72], rt[:, 72:80], rt[:, 80:88]
                ts(S, 'dve', lein, rt[:, 4:12], oh[:, 0:1], None, ALU.mult, None, **RK)
                for gq in range(1, 4):
                    stt(S, lein, rt[:, 4 + 8 * gq:12 + 8 * gq], oh[:, gq:gq + 1], lein, ALU.mult, ALU.add, **RK)
                m1, m2, dd, ed, w1, w2 = (rt[:, 88 + i:89 + i] for i in range(6))
                S.op('dve', lambda e: e.tensor_reduce(out=m1, in_=lein, axis=AX.X, op=ALU.max), **RK)
                ts(S, 'dve', mk1, lein, m1, None, ALU.is_equal, None, **RK)
                stt(S, le2, mk1, -1e30, lein, ALU.mult, ALU.add, **RK)
                S.op('dve', lambda e: e.tensor_reduce(out=m2, in_=le2, axis=AX.X, op=ALU.max), **RK)
                ts(S, 'dve', mk2, le2, m2, None, ALU.is_equal, None, **RK)
                tt(S, 'dve', dd, m2, m1, ALU.subtract, **RK)
                act(S, ed, dd, AF.Exp, **RK)
                ts(S, 'dve', w1, ed, 1.0, None, ALU.add, None, **RK)
                S.op('dve', lambda e: e.reciprocal(out=w1, in_=w1), **RK)
                tt(S, 'dve', w1, w1, pgp, ALU.mult, **RK)
                tt(S, 'dve', w2, ed, w1, ALU.mult, **RK)
                ts(S, 'dve', c8, mk1, w1, None, ALU.mult, None, **RK)
                stt(S, c8, mk2, w2, c8, ALU.mult, ALU.add, **RK)
                comb = rt[:, 96:128]
                for gq in range(4):
                    ts(S, 'dve', comb[:, 8 * gq:8 * gq + 8], c8, oh[:, gq:gq + 1], None, ALU.mult, None, **RK)
                pT, kT = P[7], pk[7]
                S.op('pe', lambda e: e.transpose(out=pT[0:32, 0:128], in_=comb, identity=ident[:, :]), r=['rt', 'ident'], w=[kT])
                act(S, combT[:, l0 + t0:l0 + t0 + 128], pT[0:32, 0:128], AF.Copy, r=[kT], w=[('combT', ci)])
        def load_exp(e):
            b = e % NB
            S.dma('pool', wg_v(b), wg_d[e].rearrange("(k p) f -> p k f", p=128), w=[('wg', b)] + rk)
            S.dma('pool', wu_v(b), wu_d[e].rearrange("(k p) f -> p k f", p=128), w=[('wu', b)] + rk)
            S.dma('pool', wd_v(b), wd_d[e].rearrange("(c p) d -> p c d", p=128), w=[('wd', b)] + rk)
        load_exp(0)
        load_exp(1)
        yrot = [0]
        items = [(e, ci) for e in range(NEXP) for ci in g]

        def stage_a(n, e, ci):
            b = e % NB
            wg, wu = wg_v(b), wu_v(b)
            c0, w, isctx = chunks[ci]
            l0 = c0 - g0
            q = n % 2
            pb, kb = P[4], pk[4]
            mm(S, pb[:, :w], sel[:, e * 128:(e + 1) * 128], combT[:, l0:l0 + w], True, True, r=['sel', ('combT', ci)], w=[kb])
            for fc in range(2):
                phg, phu = P[2 * fc], P[2 * fc + 1]
                kg_, ku_ = pk[2 * fc], pk[2 * fc + 1]
                for k in range(KD):
                    mm(S, phg[:, :w], wg[:, k, fc * 128:(fc + 1) * 128], x2b[:, k, l0:l0 + w], k == 0, k == KD - 1, r=[('wg', b), ('x2b', ci, k)], w=[kg_])
                for k in range(KD):
                    mm(S, phu[:, :w], wu[:, k, fc * 128:(fc + 1) * 128], x2b[:, k, l0:l0 + w], k == 0, k == KD - 1, r=[('wu', b), ('x2b', ci, k)], w=[ku_])
                act(S, sg[q][:, fc, :w], phg[:, :w], AF.Silu, r=[kg_], w=[('sg', q, fc)])
                tt(S, 'dve', tmu[q][:, fc, :w], phu[:, :w], sg[q][:, fc, :w], ALU.mult, r=[ku_, ('sg', q, fc)], w=[('tmu', q, fc)])
                tt(S, 'dve', actb[q][:, fc, :w], tmu[q][:, fc, :w], pb[:, :w], ALU.mult, r=[kb, ('tmu', q, fc)], w=[('actb', q, fc)])

        def stage_b(n, e, ci):
            b = e % NB
            wd = wd_v(b)
            c0, w, isctx = chunks[ci]
            l0 = c0 - g0
            q = n % 2
            g2a = (lambda j: aux[:, 2, j:j + 1]) if isctx else (lambda j: col(V_G2A, j))
            for j in range(KD):
                yi = 5 + (yrot[0] % 3)
                yrot[0] += 1
                pyj, kyj = P[yi], pk[yi]
                for fc in range(2):
                    mm(S, pyj[:, :w], wd[:, fc, j * 128:(j + 1) * 128], actb[q][:, fc, :w], fc == 0, fc == 1, r=[('wd', b), ('actb', q, fc)], w=[kyj])
                a_j = acc[:, j, l0:l0 + w]
                stt(S, a_j, pyj[:, :w], g2a(j), a_j, ALU.mult, ALU.add, r=[kyj, ('acc', ci, j), 'vecs'], w=[('acc', ci, j)])

        for n in range(len(items) + 1):
            if n < len(items):
                stage_a(n, *items[n])
            if n >= 1:
                stage_b(n - 1, *items[n - 1])
            if n < len(items) and items[n][1] == g[0] and items[n][0] + 2 < NEXP:
                load_exp(items[n][0] + 2)
        for ci in g:
            c0, w, isctx = chunks[ci]
            l0 = c0 - g0
            av = acc[:, :, l0:l0 + w]
            ln_feature_major(C, av, ('acc', ci), w, P[4], P[5], ones, sqt, ('sqt',), st, ('st',), cst)
            for j in range(KD):
                act(S, hch[:, j, :w], av[:, j, :], AF.Identity, r=[('acc', ci, j), 'vecs'], w=[('hch', j)], scale=col(V_LNG2, j), bias=col(V_LNB2, j))
            S.dma('sp', outT[:, gc(c0):gc(c0) + w].rearrange("(k p) t -> p k t", p=128), hch[:, :, :w], r=[('hch', j) for j in range(KD)])
    return C.close()
def fm(v):
    return np.ascontiguousarray(np.asarray(v, np.float32).reshape(KD, 128).T)


def b_consts():
    sel = np.zeros((32, NEXP * 128), np.float32)
    for e in range(NEXP):
        sel[e, e * 128:(e + 1) * 128] = 1.0
    return dict(ident=np.eye(128, dtype=np.float32), sel=sel)


def b_vecs(mod_lat, mod_ctx, ln_g, ln_b, glu_b):
    z = np.zeros(1024, np.float32)
    gb = np.asarray(glu_b, np.float32) if glu_b is not None else np.zeros(1024, np.float32)
    bv = z.copy(); bv[:512] = gb[:512]
    bg = z.copy(); bg[:512] = gb[512:]
    lst = [mod_lat[2], mod_ctx[2], mod_lat[4], mod_ctx[4], mod_lat[3], mod_ctx[3], mod_lat[5], mod_ctx[5],
           ln_g[0], ln_b[0], ln_g[1], ln_b[1], bv, bg]
    return np.ascontiguousarray(np.concatenate([fm(v) for v in lst], axis=1))
GLA_SC = 0.125
NCOL_ODD = 2080
(C_GQ, C_GK, C_GV, C_GR, C_GZ, C_DQ, C_DQP, C_DK, C_DKP, C_DV) = (0, 128, 256, 512, 768, 800, 1056, 1312, 1568, 1824)


def barrier(S):
    return phase_barrier(S)


def _old_barrier(S):
    for e in S.eng:
        for e2 in S.eng:
            if e2 != e and S.cnt[e2] > 0:
                S._wait(e, (e2, S.cnt[e2]))
        for q in S.lanes:
            for lane in S.lanes[q]:
                if lane[1] > 0:
                    S._wait(e, (lane[0], 16 * lane[1]))
    S.lastw = {}
    S.readers = {}


def rev_ap(ap2d, n):
    a = ap2d
    return AP(a.tensor, a.offset + (n - 1), [list(a.ap[0]), [-1, n]])


def modulate_aT(C, hT, vecs, aT, blocks, hb, hbk):
    S = C.S
    for (b0, bw, isctx) in blocks:
        S.dma('sp', hb[:, :, :bw], hT[:, b0:b0 + bw].rearrange("(k p) t -> p k t", p=128), w=[(hbk, j) for j in range(KD)])
        vsh, vsc = (2, 3) if isctx else (0, 1)
        for j in range(KD):
            if j % 2 == 0:
                act(S, aT[:, j, b0:b0 + bw], hb[:, j, :bw], AF.Identity, r=[(hbk, j), 'vecs'], w=[('aT', j, b0)],
                    scale=vecs[:, vsc, j:j + 1], bias=vecs[:, vsh, j:j + 1])
            else:
                ts(S, 'dve', aT[:, j, b0:b0 + bw], hb[:, j, :bw], vecs[:, vsc, j:j + 1], vecs[:, vsh, j:j + 1], ALU.mult, ALU.add,
                   r=[(hbk, j), 'vecs'], w=[('aT', j, b0)])


def build_A_odd(TC, TL, C=None, rowmap=None):
    T = TC + TL
    C = C or Ctx()
    rm = rowmap or (lambda r0: r0)
    S = C.S
    hT = C.din("hT", [D, T])
    vecs_d = C.din("vecsA", [128, 4 * KD])
    W_d = C.din("W", [D, NCOL_ODD])
    gw_d = C.din("gate_w_aug", [17, 256])
    gn_d = C.din("gla_g", [128, 2])
    dn_d = C.din("diff_g", [128, 1])
    lam_d = C.din("lam_rows", [128, 256])
    rope_d = C.din("rope", [64, 128])
    maskf_d = C.din("maskf", [64, 64])
    maskb_d = C.din("maskb", [64, 64])
    scanm_d = C.din("scanmask", [64, 512])
    ident_d = C.din("ident", [128, 128])
    selq_d = C.din("selq", [64, 65])
    li_d = C.din("lam_init", [128, 2])
    out = C.dout("out", [512, T])

    blocks = [(0, TC, True)] + [(TC + 512 * i, 512, False) for i in range(TL // 512)]
    NCH = T // 64
    NKC = T // 128

    vecs = C.sb([128, 4, KD])
    aT = C.sb([128, KD, T], BF16)
    W = C.sb([128, KD, NCOL_ODD], BF16)
    gw = C.sb([17, 256], BF16)
    gn = C.sb([128, 2])
    dn = C.sb([128, 1])
    lamr = C.sb([128, 256])
    lamc = C.sb([128, 8])
    li = C.sb([128, 2])
    rope = C.sb([64, 128])
    maskf = C.sb([64, 64])
    maskb = C.sb([64, 64])
    scanm = C.sb([64, 512])
    identb = C.sb([128, 128], BF16)
    selq = C.sb([64, 65])
    ones = C.sb([128, 128])
    onesb = C.sb([128, 128], BF16)
    cst = C.sb([128, 4])
    USZ = int(3.5 * T + T // 64) + 5800
    U = C.sb([128, USZ])
    P = [C.ps([128, 512]) for _ in range(8)]
    pk = ['ps%d' % i for i in range(8)]

    S.dma('sp', vecs[:, :, :], vecs_d.rearrange("p (v k) -> p v k", v=4), w=['vecs'])
    S.dma('pool', W[:, :, :], W_d.rearrange("(k p) n -> p k n", p=128), w=['W'])
    S.dma('pool', gw[:, :], gw_d, w=['gw'])
    S.dma('sp', gn[:, :], gn_d, w=['gn'])
    S.dma('sp', dn[:, :], dn_d, w=['dn'])
    S.dma('sp', lamr[:, :], lam_d, w=['lamr'])
    S.dma('sp', li[:, :], li_d, w=['li'])
    S.dma('sp', rope[:, :], rope_d, w=['rope'])
    S.dma('sp', maskf[:, :], maskf_d, w=['maskf'])
    S.dma('sp', maskb[:, :], maskb_d, w=['maskb'])
    S.dma('sp', scanm[:, :], scanm_d, w=['scanm'])
    S.dma('pool', identb[:, :], ident_d, w=['identb'])
    S.dma('sp', selq[:, :], selq_d, w=['selq'])
    S.op('dve', lambda e: e.memset(ones[:, :], 1.0), w=['ones'])
    S.op('dve', lambda e: e.memset(onesb[:, :], 1.0), w=['onesb'])
    S.op('dve', lambda e: e.memset(cst[:, 0:1], EPS), w=['cst'])
    S.op('dve', lambda e: e.memset(cst[:, 1:2], 1.0), w=['cst'])
    ts(S, 'dve', vecs[:, 1, :], vecs[:, 1, :], 1.0, None, ALU.add, None, r=['vecs'], w=['vecs'])
    ts(S, 'dve', vecs[:, 3, :], vecs[:, 3, :], 1.0, None, ALU.add, None, r=['vecs'], w=['vecs'])
    tt(S, 'dve', lamr[:, 0:64], lamr[:, 0:64], lamr[:, 64:128], ALU.mult, r=['lamr'], w=['lamr'])
    tt(S, 'dve', lamr[:, 128:192], lamr[:, 128:192], lamr[:, 192:256], ALU.mult, r=['lamr'], w=['lamr'])
    S.op('dve', lambda e: e.tensor_reduce(out=lamc[:, 0:1], in_=lamr[:, 0:64], axis=AX.X, op=ALU.add), r=['lamr'], w=['lamc'])
    S.op('dve', lambda e: e.tensor_reduce(out=lamc[:, 1:2], in_=lamr[:, 128:192], axis=AX.X, op=ALU.add), r=['lamr'], w=['lamc'])
    act(S, lamc[:, 2:4], lamc[:, 0:2], AF.Exp, r=['lamc'], w=['lamc'])
    tt(S, 'dve', lamc[:, 4:5], lamc[:, 2:3], lamc[:, 3:4], ALU.subtract, r=['lamc'], w=['lamc'])
    ts(S, 'dve', lamc[:, 5:6], lamc[:, 4:5], li[:, 0:1], -1.0, ALU.add, ALU.mult, r=['lamc', 'li'], w=['lamc'])
    tt(S, 'dve', dn[:, :], dn[:, :], li[:, 1:2], ALU.mult, r=['dn', 'li'], w=['dn'])

    off = [0]

    def carve(shape, dt=F32):
        n = int(np.prod(shape[1:]))
        words = n if dt == F32 else (n + 1) // 2
        o = off[0]
        off[0] += words
        v = U[0:shape[0], o:o + words]
        if dt != F32:
            v = v.bitcast(dt)[:, 0:n]
        if len(shape) == 3:
            v = v.rearrange("p (a b) -> p a b", a=shape[1])
        return v

    hb = carve([128, KD, 512])
    modulate_aT(C, hT, vecs, aT, blocks, hb, 'hb')

    WK = ['W'] + [('aT', j, b[0]) for j in range(KD) for b in blocks]

    def inproj(ps_out, kps, col0, ncol, t0, tw):
        for k in range(KD):
            mm(S, ps_out, W[:, k, col0:col0 + ncol], aT[:, k, t0:t0 + tw], k == 0, k == KD - 1, r=WK, w=[kps])

    barrier(S)
    off[0] = 0
    vtok = carve([64, NCH, 128], BF16)
    osum = carve([128, T])
    qdec = carve([64, T], BF16)
    kinc = carve([64, T], BF16)
    kdtok = carve([64, NCH, 64], BF16)
    decay = carve([64, NCH])
    zaug = carve([17, 512], BF16)
    xs = carve([64, 512])
    cs = carve([64, 512])
    eg = carve([64, 512])
    egi = carve([64, 512])
    ekd = carve([64, 512])
    kdT = carve([64, 512], BF16)
    attm = carve([64, 8, 64], BF16)
    Sf = carve([64, 128])
    Sb = [carve([64, 128], BF16), carve([64, 128], BF16)]
    fin = carve([128, 4, 512])
    assert off[0] <= USZ, (off[0], USZ)
    PTb = P[6][0:64, 0:256].bitcast(BF16).rearrange("p (a b) -> p a b", b=64)

    S.op('dve', lambda e: e.memset(zaug[:, :], 1.0), w=['zaug'])
    for hg in range(2):
        for n0 in range(0, NCH, 4):
            nn = min(4, NCH - n0)
            pv, kv = P[(n0 // 4) % 2], pk[(n0 // 4) % 2]
            pvv = pv[0:64, :].rearrange("p (a b) -> p a b", a=4)
            for n in range(nn):
                for k in range(KD):
                    mm(S, pvv[:, n, :], aT[:, k, (n0 + n) * 64:(n0 + n + 1) * 64], W[:, k, C_GV + hg * 128:C_GV + (hg + 1) * 128],
                       k == 0, k == KD - 1, r=WK, w=[kv])
            act(S, vtok[:, n0:n0 + nn, :], pvv[:, 0:nn, :], AF.Copy, r=[kv], w=[('vtok', n0)])
        VK = [('vtok', n0) for n0 in range(0, NCH, 4)]
        for d in range(2):
            for (b0, bw, isctx) in blocks:
                nch = bw // 64
                c0 = b0 // 64
                pq, pkk, pz, pg = P[2], P[3], P[4], P[5]
                inproj(pq[0:64, :bw], pk[2], C_GQ + hg * 64, 64, b0, bw)
                inproj(pkk[0:64, :bw], pk[3], C_GK + hg * 64, 64, b0, bw)
                inproj(pz[0:16, :bw], pk[4], C_GZ + d * 16, 16, b0, bw)
                act(S, zaug[0:16, :bw], pz[0:16, :bw], AF.Copy, r=[pk[4]], w=['zaug'])
                mm(S, pg[0:64, :bw], gw[0:17, d * 128 + hg * 64:d * 128 + (hg + 1) * 64], zaug[0:17, :bw], True, True, r=['gw', 'zaug'], w=[pk[5]])
                ts(S, 'dve', xs[:, :bw], pg[0:64, :bw], -80.0, None, ALU.max, None, r=[pk[5]], w=['xs'])
                act(S, xs[:, :bw], xs[:, :bw], AF.Exp, r=['xs'], w=['xs'], scale=-1.0)
                act(S, xs[:, :bw], xs[:, :bw], AF.Ln, r=['xs', 'cst'], w=['xs'], bias=cst[0:64, 1:2])
                if d == 0:
                    S.op('dve', lambda e: e.tensor_tensor_scan(out=cs[:, :bw], data0=scanm[:, :bw], data1=xs[:, :bw], initial=0.0,
                                                               op0=ALU.mult, op1=ALU.add), r=['xs', 'scanm'], w=['cs'])
                else:
                    S.op('dve', lambda e: e.tensor_tensor_scan(out=rev_ap(cs[:, :bw], bw), data0=scanm[:, :bw], data1=rev_ap(xs[:, :bw], bw),
                                                               initial=0.0, op0=ALU.mult, op1=ALU.add), r=['xs', 'scanm'], w=['cs'])
                cs3 = cs[:, :bw].rearrange("p (a b) -> p a b", b=64)
                gi = 63 if d == 0 else 0
                act(S, eg[:, :bw], cs[:, :bw], AF.Exp, r=['cs'], w=['eg'], scale=-1.0 / 16)
                act(S, egi[:, :bw], cs[:, :bw], AF.Exp, r=['cs'], w=['egi'], scale=1.0 / 16)
                act(S, decay[:, c0:c0 + nch], cs3[:, :, gi], AF.Exp, r=['cs'], w=[('decay', c0)], scale=-1.0 / 16)
                csb = cs[:, :]
                Gb = AP(csb.tensor, csb.offset + gi, [list(csb.ap[0]), [64, nch], [0, 64]])
                tt(S, 'dve', ekd[:, :bw].rearrange("p (a b) -> p a b", b=64), cs3, Gb, ALU.subtract, r=['cs'], w=['ekd'])
                act(S, ekd[:, :bw], ekd[:, :bw], AF.Exp, r=['ekd'], w=['ekd'], scale=1.0 / 16)
                stt(S, qdec[:, b0:b0 + bw], pq[0:64, :bw], GLA_SC, eg[:, :bw], ALU.mult, ALU.mult, r=[pk[2], 'eg'], w=[('qdec', b0)])
                tt(S, 'dve', kinc[:, b0:b0 + bw], pkk[0:64, :bw], egi[:, :bw], ALU.mult, r=[pk[3], 'egi'], w=[('kinc', b0)])
                tt(S, 'dve', kdT[:, :bw], pkk[0:64, :bw], ekd[:, :bw], ALU.mult, r=[pk[3], 'ekd'], w=['kdT'])
                for n in range(nch):
                    S.op('pe', lambda e: e.transpose(out=PTb[:, n, :], in_=kdT[:, n * 64:(n + 1) * 64], identity=identb[0:64, 0:64]),
                         r=['kdT', 'identb'], w=[pk[6]])
                act(S, kdtok[:, c0:c0 + nch, :], PTb[:, 0:nch, :], AF.Copy, r=[pk[6]], w=[('kdtok', c0)])
            S.op('dve', lambda e: e.memset(Sf[:, :], 0.0), w=['Sf'])
            S.op('dve', lambda e: e.memset(Sb[0][:, :], 0.0), w=[('Sb', 0)])
            cur = 0
            blk_order = [blocks[0]] + (blocks[1:] if d == 0 else blocks[1:][::-1])
            mask = maskf if d == 0 else maskb
            for (b0, bw, isctx) in blk_order:
                nch = bw // 64
                c0 = b0 // 64
                pa, pu0, pu1, po = P[0], P[1], P[6], P[7]
                pa3 = pa[0:64, :].rearrange("p (a b) -> p a b", b=64)
                for n in range(nch):
                    t0 = b0 + n * 64
                    mm(S, pa3[:, n, :], kinc[:, t0:t0 + 64], qdec[:, t0:t0 + 64], True, True, r=[('kinc', b0), ('qdec', b0)], w=[pk[0]])
                tt(S, 'dve', attm[:, 0:nch, :], pa3[:, 0:nch, :], AP(mask[:, :].tensor, mask[:, :].offset, [list(mask[:, :].ap[0]), [0, nch], [1, 64]]), ALU.mult,
                   r=[pk[0], 'maskf', 'maskb'], w=['attm'])
                pus = [pu0[0:64, :].rearrange("p (a b) -> p a b", b=128), pu1[0:64, :].rearrange("p (a b) -> p a b", b=128)]
                pukey = [pk[1], pk[6]]
                for n in range(nch):
                    mm(S, pus[n // 4][:, n % 4, :], kdtok[:, c0 + n, :], vtok[:, c0 + n, :], True, True, r=[('kdtok', c0)] + VK, w=[pukey[n // 4]])
                order = list(range(nch)) if d == 0 else list(range(nch))[::-1]
                for n in order:
                    t0 = b0 + n * 64
                    mm(S, po[:, n * 64:(n + 1) * 64], vtok[:, c0 + n, :], attm[:, n, :], True, False, r=VK + ['attm'], w=[pk[7]])
                    mm(S, po[:, n * 64:(n + 1) * 64], Sb[cur][:, :], qdec[:, t0:t0 + 64], False, True, r=[('Sb', cur), ('qdec', b0)], w=[pk[7]])
                    stt(S, Sf[:, :], Sf[:, :], decay[:, c0 + n:c0 + n + 1], pus[n // 4][:, n % 4, :], ALU.mult, ALU.add,
                        r=['Sf', ('decay', c0), pukey[n // 4]], w=['Sf'])
                    act(S, Sb[1 - cur][:, :], Sf[:, :], AF.Copy, r=['Sf'], w=[('Sb', 1 - cur)])
                    cur = 1 - cur
                if d == 0:
                    act(S, osum[:, b0:b0 + bw], po[:, :bw], AF.Copy, r=[pk[7]], w=[('osum', b0)])
                else:
                    tt(S, 'dve', osum[:, b0:b0 + bw], osum[:, b0:b0 + bw], po[:, :bw], ALU.add, r=[pk[7], ('osum', b0)], w=[('osum', b0)])
        for (b0, bw, isctx) in blocks:
            sq, rs, y, sr = fin[:, 0, :bw], fin[:, 1, :bw], fin[:, 2, :bw], fin[:, 3, :bw]
            act(S, sq, osum[:, b0:b0 + bw], AF.Square, r=[('osum', b0)], w=['fin0'])
            mm(S, P[2][:, :bw], ones[:, :], sq, True, True, r=['fin0', 'ones'], w=[pk[2]])
            act(S, rs, P[2][:, :bw], AF.Sqrt, r=[pk[2], 'cst'], w=['fin1'], scale=1.0 / 128, bias=cst[:, 0:1])
            S.op('dve', lambda e: e.reciprocal(out=rs, in_=rs), r=['fin1'], w=['fin1'])
            tt(S, 'dve', y, osum[:, b0:b0 + bw], rs, ALU.mult, r=[('osum', b0), 'fin1'], w=['fin2'])
            inproj(P[3][:, :bw], pk[3], C_GR + hg * 128, 128, b0, bw)
            act(S, sr, P[3][:, :bw], AF.Silu, r=[pk[3]], w=['fin3'])
            stt(S, y, y, gn[:, hg:hg + 1], sr, ALU.mult, ALU.mult, r=['fin2', 'fin3', 'gn'], w=['fin2'])
            S.dma('sp', out[rm(hg * 128):rm(hg * 128) + 128, b0:b0 + bw], y, r=['fin2'])

    barrier(S)
    off[0] = 0
    Qa = [carve([65, T], BF16), carve([65, T], BF16)]
    Ka = [carve([65, T], BF16), carve([65, T], BF16)]
    Vt = carve([128, NKC, 128], BF16)
    nrm = carve([65, T])
    kmx = carve([65, 4])
    t1 = carve([64, 512])
    t2 = carve([64, 512])
    kr = carve([64, 512])
    sqn = carve([64, 512])
    E = [carve([128, 512], BF16) for _ in range(3)]
    o1 = carve([128, 512])
    o2 = carve([128, 512])
    rl = carve([128, 512])
    assert off[0] <= USZ, (off[0], USZ)

    def rope_tab(which, b0, bw, isctx):
        r0 = (b0 - TC) // 64
        nr = bw // 64
        base = rope[:, :]
        pitch = base.ap[0][0]
        lo = AP(base.tensor, base.offset + which * 64 + r0, [[pitch, 32], [1, nr], [0, 64]])
        hi = AP(base.tensor, base.offset + 32 * pitch + which * 64, [[pitch, 32], [0, nr], [1, 64]])
        return lo, hi

    def qk_block(dst, col, colp, b0, bw, isctx, scale, m, key):
        p1, p2, pn = P[0], P[1], P[2]
        inproj(p1[0:64, :bw], pk[0], col, 64, b0, bw)
        if isctx:
            act(S, kr[:, :bw], p1[0:64, :bw], AF.Copy, r=[pk[0]], w=['kr'], scale=scale)
        else:
            inproj(p2[0:64, :bw], pk[1], colp, 64, b0, bw)
            clo, chi = rope_tab(0, b0, bw, isctx)
            slo, shi = rope_tab(1, b0, bw, isctx)
            v3 = lambda a, lo_: a[(0 if lo_ else 32):(32 if lo_ else 64), :bw].rearrange("p (a b) -> p a b", b=64)
            for lo_, ct, stb in ((True, clo, slo), (False, chi, shi)):
                stt(S, v3(t1, lo_), v3(p1, lo_), scale, ct, ALU.mult, ALU.mult, r=[pk[0], 'rope'], w=['t1'])
                stt(S, v3(t2, lo_), v3(p2, lo_), scale, stb, ALU.mult, ALU.mult, r=[pk[1], 'rope'], w=['t2'])
            tt(S, 'pool', kr[:, :bw], t1[:, :bw], t2[:, :bw], ALU.add, r=['t1', 't2'], w=['kr'])
        act(S, dst[0:64, b0:b0 + bw], kr[:, :bw], AF.Copy, r=['kr'], w=[(key, m, b0)])
        act(S, sqn[:, :bw], kr[:, :bw], AF.Square, r=['kr'], w=['sqn'])
        mm(S, pn[0:65, :bw], selq[:, :], sqn[:, :bw], True, True, r=['sqn', 'selq'], w=[pk[2]])
        act(S, nrm[64:65, b0:b0 + bw], pn[64:65, :bw], AF.Copy, r=[pk[2]], w=[('nrm', m, b0)])

    for hd in range(2):
        for m in range(2):
            for (b0, bw, isctx) in blocks:
                qk_block(Ka[m], C_DK + hd * 128 + m * 64, C_DKP + hd * 128 + m * 64, b0, bw, isctx, 1.0, m, 'Ka')
            S.op('dve', lambda e: e.tensor_reduce(out=kmx[64:65, m:m + 1], in_=nrm[64:65, :], axis=AX.X, op=ALU.max),
                 r=[('nrm', m, b[0]) for b in blocks], w=[('kmx', m)])
            act(S, kmx[64:65, m:m + 1], kmx[64:65, m:m + 1], AF.Sqrt, r=[('kmx', m)], w=[('kmx', m)])
            S.op('dve', lambda e: e.memset(Ka[m][64:65, :], 1.0), w=[('Ka1', m)])
            for (b0, bw, isctx) in blocks:
                qk_block(Qa[m], C_DQ + hd * 128 + m * 64, C_DQP + hd * 128 + m * 64, b0, bw, isctx, 0.125, m, 'Qa')
                act(S, nrm[64:65, b0:b0 + bw], nrm[64:65, b0:b0 + bw], AF.Sqrt, r=[('nrm', m, b0)], w=[('nrm', m, b0)])
                ts(S, 'dve', Qa[m][64:65, b0:b0 + bw], nrm[64:65, b0:b0 + bw], kmx[64:65, m:m + 1], -1.0, ALU.mult, ALU.mult,
                   r=[('nrm', m, b0), ('kmx', m)], w=[('Qa', m, b0)])
        for kc in range(NKC):
            pv, kv = P[3 + kc % 2], pk[3 + kc % 2]
            for k in range(KD):
                mm(S, pv[:, 0:128], aT[:, k, kc * 128:(kc + 1) * 128], W[:, k, C_DV + hd * 128:C_DV + (hd + 1) * 128], k == 0, k == KD - 1, r=WK, w=[kv])
            act(S, Vt[:, kc, :], pv[:, 0:128], AF.Copy, r=[kv], w=[('Vt', kc)])
        KQ = [('Ka', m, b[0]) for m in range(2) for b in blocks] + [('Ka1', m) for m in range(2)] + [('Qa', m, b[0]) for m in range(2) for b in blocks]
        items = []
        for (b0, bw, isctx) in blocks:
            kcs = list(range(TC // 128)) if isctx else list(range(NKC))
            for m in range(2):
                for ii, kc in enumerate(kcs):
                    items.append((b0, bw, m, ii, kc, len(kcs)))
        LA = 2

        def stage1(si, it):
            b0, bw, m, ii, kc, nk = it
            ps_, ks_ = P[si % 3], pk[si % 3]
            Et, ke = E[si % 3], ('E', si % 3)
            mm(S, ps_[:, :bw], Ka[m][0:65, kc * 128:(kc + 1) * 128], Qa[m][0:65, b0:b0 + bw], True, True, r=KQ, w=[ks_])
            act(S, Et[:, :bw], ps_[:, :bw], AF.Exp, r=[ks_], w=[ke])

        def stage2(si, it):
            b0, bw, m, ii, kc, nk = it
            Et, ke = E[si % 3], ('E', si % 3)
            pO, pL = P[3 + 2 * m], P[4 + 2 * m]
            kO, kL = pk[3 + 2 * m], pk[4 + 2 * m]
            mm(S, pO[:, :bw], Vt[:, kc, :], Et[:, :bw], ii == 0, ii == nk - 1, r=[('Vt', kc), ke], w=[kO])
            mm(S, pL[:, :bw], onesb[:, :], Et[:, :bw], ii == 0, ii == nk - 1, r=['onesb', ke], w=[kL])
            if ii != nk - 1:
                return
            S.op('dve', lambda e: e.reciprocal(out=rl[:, :bw], in_=pL[:, :bw]), r=[kL], w=['rl'])
            tt(S, 'dve', (o1 if m == 0 else o2)[:, :bw], pO[:, :bw], rl[:, :bw], ALU.mult, r=[kO, 'rl'], w=['o%d' % (m + 1)])
            if m == 0:
                return
            stt(S, o1[:, :bw], o2[:, :bw], lamc[:, 5:6], o1[:, :bw], ALU.mult, ALU.add, r=['o1', 'o2', 'lamc'], w=['o1'])
            act(S, o2[:, :bw], o1[:, :bw], AF.Square, r=['o1'], w=['o2'])
            mm(S, P[7][:, :bw], ones[:, :], o2[:, :bw], True, True, r=['o2', 'ones'], w=[pk[7]])
            act(S, rl[:, :bw], P[7][:, :bw], AF.Sqrt, r=[pk[7], 'cst'], w=['rl'], scale=1.0 / 128, bias=cst[:, 0:1])
            S.op('dve', lambda e: e.reciprocal(out=rl[:, :bw], in_=rl[:, :bw]), r=['rl'], w=['rl'])
            stt(S, o2[:, :bw], o1[:, :bw], dn[:, 0:1], rl[:, :bw], ALU.mult, ALU.mult, r=['o1', 'rl', 'dn'], w=['o2'])
            S.dma('sp', out[rm(256 + hd * 128):rm(256 + hd * 128) + 128, b0:b0 + bw], o2[:, :bw], r=['o2'])

        for idx in range(len(items) + LA):
            if idx < len(items):
                stage1(idx, items[idx])
            if idx - LA >= 0:
                stage2(idx - LA, items[idx - LA])
    return C.close()
ODD_SPLITS_ = (256, 256, 512, 512, 32, 512, 512, 512)


def perm64():
    Pm = np.arange(64)
    Pm[0:16] = np.arange(16, 32)
    Pm[16:32] = np.arange(0, 16)
    Pm[32:48] = np.arange(48, 64)
    Pm[48:64] = np.arange(32, 48)
    return Pm


def rope_table():
    inv = (10000.0 ** (-np.arange(16, dtype=np.float32) / 16)).astype(np.float32)
    pos = np.arange(64, dtype=np.float32)
    tab = np.zeros((64, 128), np.float32)
    for p in range(64):
        i = p % 16
        ang = (pos * inv[i]).astype(np.float32)
        tab[p, 0:64] = np.cos(ang)
        sgn = -1.0 if (p % 32) < 16 else 1.0
        tab[p, 64:128] = sgn * np.sin(ang)
    return tab


def a_odd_consts():
    j = np.arange(64)
    selq = np.zeros((64, 65), np.float32)
    selq[:, 64] = 1.0
    sm = np.ones((64, 512), np.float32)
    sm[:, ::64] = 0.0
    return dict(rope=rope_table(), maskf=(j[:, None] <= j[None, :]).astype(np.float32),
                maskb=(j[:, None] >= j[None, :]).astype(np.float32), scanmask=sm,
                ident=np.eye(128, dtype=np.float32), selq=selq)


def a_odd_params(half, w_in, gate_w, gate_b, gla_g, lq1, lk1, lq2, lk2, diff_g):
    offs = np.concatenate([[0], np.cumsum(ODD_SPLITS_)])
    heads = [2 * half, 2 * half + 1]
    Pm = perm64()
    cols = []
    for gh in heads:
        cols += list(offs[0] + gh * 64 + np.arange(64))
    for gh in heads:
        cols += list(offs[1] + gh * 64 + np.arange(64))
    for gh in heads:
        cols += list(offs[2] + gh * 128 + np.arange(128))
    for gh in heads:
        cols += list(offs[3] + gh * 128 + np.arange(128))
    cols += list(offs[4] + np.arange(32))
    for base, pm in ((offs[5], None), (offs[5], Pm), (offs[6], None), (offs[6], Pm)):
        for gh in heads:
            for m in range(2):
                idx = np.arange(64) if pm is None else pm
                cols += list(base + gh * 128 + m * 64 + idx)
    for gh in heads:
        cols += list(offs[7] + gh * 128 + np.arange(128))
    W = np.ascontiguousarray(np.asarray(w_in)[:, np.array(cols)])
    gcols = np.concatenate([gh * 64 + np.arange(64) for gh in heads])
    gwa = np.zeros((17, 256), np.float32)
    for d in range(2):
        gwa[0:16, d * 128:(d + 1) * 128] = np.asarray(gate_w)[d][:, gcols]
        gwa[16, d * 128:(d + 1) * 128] = np.asarray(gate_b)[d][gcols]
    gg = np.ascontiguousarray(np.asarray(gla_g).reshape(4, 128)[heads].T)
    lam = np.tile(np.concatenate([lq1, lk1, lq2, lk2])[None].astype(np.float32), (128, 1))
    return dict(W=W, gate_w_aug=gwa, gla_g=gg, diff_g=np.asarray(diff_g, np.float32).reshape(128, 1), lam_rows=lam)


def a_vecs(mod_lat, mod_ctx):
    return np.ascontiguousarray(np.concatenate([fm(mod_lat[0]), fm(mod_lat[1]), fm(mod_ctx[0]), fm(mod_ctx[1])], axis=1))


def a_even_consts():
    selq = np.zeros((64, 65), np.float32)
    selq[:, 64] = 1.0
    io = np.arange(1, S5L + 1, dtype=np.float32)
    iota = np.tile(np.concatenate([io, io[::-1]])[None], (128, 1))
    return dict(ident=np.eye(128, dtype=np.float32), selq=selq, iota=np.ascontiguousarray(iota))


def natten_bm(rpb, half):
    rpb = np.asarray(rpb, np.float32)
    bm = np.full((4, 128, 32 * 64), -30000.0, np.float32)
    w = np.arange(64)
    cs = np.clip(w - 8, 0, 48)
    for hh in range(4):
        h = 4 * half + hh
        for dl in range(8):
            for i in range(4):
                for jj in range(2):
                    dr = 2 * i + jj - dl + 7
                    for kcol in range(64):
                        ok = (kcol >= cs) & (kcol < cs + 16)
                        dc = kcol - w + 15
                        wi = w[ok]
                        bm[hh, jj * 64 + kcol, (dl * 4 + i) * 64 + wi] = rpb[h, dr, dc[ok]]
    return bm


def a_even_params(half, w_in, lam_re, lam_im, log_dt, b_re, b_im, c_re, c_im, d_skip, rpb):
    w_in = np.asarray(w_in)
    cols = list(256 * half + np.arange(256))
    for base in (512, 1024, 1536):
        cols += list(base + 256 * half + np.arange(256))
    W = np.ascontiguousarray(w_in[:, np.array(cols)])
    lam = np.zeros((128, 48), np.float32)
    brb = np.zeros((128, 16, 128), np.float32)
    bib = np.zeros((128, 16, 128), np.float32)
    cre = np.zeros((128, 16, 128), np.float32)
    cim = np.zeros((128, 16, 128), np.float32)
    for d in range(2):
        for j in range(8):
            s = d * 8 + j
            for gl in range(2):
                g = 16 * half + 2 * j + gl
                rows = slice(gl * 64, (gl + 1) * 64)
                lam[rows, s] = lam_re[d, g]
                lam[rows, 16 + s] = lam_im[d, g]
                lam[rows, 32 + s] = log_dt[d, g]
                c0 = 32 * (j % 4) + gl * 16
                brb[rows, s, c0:c0 + 16] = b_re[d, g]
                bib[rows, s, c0:c0 + 16] = b_im[d, g]
                cc = 32 * (j % 4) + gl * 16
                cre[rows, s, cc:cc + 16] = np.asarray(c_re[d, g]).T
                cim[rows, s, cc:cc + 16] = np.asarray(c_im[d, g]).T
    dsk = np.ascontiguousarray(np.asarray(d_skip, np.float32)[256 * half:256 * half + 256].reshape(2, 128).T)
    return dict(W=W, s5_lam=lam, s5_brb=brb.reshape(128, -1), s5_bib=bib.reshape(128, -1), s5_cre=cre.reshape(128, -1),
                s5_cim=cim.reshape(128, -1), s5_d=dsk, bm=natten_bm(rpb, half))
C_U, C_NQ, C_NK, C_NV = 0, 256, 512, 768
NCOL_EVEN = 1024
TWO_PI = 2.0 * math.pi
CW1 = 6.28125
CW2 = TWO_PI - CW1
S5L = 256


def reduce_angle(S, dst, src, kf, ki, key_src, key_dst, key_tmp):
    ts(S, 'dve', kf, src, 1.0 / TWO_PI, None, ALU.mult, None, r=[key_src], w=[key_tmp])
    S.op('dve', lambda e: e.tensor_copy(out=ki, in_=kf), r=[key_tmp], w=[key_tmp + 'i'])
    S.op('dve', lambda e: e.tensor_copy(out=kf, in_=ki), r=[key_tmp + 'i'], w=[key_tmp])
    stt(S, dst, kf, -CW1, src, ALU.mult, ALU.add, r=[key_tmp, key_src], w=[key_dst])
    stt(S, dst, kf, -CW2, dst, ALU.mult, ALU.add, r=[key_tmp, key_dst], w=[key_dst])
    ts(S, 'dve', kf, dst, math.pi, TWO_PI, ALU.is_gt, ALU.mult, r=[key_dst], w=[key_tmp])
    tt(S, 'dve', dst, dst, kf, ALU.subtract, r=[key_dst, key_tmp], w=[key_dst])
    ts(S, 'dve', kf, dst, -math.pi, TWO_PI, ALU.is_lt, ALU.mult, r=[key_dst], w=[key_tmp])
    tt(S, 'dve', dst, dst, kf, ALU.add, r=[key_dst, key_tmp], w=[key_dst])
    ts(S, 'dve', dst, dst, math.pi, -math.pi, ALU.min, ALU.max, r=[key_dst], w=[key_dst])


def build_A_even(TC, TL, stage=9, nst=16, C=None, rowmap=None):
    T = TC + TL
    ROWS = TL // 64
    C = C or Ctx()
    rm = rowmap or (lambda r0: r0)
    S = C.S
    hT = C.din("hT", [D, T])
    vecs_d = C.din("vecsA", [128, 4 * KD])
    W_d = C.din("W", [D, NCOL_EVEN])
    bm_d = C.din("bm", [4, 128, 32 * 64])
    ident_d = C.din("ident", [128, 128])
    selq_d = C.din("selq", [64, 65])
    lam_d = C.din("s5_lam", [128, 48])
    brb_d = C.din("s5_brb", [128, 16 * 128])
    bib_d = C.din("s5_bib", [128, 16 * 128])
    cre_d = C.din("s5_cre", [128, 16 * 128])
    cim_d = C.din("s5_cim", [128, 16 * 128])
    dsk_d = C.din("s5_d", [128, 2])
    iota_d = C.din("iota", [128, 2 * S5L])
    out = C.dout("out", [512, T])

    blocks = [(0, TC, True)] + [(TC + 512 * i, 512, False) for i in range(TL // 512)]
    NKC_C = TC // 128
    NKE = TL // 128
    NKO = TL // 128 - 1

    vecs = C.sb([128, 4, KD])
    uT = C.sb([128, 2, T], BF16)
    identb = C.sb([128, 128], BF16)
    identf = C.sb([128, 128])
    selq = C.sb([64, 65])
    onesb = C.sb([128, 128], BF16)
    cst = C.sb([128, 4])
    USZ = max(int(2.0 * T) + 4096 + 4096 + 12000, 41500)
    U = C.sb([128, USZ])
    P = [C.ps([128, 512]) for _ in range(8)]
    pk = ['ps%d' % i for i in range(8)]

    S.dma('sp', vecs[:, :, :], vecs_d.rearrange("p (v k) -> p v k", v=4), w=['vecs'])
    S.dma('pool', identb[:, :], ident_d, w=['identb'])
    S.dma('sp', identf[:, :], ident_d, w=['identf'])
    S.dma('sp', selq[:, :], selq_d, w=['selq'])
    S.op('dve', lambda e: e.memset(onesb[:, :], 1.0), w=['onesb'])
    S.op('dve', lambda e: e.memset(cst[:, 0:1], EPS), w=['cst'])
    ts(S, 'dve', vecs[:, 1, :], vecs[:, 1, :], 1.0, None, ALU.add, None, r=['vecs'], w=['vecs'])
    ts(S, 'dve', vecs[:, 3, :], vecs[:, 3, :], 1.0, None, ALU.add, None, r=['vecs'], w=['vecs'])

    off = [0]

    def carve(shape, dt=F32):
        n = int(np.prod(shape[1:]))
        words = n if dt == F32 else (n + 1) // 2
        o = off[0]
        off[0] += words
        assert off[0] <= USZ, (off[0], USZ)
        v = U[0:shape[0], o:o + words]
        if dt != F32:
            v = v.bitcast(dt)[:, 0:n]
        if len(shape) == 3:
            v = v.rearrange("p (a b) -> p a b", a=shape[1])
        return v

    aT = carve([128, KD, T], BF16)
    W = carve([128, KD, NCOL_EVEN], BF16)
    hb = carve([128, KD, 512])
    S.dma('pool', W[:, :, :], W_d.rearrange("(k p) n -> p k n", p=128), w=['W'])
    modulate_aT(C, hT, vecs, aT, blocks, hb, 'hb')
    barrier(S)
    off[0] -= KD * 512
    WK = []

    def inproj(ps_out, kps, col0, ncol, t0, tw):
        for k in range(KD):
            mm(S, ps_out, W[:, k, col0:col0 + ncol], aT[:, k, t0:t0 + tw], k == 0, k == KD - 1, r=WK, w=[kps])

    for (b0, bw, isctx) in blocks:
        for a in range(2):
            pu, ku = P[a], pk[a]
            inproj(pu[:, :bw], ku, C_U + a * 128, 128, b0, bw)
            act(S, uT[:, a, b0:b0 + bw], pu[:, :bw], AF.Copy, r=[ku], w=[('uT', a, b0)])

    if stage <= 1:
        return C.close()
    base_n = off[0]
    Qa = carve([65, T], BF16)
    Ka = carve([65, T], BF16)
    Vc = carve([128, NKC_C, 64], BF16)
    Ve = carve([128, NKE, 64], BF16)
    Vo = carve([128, max(NKO, 1), 64], BF16)
    BMt = carve([128, 32, 64], BF16)
    nrm = carve([65, 512])
    kmx = carve([65, 4])
    kr = carve([64, 512])
    sqn = carve([64, 512])
    E = [carve([128, 6, 64], BF16) for _ in range(3)]
    Ec = [carve([128, 512], BF16) for _ in range(2)]
    rl = carve([64, 512])
    ob = carve([64, 512])

    def qk_block(dst, col, b0, bw, scale, key):
        p1, pn = P[0], P[2]
        inproj(p1[0:64, :bw], pk[0], col, 64, b0, bw)
        act(S, kr[:, :bw], p1[0:64, :bw], AF.Copy, r=[pk[0]], w=['kr'], scale=scale)
        act(S, dst[0:64, b0:b0 + bw], kr[:, :bw], AF.Copy, r=['kr'], w=[(key, b0)])
        act(S, sqn[:, :bw], kr[:, :bw], AF.Square, r=['kr'], w=['sqn'])
        mm(S, pn[0:65, :bw], selq[:, :], sqn[:, :bw], True, True, r=['sqn', 'selq'], w=[pk[2]])
        act(S, nrm[64:65, :bw], pn[64:65, :bw], AF.Copy, r=[pk[2]], w=['nrm'])

    for hh in range(4):
        S.dma('pool', BMt[:, :, :], bm_d[hh].rearrange("p (a b) -> p a b", b=64), w=['BMt'])
        S.op('dve', lambda e: e.memset(kmx[64:65, 0:1], 0.0), w=['kmx'])
        for (b0, bw, isctx) in blocks:
            qk_block(Ka, C_NK + hh * 64, b0, bw, 1.0, 'Ka')
            S.op('dve', lambda e: e.tensor_reduce(out=kmx[64:65, 1:2], in_=nrm[64:65, :bw], axis=AX.X, op=ALU.max), r=['nrm'], w=['kmx1'])
            tt(S, 'dve', kmx[64:65, 0:1], kmx[64:65, 0:1], kmx[64:65, 1:2], ALU.max, r=['kmx', 'kmx1'], w=['kmx'])
        act(S, kmx[64:65, 0:1], kmx[64:65, 0:1], AF.Sqrt, r=['kmx'], w=['kmx'])
        S.op('dve', lambda e: e.memset(Ka[64:65, :], 1.0), w=['Ka1'])
        for (b0, bw, isctx) in blocks:
            qk_block(Qa, C_NQ + hh * 64, b0, bw, 0.125, 'Qa')
            act(S, nrm[64:65, :bw], nrm[64:65, :bw], AF.Sqrt, r=['nrm'], w=['nrm'])
            ts(S, 'dve', Qa[64:65, b0:b0 + bw], nrm[64:65, :bw], kmx[64:65, 0:1], -1.0, ALU.mult, ALU.mult, r=['nrm', 'kmx'], w=[('Qa', b0)])
        vi = 0
        for (dstV, vn, n, tbase) in ((Vc, 'Vc', NKC_C, 0), (Ve, 'Ve', NKE, TC), (Vo, 'Vo', NKO, TC + 64)):
            for kc in range(n):
                pv, kv = P[3 + vi % 2], pk[3 + vi % 2]
                vi += 1
                t0 = tbase + kc * 128
                for k in range(KD):
                    mm(S, pv[:, 0:64], aT[:, k, t0:t0 + 128], W[:, k, C_NV + hh * 64:C_NV + (hh + 1) * 64], k == 0, k == KD - 1, r=WK, w=[kv])
                act(S, dstV[:, kc, :], pv[:, 0:64], AF.Copy, r=[kv], w=[('V', vn, kc)])
        VK = [('V', vn, kc) for (vn, n) in (('Vc', NKC_C), ('Ve', NKE), ('Vo', NKO)) for kc in range(n)]
        KQ = [('Ka', b[0]) for b in blocks] + ['Ka1'] + [('Qa', b[0]) for b in blocks]
        pO, pL = P[5], P[6]
        for kc in range(NKC_C):
            ps_, ks_ = P[kc % 2], pk[kc % 2]
            mm(S, ps_[:, :TC], Ka[0:65, kc * 128:(kc + 1) * 128], Qa[0:65, 0:TC], True, True, r=KQ, w=[ks_])
            act(S, Ec[kc % 2][:, :TC], ps_[:, :TC], AF.Exp, r=[ks_], w=[('Ec', kc % 2)])
            mm(S, pO[0:64, :TC], Vc[:, kc, :], Ec[kc % 2][:, :TC], kc == 0, kc == NKC_C - 1, r=VK + [('Ec', kc % 2)], w=[pk[5]])
            mm(S, pL[0:64, :TC], onesb[:, 0:64], Ec[kc % 2][:, :TC], kc == 0, kc == NKC_C - 1, r=['onesb', ('Ec', kc % 2)], w=[pk[6]])
        S.op('dve', lambda e: e.reciprocal(out=rl[:, :TC], in_=pL[0:64, :TC]), r=[pk[6]], w=['rl'])
        tt(S, 'dve', ob[:, :TC], pO[0:64, :TC], rl[:, :TC], ALU.mult, r=[pk[5], 'rl'], w=['ob'])
        S.dma('sp', out[rm(256 + hh * 64):rm(256 + hh * 64) + 64, 0:TC], ob[:, :TC], r=['ob'])
        LA = 2
        rowinfo = {}

        def n_stage1(r):
            rs = min(max(r - 4, 0), ROWS - 8)
            dl = r - rs
            q0 = TC + 64 * r
            ps_, ks_ = P[r % 3], pk[r % 3]
            Et, ke = E[r % 3], ('E', r % 3)
            ps3 = ps_[:, 0:384].rearrange("p (a b) -> p a b", b=64)
            chunks = []
            for i in range(4):
                k0 = TC + 64 * rs + 128 * i
                mm(S, ps3[:, i, :], Ka[0:65, k0:k0 + 128], Qa[0:65, q0:q0 + 64], True, False, r=KQ, w=[ks_])
                mm(S, ps3[:, i, :], identb[:, :], BMt[:, dl * 4 + i, :], False, True, r=['identb', 'BMt'], w=[ks_])
                if rs % 2 == 0:
                    chunks.append(Ve[:, rs // 2 + i, :])
                else:
                    chunks.append(Vo[:, (rs - 1) // 2 + i, :])
            for kc in range(NKC_C):
                mm(S, ps3[:, 4 + kc, :], Ka[0:65, kc * 128:(kc + 1) * 128], Qa[0:65, q0:q0 + 64], True, True, r=KQ, w=[ks_])
                chunks.append(Vc[:, kc, :])
            nck = 4 + NKC_C
            act(S, Et[:, 0:nck, :], ps3[:, 0:nck, :], AF.Exp, r=[ks_], w=[ke])
            rowinfo[r] = chunks

        def n_stage2(r):
            r0 = r - (r % 4)
            rr = r % 4
            pOL, kOL = P[5 + (r0 // 4) % 2], pk[5 + (r0 // 4) % 2]
            pol4 = pOL[0:64, :].rearrange("p (r a b) -> p r a b", r=4, a=2)
            Et, ke = E[r % 3], ('E', r % 3)
            chunks = rowinfo.pop(r)
            nck = 4 + NKC_C
            for i in range(nck):
                mm(S, pol4[:, rr, 0, :], chunks[i], Et[:, i, :], i == 0, i == nck - 1, r=VK + [ke], w=[kOL])
            for i in range(nck):
                mm(S, pol4[:, rr, 1, :], onesb[:, 0:64], Et[:, i, :], i == 0, i == nck - 1, r=['onesb', ke], w=[kOL])
            if rr != 3:
                return
            c4 = (r0 % 8) * 64
            rl4 = rl[:, c4:c4 + 256].rearrange("p (r b) -> p r b", b=64)
            ob4 = ob[:, c4:c4 + 256].rearrange("p (r b) -> p r b", b=64)
            S.op('dve', lambda e: e.reciprocal(out=rl4, in_=pol4[:, :, 1, :]), r=[kOL], w=['rl'])
            tt(S, 'dve', ob4, pol4[:, :, 0, :], rl4, ALU.mult, r=[kOL, 'rl'], w=['ob'])
            if r0 % 8 == 4 or r0 + 4 >= ROWS:
                t0 = TC + 64 * (r0 - (r0 % 8))
                nw = 64 * ((r0 % 8) + 4)
                S.dma('sp', out[rm(256 + hh * 64):rm(256 + hh * 64) + 64, t0:t0 + nw], ob[:, 0:nw], r=['ob'])

        for r in range(ROWS + LA):
            if r < ROWS:
                n_stage1(r)
            if r - LA >= 0:
                n_stage2(r - LA)

    if stage <= 2:
        return C.close()
    barrier(S)
    off[0] = 0
    L = S5L
    ysum = carve([128, 2, T])
    cosT = carve([128, 16, L])
    sinT = carve([128, 16, L])
    BT = [carve([128, 16, 128], BF16), carve([128, 16, 128], BF16)]
    CT = [carve([128, 16, 128], BF16), carve([128, 16, 128], BF16), carve([128, 16, 128], BF16)]
    negI = carve([128, 128])
    cry = carve([128, 2, 4])
    lam = carve([128, 3, 16])
    sc = carve([128, 16, 16])
    hprev = carve([128, 16, 2])
    dsk = carve([128, 2])
    iota = carve([128, 2, L])
    wk = carve([128, 16, L])
    wkB = carve([128, 16, L])
    hbf = carve([128, 8, L], BF16)
    stg0 = off[0]
    brb = carve([128, 16, 128])
    bib = carve([128, 16, 128])
    cst32 = carve([128, 2, 16 * 128])
    kI = U[:, off[0]:off[0] + L].bitcast(I32)
    off[0] += L
    assert off[0] <= USZ, (off[0], USZ)

    S.dma('sp', lam[:, :, :], lam_d.rearrange("p (a b) -> p a b", a=3), w=['lam'])
    S.dma('sp', brb[:, :, :], brb_d.rearrange("p (a b) -> p a b", a=16), w=['brb'])
    S.dma('sp', bib[:, :, :], bib_d.rearrange("p (a b) -> p a b", a=16), w=['bib'])
    S.dma('sp', cst32[:, 0, :], cre_d, w=['c32'])
    S.dma('sp', cst32[:, 1, :], cim_d, w=['c32'])
    S.dma('sp', dsk[:, :], dsk_d, w=['dsk'])
    S.dma('sp', iota[:, :, :], iota_d.rearrange("p (a b) -> p a b", a=2), w=['iota'])
    S.op('dve', lambda e: e.tensor_copy(out=CT[0][:, :, :], in_=cst32[:, 0, :].rearrange("p (a b) -> p a b", b=128)), r=['c32'], w=['CT'])
    ts(S, 'dve', CT[1][:, :, :], cst32[:, 1, :].rearrange("p (a b) -> p a b", b=128), -1.0, None, ALU.mult, None, r=['c32'], w=['CT'])
    ts(S, 'dve', CT[2][:, :, :], cst32[:, 0, :].rearrange("p (a b) -> p a b", b=128), -1.0, None, ALU.mult, None, r=['c32'], w=['CT'])
    ts(S, 'dve', negI[:, :], identf[:, :], -1.0, None, ALU.mult, None, r=['identf'], w=['negI'])
    if stage <= 2.2:
        return C.close()
    (Q_DT, Q_LR, Q_TH, Q_R, Q_THR, Q_SIN, Q_COS, Q_ARE, Q_AIM, Q_DEN, Q_FRE, Q_FIM, Q_T1, Q_T2, Q_NFIM, Q_KF) = range(16)
    q = lambda i: sc[:, i, :]
    SK = dict(r=['sc', 'lam'], w=['sc'])
    act(S, q(Q_DT), lam[:, 2, :], AF.Exp, **SK)
    tt(S, 'dve', q(Q_LR), lam[:, 0, :], q(Q_DT), ALU.mult, **SK)
    tt(S, 'dve', q(Q_TH), lam[:, 1, :], q(Q_DT), ALU.mult, **SK)
    act(S, q(Q_R), q(Q_LR), AF.Exp, **SK)
    kI16 = kI[:, 0:16]
    reduce_angle(S, q(Q_THR), q(Q_TH), q(Q_KF), kI16, 'sc', 'sc', 'sc')
    act(S, q(Q_SIN), q(Q_THR), AF.Sin, **SK)
    ts(S, 'dve', q(Q_T1), q(Q_THR), math.pi / 2, None, ALU.add, None, **SK)
    reduce_angle(S, q(Q_T2), q(Q_T1), q(Q_KF), kI16, 'sc', 'sc', 'sc')
    act(S, q(Q_COS), q(Q_T2), AF.Sin, **SK)
    tt(S, 'dve', q(Q_ARE), q(Q_R), q(Q_COS), ALU.mult, **SK)
    tt(S, 'dve', q(Q_AIM), q(Q_R), q(Q_SIN), ALU.mult, **SK)
    tt(S, 'dve', q(Q_T1), lam[:, 0, :], lam[:, 0, :], ALU.mult, **SK)
    tt(S, 'dve', q(Q_T2), lam[:, 1, :], lam[:, 1, :], ALU.mult, **SK)
    tt(S, 'dve', q(Q_DEN), q(Q_T1), q(Q_T2), ALU.add, **SK)
    S.op('dve', lambda e: e.reciprocal(out=q(Q_DEN), in_=q(Q_DEN)), **SK)
    ts(S, 'dve', q(Q_ARE), q(Q_ARE), -1.0, None, ALU.add, None, **SK)
    tt(S, 'dve', q(Q_T1), q(Q_ARE), lam[:, 0, :], ALU.mult, **SK)
    tt(S, 'dve', q(Q_T2), q(Q_AIM), lam[:, 1, :], ALU.mult, **SK)
    tt(S, 'dve', q(Q_FRE), q(Q_T1), q(Q_T2), ALU.add, **SK)
    tt(S, 'dve', q(Q_FRE), q(Q_FRE), q(Q_DEN), ALU.mult, **SK)
    tt(S, 'dve', q(Q_T1), q(Q_AIM), lam[:, 0, :], ALU.mult, **SK)
    tt(S, 'dve', q(Q_T2), q(Q_ARE), lam[:, 1, :], ALU.mult, **SK)
    tt(S, 'dve', q(Q_FIM), q(Q_T1), q(Q_T2), ALU.subtract, **SK)
    tt(S, 'dve', q(Q_FIM), q(Q_FIM), q(Q_DEN), ALU.mult, **SK)
    ts(S, 'dve', q(Q_NFIM), q(Q_FIM), -1.0, None, ALU.mult, None, **SK)
    if stage <= 2.5:
        return C.close()
    for s in range(nst):
        d, j = s // 8, s % 8
        pb = 64 * ((j % 4) // 2)
        col = lambda i: sc[:, i, s:s + 1]
        for part, (x0, f0, x1, f1) in enumerate(((brb, Q_FRE, bib, Q_NFIM), (bib, Q_FRE, brb, Q_FIM))):
            tmpb = wk[:, part, 0:128]
            ts(S, 'dve', tmpb, x0[:, s, :], col(f0), None, ALU.mult, None, r=['sc', 'brb', 'bib'], w=[('wk', part)])
            stt(S, tmpb, x1[:, s, :], col(f1), tmpb, ALU.mult, ALU.add, r=['sc', 'brb', 'bib', ('wk', part)], w=[('wk', part)])
            S.op('pe', lambda e: e.transpose(out=P[part][:, 0:128], in_=tmpb, identity=identf[:, :]), r=[('wk', part), 'identf'], w=[pk[part]])
            act(S, BT[part][:, s, :], P[part][:, 0:128], AF.Copy, r=[pk[part]], w=['BT'])
        ang, ang2, kf = wk[:, 2, :], wk[:, 3, :], wk[:, 4, :]
        ts(S, 'dve', ang, iota[:, d, :], col(Q_THR), None, ALU.mult, None, r=['sc', 'iota'], w=[('wk', 2)])
        reduce_angle(S, ang2, ang, kf, kI[:, 0:L], ('wk', 2), ('wk', 3), 'kf')
        act(S, sinT[:, s, :], ang2, AF.Sin, r=[('wk', 3)], w=[('tab', s)])
        ts(S, 'dve', ang, ang, math.pi / 2, None, ALU.add, None, r=[('wk', 2)], w=[('wk', 2)])
        reduce_angle(S, ang2, ang, kf, kI[:, 0:L], ('wk', 2), ('wk', 3), 'kf')
        act(S, cosT[:, s, :], ang2, AF.Sin, r=[('wk', 3)], w=[('tab', s)])
        if stage <= 2.8 and s == nst - 1:
            return C.close()
    off[0] = stg0
    barrier(S)

    if stage <= 3:
        return C.close()
    chunks_f = [(0, TC)] if TC <= L else [(i, L) for i in range(0, TC, L)]
    lat_ch = [(TC + i, L) for i in range(0, TL, L)]
    items = []
    for d in range(2):
        order = (chunks_f + lat_ch) if d == 0 else (chunks_f[::-1] + lat_ch[::-1])
        for ci_, (t0, Lc) in enumerate(order):
            for a in range(2):
                for jj in range(4):
                    items.append((d, t0, Lc, a, jj, ci_ == 0))

    def geom(n):
        d, t0, Lc, a, jj, first = items[n]
        j = 4 * a + jj
        s = d * 8 + j
        wsel = n % 2
        wkc = wk if wsel == 0 else wkB
        if d == 0:
            cs_, sn_ = cosT[:, s, 0:Lc], sinT[:, s, 0:Lc]
        else:
            cs_, sn_ = cosT[:, s, L - Lc:L], sinT[:, s, L - Lc:L]
        return d, t0, Lc, a, jj, first, s, wsel, wkc, cs_, sn_

    def front(n):
        d, t0, Lc, a, jj, first, s, wsel, wkc, cs_, sn_ = geom(n)
        pb = 64 * (jj // 2)
        X, kX = P[n % 2], pk[n % 2]
        V, kV = P[4 + n % 2], pk[4 + n % 2]
        A_, B_ = X[:, 0:Lc], X[:, 256:256 + Lc]
        mm(S, A_, BT[0][:, s, :], uT[:, a, t0:t0 + Lc], True, True, r=['BT', ('uT', a)], w=[kX])
        mm(S, B_, BT[1][:, s, :], uT[:, a, t0:t0 + Lc], True, True, r=['BT', ('uT', a)], w=[kX])
        w_ = lambda i: wkc[:, i, 0:Lc]
        tt(S, 'dve', w_(0), A_, cs_, ALU.mult, r=[kX], w=[('wk', wsel, 0)])
        tt(S, 'dve', w_(1), B_, sn_, ALU.mult, r=[kX], w=[('wk', wsel, 1)])
        tt(S, 'dve', w_(2), B_, cs_, ALU.mult, r=[kX], w=[('wk', wsel, 2)])
        tt(S, 'dve', w_(3), A_, sn_, ALU.mult, r=[kX], w=[('wk', wsel, 3)])
        mm(S, V[:, 0:Lc], identf[:, :], w_(0), True, False, r=['identf', ('wk', wsel, 0)], w=[kV])
        mm(S, V[:, 0:Lc], identf[:, :], w_(1), False, True, r=['identf', ('wk', wsel, 1)], w=[kV])
        mm(S, V[:, 256:256 + Lc], identf[:, :], w_(2), True, False, r=['identf', ('wk', wsel, 2)], w=[kV])
        mm(S, V[:, 256:256 + Lc], negI[:, :], w_(3), False, True, r=['negI', ('wk', wsel, 3)], w=[kV])

    def back(n):
        d, t0, Lc, a, jj, first, s, wsel, wkc, cs_, sn_ = geom(n)
        pb = 64 * (jj // 2)
        py, kpy = P[2 + a], pk[2 + a]
        V, kV = P[4 + n % 2], pk[4 + n % 2]
        w_ = lambda i: wkc[:, i, 0:Lc]
        rcol = sc[:, Q_R, s:s + 1]
        rbc = AP(rcol.tensor, rcol.offset, [list(rcol.ap[0]), [0, Lc]])
        for part in range(2):
            src, dst = V[:, 256 * part:256 * part + Lc], w_(6 + part)
            ini = 0.0 if first else hprev[:, s, part:part + 1]
            if d == 0:
                S.op('dve', lambda e: e.tensor_tensor_scan(out=dst, data0=rbc, data1=src, initial=ini, op0=ALU.mult, op1=ALU.add),
                     r=[kV, ('hprev', s)], w=[('wk', wsel, 6 + part)])
            else:
                S.op('dve', lambda e: e.tensor_tensor_scan(out=rev_ap(dst, Lc), data0=rbc, data1=rev_ap(src, Lc), initial=ini,
                                                           op0=ALU.mult, op1=ALU.add), r=[kV, ('hprev', s)], w=[('wk', wsel, 6 + part)])
        gre, gim = w_(6), w_(7)
        q = 4 * (n % 2)
        u_ = lambda i: hbf[:, q + i, 0:Lc]
        tt(S, 'pool', u_(0), gre, cs_, ALU.mult, r=[('wk', wsel, 6)], w=[('hbf', q + 0)])
        tt(S, 'pool', u_(1), gim, sn_, ALU.mult, r=[('wk', wsel, 7)], w=[('hbf', q + 1)])
        tt(S, 'dve', u_(2), gre, sn_, ALU.mult, r=[('wk', wsel, 6)], w=[('hbf', q + 2)])
        tt(S, 'dve', u_(3), gim, cs_, ALU.mult, r=[('wk', wsel, 7)], w=[('hbf', q + 3)])
        lastc = Lc - 1 if d == 0 else 0
        gl = lambda i: wkc[:, i, lastc:lastc + 1]
        cc, sn1 = cs_[:, lastc:lastc + 1], sn_[:, lastc:lastc + 1]
        ck = ('cry', n % 2)
        cr = lambda i: cry[:, n % 2, i:i + 1]
        act(S, cr(0), gl(6), AF.Identity, r=[('wk', wsel, 6)], w=[ck], scale=cc)
        act(S, cr(1), gl(7), AF.Identity, r=[('wk', wsel, 7)], w=[ck], scale=sn1)
        act(S, hprev[:, s, 0:1], cr(1), AF.Identity, r=[ck], w=[('hprev', s)], scale=-1.0, bias=cr(0))
        act(S, cr(2), gl(6), AF.Identity, r=[('wk', wsel, 6)], w=[ck], scale=sn1)
        act(S, hprev[:, s, 1:2], gl(7), AF.Identity, r=[ck, ('wk', wsel, 7)], w=[('hprev', s)], scale=cc, bias=cr(2))
        mm(S, py[:, 0:Lc], CT[0][:, s, :], u_(0), jj == 0, False, r=['CT', ('hbf', q + 0)], w=[kpy])
        mm(S, py[:, 0:Lc], CT[2][:, s, :], u_(1), False, False, r=['CT', ('hbf', q + 1)], w=[kpy])
        mm(S, py[:, 0:Lc], CT[1][:, s, :], u_(2), False, False, r=['CT', ('hbf', q + 2)], w=[kpy])
        mm(S, py[:, 0:Lc], CT[1][:, s, :], u_(3), False, jj == 3, r=['CT', ('hbf', q + 3)], w=[kpy])
        if jj != 3:
            return
        ys_ = ysum[:, a, t0:t0 + Lc]
        if d == 0:
            stt(S, ys_, uT[:, a, t0:t0 + Lc], dsk[:, a:a + 1], py[:, 0:Lc], ALU.mult, ALU.add, r=[kpy, ('uT', a), 'dsk'], w=[('ys', a, t0)])
        else:
            tt(S, 'dve', ys_, ys_, py[:, 0:Lc], ALU.add, r=[kpy, ('ys', a, t0)], w=[('ys', a, t0)])
            S.dma('sp', out[rm(a * 128):rm(a * 128) + 128, t0:t0 + Lc], ys_, r=[('ys', a, t0)])

    for n in range(len(items) + 1):
        if n < len(items):
            front(n)
        if n >= 1:
            back(n - 1)
    return C.close()
def build_M(ncols):
    C = Ctx()
    S = C.S
    cT_d = C.din("cT", [128, KD * 5])
    w_d = C.din("w", [D, ncols])
    b_d = C.din("b", [5, ncols])
    out = C.dout("out", [5, ncols])
    cT = C.sb([128, KD, 5])
    sg = C.sb([128, KD, 5])
    wt = [C.sb([128, KD, 512]) for _ in range(2)]
    bt = C.sb([5, ncols])
    ot = C.sb([5, ncols])
    P = [C.ps([128, 512]) for _ in range(2)]
    S.dma('sp', cT[:, :, :], cT_d.rearrange("p (k c) -> p k c", k=KD), w=['cT'])
    S.dma('sp', bt[:, :], b_d, w=['bt'])
    act(S, sg[:, :, :], cT[:, :, :], AF.Sigmoid, r=['cT'], w=['sg'])
    tt(S, 'dve', cT[:, :, :], cT[:, :, :], sg[:, :, :], ALU.mult, r=['cT', 'sg'], w=['cT'])
    for i in range(ncols // 512):
        w_ = wt[i % 2]
        S.dma('sp' if i % 2 == 0 else 'act', w_[:, :, :], w_d[:, i * 512:(i + 1) * 512].rearrange("(k p) n -> p k n", p=128), w=[('w', i % 2)])
        for k in range(KD):
            mm(S, P[i % 2][0:5, :], cT[:, k, :], w_[:, k, :], k == 0, k == KD - 1, r=['cT', ('w', i % 2)], w=[('p', i % 2)])
        tt(S, 'dve', ot[:, i * 512:(i + 1) * 512], P[i % 2][0:5, :], bt[:, i * 512:(i + 1) * 512], ALU.add, r=[('p', i % 2), 'bt'], w=['ot'])
    S.dma('sp', out, ot[:, :], r=['ot'])
    return C.close()
TC_FULL, TL_FULL, NB = 256, 4096, 4
_PROG = {}


def _prog(name, fn):
    if name not in _PROG:
        _PROG[name] = fn()
    return _PROG[name]


def _run(nc, in_maps):
    in_maps = [{k: np.ascontiguousarray(v, dtype=np.float32) for k, v in m.items()} for m in in_maps]
    return run_bass_kernel_spmd(nc, in_maps, core_ids=list(range(8))).results


def kernel_unfused(**I):
    I = {k: np.asarray(v) for k, v in I.items()}
    T = TC_FULL + TL_FULL
    c_all = np.concatenate([I['c'], I['c_ctx'][None]], 0).astype(np.float32)
    cT = np.ascontiguousarray(c_all.reshape(5, KD, 128).transpose(2, 1, 0).reshape(128, KD * 5))
    maps = []
    for c in range(8):
        li, hf = c // 2, c % 2
        maps.append(dict(cT=cT, w=I['mod_w'][li][:, hf * 3072:(hf + 1) * 3072],
                         b=np.tile(I['mod_b'][li][None, hf * 3072:(hf + 1) * 3072], (5, 1))))
    r = _run(_prog('M', lambda: build_M(3072)), maps)
    mods = [np.concatenate([r[2 * li]['out'], r[2 * li + 1]['out']], 1).reshape(5, 6, D) for li in range(DEPTH)]
    hT = [np.ascontiguousarray(np.concatenate([I['ctx'][b], I['x'][b]], 0).T) for b in range(NB)]
    chunksB = [(0, 128, True)] + [(128 + 512 * i, 512, False) for i in range(4)]
    groupsB = [[0, 1, 2], [3, 4]]
    idxB = [np.concatenate([np.arange(128) + 128 * th, TC_FULL + 2048 * th + np.arange(2048)]) for th in range(2)]
    bc = b_consts()
    for li in range(DEPTH):
        j = li // 2
        even = li % 2 == 0
        maps = []
        for c in range(8):
            b, hf = c // 2, c % 2
            m = dict(hT=hT[b], vecsA=a_vecs(mods[li][b], mods[li][4]))
            if even:
                m.update(a_even_consts())
                m.update(a_even_params(hf, I['even_w_in'][j], I['s5_lam_re'][j], I['s5_lam_im'][j], I['s5_log_dt'][j], I['s5_b_re'][j],
                                       I['s5_b_im'][j], I['s5_c_re'][j], I['s5_c_im'][j], I['s5_d'][j], I['na_rpb'][j]))
            else:
                lam0 = 0.8 - 0.6 * math.exp(-0.3 * li)
                m.update(a_odd_consts())
                m.update(a_odd_params(hf, I['odd_w_in'][j], I['gla_gate_w'][j], I['gla_gate_b'][j], I['gla_norm_g'][j], I['diff_lq1'][j],
                                      I['diff_lk1'][j], I['diff_lq2'][j], I['diff_lk2'][j], I['diff_norm_g'][j]))
                m['lam_init'] = np.tile(np.array([[lam0, 1.0 - lam0]], np.float32), (128, 1))
            maps.append(m)
        if even:
            r = _run(_prog('Ae', lambda: build_A_even(TC_FULL, TL_FULL)), maps)
        else:
            r = _run(_prog('Ao', lambda: build_A_odd(TC_FULL, TL_FULL)), maps)
        mix = []
        for b in range(NB):
            o0, o1 = r[2 * b]['out'], r[2 * b + 1]['out']
            mix.append(np.concatenate([o0[0:256], o1[0:256], o0[256:512], o1[256:512]], 0))
        w_r = np.concatenate([I['moe_w_grp'][li], I['moe_w_exp'][li]], 1)
        b_r = np.tile(np.concatenate([I['moe_b_grp'][li], I['moe_b_exp'][li]])[None], (128, 1))
        w_out = I['even_w_out'][j] if even else I['odd_w_out'][j]
        glu_w = I['s5_glu_w'][j] if even else np.zeros((512, D), np.float32)
        glu_b = I['s5_glu_b'][j] if even else None
        maps = []
        for c in range(8):
            b, th = c // 2, c % 2
            maps.append(dict(hT=hT[b][:, idxB[th]], mixT=mix[b][:, idxB[th]],
                             vecs=b_vecs(mods[li][b], mods[li][4], I['ln_g'][li], I['ln_b'][li], glu_b),
                             w_out=w_out, glu_w=glu_w, w_r=w_r, b_r_bc=b_r, w_gate=I['moe_w_gate'][li], w_up=I['moe_w_up'][li],
                             w_down=I['moe_w_down'][li], **bc))
        nm = 'Be' if even else 'Bo'
        r = _run(_prog(nm, lambda: build_B(2176, chunksB, groupsB, even)), maps)
        for c in range(8):
            b, th = c // 2, c % 2
            hT[b][:, idxB[th]] = r[c]['outT']
    return np.ascontiguousarray(np.stack([hT[b][:, TC_FULL:].T for b in range(NB)], 0)).astype(np.float32)
def emit_M(C, vecsA_s, vecsB_s):
    S = C.S
    cT_d = C.din("cT", [128, KD * 2])
    w_d = C.din("mod_w", [DEPTH, D, 6 * D])
    bT_d = C.din("mod_bT", [DEPTH, 128, 48])
    lnv_d = C.din("lnv", [DEPTH, 128, 48])
    cT = C.sb([128, KD, 2])
    sg = C.sb([128, KD, 2])
    wt = [C.sb([128, KD, 512]) for _ in range(2)]
    bT = C.sb([128, DEPTH, 48])
    mods = C.sb([128, 48, 2])
    va = C.sb([128, 4, KD])
    vb = C.sb([128, NVB, KD])
    P = [C.ps([128, 512]) for _ in range(2)]
    S.dma('sp', cT[:, :, :], cT_d.rearrange("p (k c) -> p k c", k=KD), w=['cT'])
    S.dma('sp', bT[:, :, :], bT_d.rearrange("l p c -> p l c"), w=['bT'])
    act(S, sg[:, :, :], cT[:, :, :], AF.Sigmoid, r=['cT'], w=['sg'])
    tt(S, 'dve', cT[:, :, :], cT[:, :, :], sg[:, :, :], ALU.mult, r=['cT', 'sg'], w=['cT'])
    wi = 0
    for li in range(DEPTH):
        pm = P[li % 2]
        pm3 = pm[:, 0:96].rearrange("p (c r) -> p c r", r=2)
        for blk in range(12):
            w_ = wt[wi % 2]
            S.dma('sp' if wi % 2 == 0 else 'act', w_[:, :, :], w_d[li, :, blk * 512:(blk + 1) * 512].rearrange("(k p) n -> p k n", p=128), w=[('w', wi % 2)])
            for c4 in range(4):
                ch = blk * 4 + c4
                for k in range(KD):
                    mm(S, pm3[:, ch, :], w_[:, k, c4 * 128:(c4 + 1) * 128], cT[:, k, :], k == 0, k == KD - 1, r=['cT', ('w', wi % 2)], w=[('pm', li % 2)])
            wi += 1
        bsrc = bT[:, li, :]
        bb = AP(bsrc.tensor, bsrc.offset, [list(bsrc.ap[0]), [1, 48], [0, 2]])
        tt(S, 'dve', mods[:, :, :], pm3, bb, ALU.add, r=[('pm', li % 2), 'bT'], w=['mods'])
        cp = lambda dst, c0, row: S.op('dve', lambda e: e.tensor_copy(out=dst, in_=mods[:, c0:c0 + 8, row]), r=['mods'], w=['vv'])
        cp(va[:, 0, :], 0, 0)
        cp(va[:, 1, :], 8, 0)
        cp(va[:, 2, :], 0, 1)
        cp(va[:, 3, :], 8, 1)
        for v, (c0, row) in enumerate(((16, 0), (16, 1), (32, 0), (32, 1), (24, 0), (24, 1), (40, 0), (40, 1))):
            cp(vb[:, v, :], c0, row)
        S.dma('sp', vb[:, 8:14, :], lnv_d[li].rearrange("p (v k) -> p v k", k=KD), r=['vv'], w=['vv'])
        S.dma('sp', vecsA_s[li], va[:, :, :].rearrange("p v k -> p (v k)"), r=['vv', 'mods'], w=[('vAs', li)])
        S.dma('sp', vecsB_s[li], vb[:, :, :].rearrange("p v k -> p (v k)"), r=['vv', 'mods'], w=[('vBs', li)])


def build_fused(TC=256, TL=4096):
    T = TC + TL
    C = Ctx()
    C.fused = True
    nc = C.nc
    h0 = nc.dram_tensor("hT0", [D, T], F32, kind="ExternalInput").ap()
    hS = C.scratch("hS", [D, T])
    mixS = C.scratch("mixS", [D, T])
    hOut = nc.dram_tensor("hOut", [D, T], F32, kind="ExternalOutput").ap()
    vecsA_s = [C.scratch("vecsA_s%d" % li, [128, 4 * KD]) for li in range(DEPTH)]
    vecsB_s = [C.scratch("vecsB_s%d" % li, [128, NVB * KD]) for li in range(DEPTH)]
    C.begin("M_", {})
    emit_M(C, vecsA_s, vecsB_s)
    C.end()
    nB = (T // 2)
    chunksB = [(0, TC // 2, True)] + [(TC // 2 + 512 * i, 512, False) for i in range(TL // 2 // 512)]
    groupsB = [[0, 1, 2], [3, 4]] if len(chunksB) == 5 else [list(range(len(chunksB)))]
    for li in range(DEPTH):
        even = li % 2 == 0
        hin = h0 if li == 0 else hS
        hout = hOut if li == DEPTH - 1 else hS
        for half in range(2):
            C.begin("L%dA%d_" % (li, half), dict(hT=hin, vecsA=vecsA_s[li], out=mixS))
            rowmap = (lambda r0, half=half: 256 * half + r0 if r0 < 256 else 512 + 256 * half + (r0 - 256))
            if even:
                build_A_even(TC, TL, C=C, rowmap=rowmap)
            else:
                build_A_odd(TC, TL, C=C, rowmap=rowmap)
            C.end()
        shared = {}
        for nm, shp in (("w_out", [D, D]), ("glu_w", [512, D]), ("w_r", [D, 36]), ("b_r_bc", [128, 36]), ("w_gate", [NEXP, D, FE]),
                        ("w_up", [NEXP, D, FE]), ("w_down", [NEXP, FE, D])):
            shared[nm] = nc.dram_tensor("L%dB_%s" % (li, nm), shp, F32, kind="ExternalInput").ap()
        if li == 0:
            cshared = {"ident": nc.dram_tensor("B_ident", [128, 128], F32, kind="ExternalInput").ap(),
                       "sel": nc.dram_tensor("B_sel", [32, NEXP * 128], F32, kind="ExternalInput").ap()}
        for th in range(2):
            al = dict(hT=hin, mixT=mixS, outT=hout, vecs=vecsB_s[li])
            al.update(shared)
            al.update(cshared)
            colmap = (lambda c0, th=th: (TC // 2) * th + c0 if c0 < TC // 2 else TC + (TL // 2) * th + (c0 - TC // 2))
            C.begin("L%dB%d_" % (li, th), al)
            build_B(nB, chunksB, groupsB, even, C=C, colmap=colmap)
            C.end()
    return C.close_all()
_FUSED = {}


def kernel(**I):
    return _kernel_fused(I, TC_FULL, TL_FULL, NB)


def _kernel_fused(I, TC, TL, nb):
    I = {k: np.asarray(v) for k, v in I.items()}
    if (TC, TL) not in _FUSED:
        _FUSED[(TC, TL)] = build_fused(TC, TL)
    nc = _FUSED[(TC, TL)]
    common = {}
    common["M_mod_w"] = I['mod_w']
    common["M_mod_bT"] = np.stack([I['mod_b'][li].reshape(48, 128).T for li in range(DEPTH)], 0)
    lnv = []
    for li in range(DEPTH):
        j = li // 2
        z = np.zeros(D, np.float32)
        bv, bg = z.copy(), z.copy()
        if li % 2 == 0:
            bv[:512] = I['s5_glu_b'][j][:512]
            bg[:512] = I['s5_glu_b'][j][512:]
        lnv.append(np.concatenate([fm(v) for v in (I['ln_g'][li, 0], I['ln_b'][li, 0], I['ln_g'][li, 1], I['ln_b'][li, 1], bv, bg)], 1))
    common["M_lnv"] = np.stack(lnv, 0)
    bc = b_consts()
    common["B_ident"] = bc['ident']
    common["B_sel"] = bc['sel']
    for li in range(DEPTH):
        j = li // 2
        even = li % 2 == 0
        for hf in range(2):
            pre = "L%dA%d_" % (li, hf)
            if even:
                m = dict(a_even_consts())
                m.update(a_even_params(hf, I['even_w_in'][j], I['s5_lam_re'][j], I['s5_lam_im'][j], I['s5_log_dt'][j], I['s5_b_re'][j],
                                       I['s5_b_im'][j], I['s5_c_re'][j], I['s5_c_im'][j], I['s5_d'][j], I['na_rpb'][j]))
            else:
                lam0 = 0.8 - 0.6 * math.exp(-0.3 * li)
                m = dict(a_odd_consts())
                m.update(a_odd_params(hf, I['odd_w_in'][j], I['gla_gate_w'][j], I['gla_gate_b'][j], I['gla_norm_g'][j], I['diff_lq1'][j],
                                      I['diff_lk1'][j], I['diff_lq2'][j], I['diff_lk2'][j], I['diff_norm_g'][j]))
                m['lam_init'] = np.tile(np.array([[lam0, 1.0 - lam0]], np.float32), (128, 1))
            for k, v in m.items():
                common[pre + k] = v
        pre = "L%dB_" % li
        common[pre + "w_out"] = I['even_w_out'][j] if even else I['odd_w_out'][j]
        common[pre + "glu_w"] = I['s5_glu_w'][j] if even else np.zeros((512, D), np.float32)
        common[pre + "w_r"] = np.concatenate([I['moe_w_grp'][li], I['moe_w_exp'][li]], 1)
        common[pre + "b_r_bc"] = np.tile(np.concatenate([I['moe_b_grp'][li], I['moe_b_exp'][li]])[None], (128, 1))
        common[pre + "w_gate"] = I['moe_w_gate'][li]
        common[pre + "w_up"] = I['moe_w_up'][li]
        common[pre + "w_down"] = I['moe_w_down'][li]
    common = {k: np.ascontiguousarray(v, dtype=np.float32) for k, v in common.items()}
    maps = []
    for c in range(8):
        b = c % nb
        m = dict(common)
        m["hT0"] = np.ascontiguousarray(np.concatenate([I['ctx'][b], I['x'][b]], 0).T.astype(np.float32))
        cc = np.stack([I['c'][b], I['c_ctx']], 0).astype(np.float32)
        m["M_cT"] = np.ascontiguousarray(cc.reshape(2, KD, 128).transpose(2, 1, 0).reshape(128, KD * 2))
        maps.append(m)
    r = run_bass_kernel_spmd(nc, maps, core_ids=list(range(8))).results
    return np.ascontiguousarray(np.stack([r[b]["hOut"][:, TC:].T for b in range(nb)], 0)).astype(np.float32)
```

```python
import contextlib
import math
import numpy as np
import concourse.bass as bass
import concourse.mybir as mybir
from concourse.bass_utils import run_bass_kernel_spmd
from concourse.ap import AP

F32 = mybir.dt.float32
BF16 = mybir.dt.bfloat16
I32 = mybir.dt.int32
ALU = mybir.AluOpType
AF = mybir.ActivationFunctionType
AX = mybir.AxisListType

D = 1024
KD = 8
NEXP = 32
FE = 256
EPS = 1e-5
DEPTH = 4
ALPHA = (2.0 * DEPTH) ** 0.25
EPS_LN = EPS / (ALPHA * ALPHA)


class Sched:
    def __init__(self, nc, es, nlanes=4):
        self.nc = nc
        self.eng = {'pe': nc.tensor, 'act': nc.scalar, 'dve': nc.vector, 'pool': nc.gpsimd, 'sp': nc.sync}
        self.semobj = {}
        self.cnt = {}
        for e in self.eng:
            self.semobj[e] = es.enter_context(nc.semaphore("s_" + e))
            self.cnt[e] = 0
        self.seen = {e: {} for e in self.eng}
        self.lanes = {}
        self.lane_rr = {}
        for q in ('sp', 'pool', 'act'):
            self.lanes[q] = []
            self.lane_rr[q] = 0
            for i in range(nlanes):
                key = ('lane', q, i)
                self.semobj[key] = es.enter_context(nc.semaphore("l_%s%d" % (q, i)))
                self.lanes[q].append([key, 0])
        self.lastw = {}
        self.readers = {}
        self.nops = 0

    def _wait(self, e, tok):
        key, val = tok
        if self.seen[e].get(key, 0) >= val:
            return
        self.seen[e][key] = val
        self.eng[e].wait_ge(self.semobj[key], val)

    def _deps(self, r, w):
        d = set()
        for k in list(r) + list(w):
            t = self.lastw.get(k)
            if t is not None:
                d.add(t)
        for k in w:
            for t in self.readers.get(k, ()):
                d.add(t)
        return d

    def _commit(self, r, w, tok):
        for k in w:
            self.lastw[k] = tok
            self.readers[k] = []
        for k in r:
            self.readers.setdefault(k, []).append(tok)

    def op(self, e, fn, r=(), w=()):
        for t in self._deps(r, w):
            if e == 'pe' and t[0] == 'pe':
                continue
            self._wait(e, t)
        ins = fn(self.eng[e])
        self.cnt[e] += 1
        ins.then_inc(self.semobj[e], 1)
        self._commit(r, w, (e, self.cnt[e]))
        self.nops += 1

    def dma(self, q, out, in_, r=(), w=()):
        lanes = self.lanes[q]
        i = self.lane_rr[q]
        self.lane_rr[q] = (i + 1) % len(lanes)
        lane = lanes[i]
        if lane[1] > 0:
            self._wait(q, (lane[0], 16 * lane[1]))
        for t in self._deps(r, w):
            self._wait(q, t)
        self.eng[q].dma_start(out=out, in_=in_).then_inc(self.semobj[lane[0]], 16)
        lane[1] += 1
        self._commit(r, w, (lane[0], 16 * lane[1]))
        self.nops += 1

    def finish(self):
        for q in self.lanes:
            for lane in self.lanes[q]:
                if lane[1] > 0:
                    self.nc.sync.wait_ge(self.semobj[lane[0]], 16 * lane[1])


class Ctx:
    def __init__(self, name="k"):
        self.nc = bass.Bass("TRN2", target_bir_lowering=False)
        self.es0 = contextlib.ExitStack()
        self.es = self.es0
        self.S = Sched(self.nc, self.es0)
        self.n = 0
        self.prefix = ""
        self.alias = {}
        self.fused = False

    def begin(self, prefix, alias):
        self.prefix = prefix
        self.alias = dict(alias)
        self.es = contextlib.ExitStack()

    def end(self):
        phase_barrier(self.S)
        self.es.close()
        self.es = self.es0
        self.prefix = ""
        self.alias = {}

    def din(self, name, shape, dt=F32):
        if name in self.alias:
            return self.alias[name]
        return self.nc.dram_tensor(self.prefix + name, list(shape), dt, kind="ExternalInput").ap()

    def dout(self, name, shape, dt=F32):
        if name in self.alias:
            return self.alias[name]
        return self.nc.dram_tensor(self.prefix + name, list(shape), dt, kind="ExternalOutput").ap()

    def scratch(self, name, shape, dt=F32):
        return self.nc.dram_tensor(name, list(shape), dt, kind="Internal").ap()

    def sb(self, shape, dt=F32, name=None):
        self.n += 1
        return self.es.enter_context(self.nc.sbuf_tensor(name or ("t%d" % self.n), list(shape), dt))

    def ps(self, shape, dt=F32, name=None):
        self.n += 1
        return self.es.enter_context(self.nc.psum_tensor(name or ("p%d" % self.n), list(shape), dt))

    def close(self):
        if self.fused:
            return None
        self.S.finish()
        self.es0.close()
        return self.nc

    def close_all(self):
        self.S.finish()
        self.es0.close()
        return self.nc


def phase_barrier(S):
    for e in S.eng:
        for e2 in S.eng:
            if e2 != e and S.cnt[e2] > 0:
                S._wait(e, (e2, S.cnt[e2]))
        for q in S.lanes:
            for lane in S.lanes[q]:
                if lane[1] > 0:
                    S._wait(e, (lane[0], 16 * lane[1]))
    S.lastw = {}
    S.readers = {}


def mm(S, out, lhsT, rhs, start, stop, r, w):
    S.op('pe', lambda e: e.matmul(out, lhsT, rhs, start=start, stop=stop), r=r, w=w)


def act(S, out, in_, func, r, w, bias=None, scale=None, accum_out=None):
    kw = {}
    if bias is not None:
        kw['bias'] = bias
    if scale is not None:
        kw['scale'] = scale
    if accum_out is not None:
        kw['accum_out'] = accum_out
    S.op('act', lambda e: e.activation(out=out, in_=in_, func=func, **kw), r=r, w=w)


def tt(S, eng, out, in0, in1, op, r, w):
    S.op(eng, lambda e: e.tensor_tensor(out=out, in0=in0, in1=in1, op=op), r=r, w=w)


def ts(S, eng, out, in0, s1, s2, op0, op1, r, w):
    if s2 is None:
        S.op(eng, lambda e: e.tensor_scalar(out=out, in0=in0, scalar1=s1, scalar2=None, op0=op0), r=r, w=w)
    else:
        S.op(eng, lambda e: e.tensor_scalar(out=out, in0=in0, scalar1=s1, scalar2=s2, op0=op0, op1=op1), r=r, w=w)


def stt(S, out, in0, scalar, in1, op0, op1, r, w):
    S.op('dve', lambda e: e.scalar_tensor_tensor(out=out, in0=in0, scalar=scalar, in1=in1, op0=op0, op1=op1), r=r, w=w)


def ln_feature_major(C, x, xk, w, ps1, ps2, ones, tmp_sq, sqk, st, stk, cst):
    S = C.S
    for j in range(KD):
        act(S, tmp_sq[:, j, :w], x[:, j, :w], AF.Square, r=[xk + (j,)], w=[sqk + (j,)])
        mm(S, ps1[:, :w], ones[:, :], x[:, j, :w], j == 0, j == KD - 1, r=[xk + (j,), 'ones'], w=['ps4'])
        mm(S, ps2[:, :w], ones[:, :], tmp_sq[:, j, :w], j == 0, j == KD - 1, r=[sqk + (j,), 'ones'], w=['ps5'])
    mean, m2, var, rstd = st[:, 0, :w], st[:, 1, :w], st[:, 2, :w], st[:, 3, :w]
    act(S, mean, ps1[:, :w], AF.Copy, r=['ps4'], w=[stk + (0,)], scale=1.0 / D)
    act(S, m2, mean, AF.Square, r=[stk + (0,)], w=[stk + (1,)])
    stt(S, var, ps2[:, :w], 1.0 / D, m2, ALU.mult, ALU.subtract, r=['ps5', stk + (1,)], w=[stk + (2,)])
    act(S, var, var, AF.Sqrt, r=[stk + (2,), 'cst'], w=[stk + (2,)], bias=cst[:, 0:1])
    S.op('dve', lambda e: e.reciprocal(out=rstd, in_=var), r=[stk + (2,)], w=[stk + (3,)])
    for j in range(KD):
        tt(S, 'pool', x[:, j, :w], x[:, j, :w], mean, ALU.subtract, r=[xk + (j,), stk + (0,)], w=[xk + (j,)])
        tt(S, 'dve', x[:, j, :w], x[:, j, :w], rstd, ALU.mult, r=[xk + (j,), stk + (3,)], w=[xk + (j,)])
(V_G1L, V_G1C, V_SC2L, V_SC2C, V_SH2L, V_SH2C, V_G2L, V_G2C, V_LNG1, V_LNB1, V_LNG2, V_LNB2,
 V_GLUBV, V_GLUBG) = range(14)
NVB = 14


def build_B(ntok, chunks, groups, even, C=None, colmap=None):
    C = C or Ctx()
    gc = colmap or (lambda c0: c0)
    S = C.S
    nc = C.nc
    hT = C.din("hT", [D, ntok])
    mixT = C.din("mixT", [D, ntok])
    vecs_d = C.din("vecs", [128, NVB * KD])
    wout_d = C.din("w_out", [D, D])
    glw_d = C.din("glu_w", [512, D])
    wr_d = C.din("w_r", [D, 36])
    brbc_d = C.din("b_r_bc", [128, 36])
    wg_d = C.din("w_gate", [NEXP, D, FE])
    wu_d = C.din("w_up", [NEXP, D, FE])
    wd_d = C.din("w_down", [NEXP, FE, D])
    ident_d = C.din("ident", [128, 128])
    sel_d = C.din("sel", [32, NEXP * 128])
    outT = C.dout("outT", [D, ntok])

    gmax = max(sum(chunks[c][1] for c in g) for g in groups)
    WM = max(c[1] for c in chunks)

    vecs = C.sb([128, NVB + 3, KD])
    V_G1A, V_A2, V_G2A = NVB, NVB + 1, NVB + 2
    aux = C.sb([128, 3, KD])
    cst = C.sb([128, 4])
    ones = C.sb([128, 128])
    ident = C.sb([128, 128])
    sel = C.sb([32, NEXP * 128], BF16)
    wr = C.sb([128, KD, 36])
    brbc = C.sb([128, 36])
    NB = 3
    R = C.sb([128, max(KD * D + 4 * D, NB * (2 * KD * FE + 2 * D))], BF16)
    wout = R[:, 0:KD * D].rearrange("p (k n) -> p k n", k=KD)
    gluw = R[:, KD * D:KD * D + 4 * D].rearrange("p (k n) -> p k n", k=4)
    ESZ = 2 * KD * FE + 2 * D

    def wg_v(b):
        return R[:, b * ESZ:b * ESZ + KD * FE].rearrange("p (k f) -> p k f", k=KD)

    def wu_v(b):
        return R[:, b * ESZ + KD * FE:b * ESZ + 2 * KD * FE].rearrange("p (k f) -> p k f", k=KD)

    def wd_v(b):
        return R[:, b * ESZ + 2 * KD * FE:(b + 1) * ESZ].rearrange("p (c d) -> p c d", c=2)

    x2b = C.sb([128, KD, gmax], BF16)
    acc = C.sb([128, KD, gmax])
    combT = C.sb([32, gmax], BF16)
    hch = C.sb([128, KD, WM])
    tq = C.sb([128, KD, WM])
    sqt = C.sb([128, KD, WM])
    mT = C.sb([128, KD, WM], BF16)
    ys = C.sb([128, 4, WM])
    zT = C.sb([128, 4, WM], BF16)
    gtmp = C.sb([128, 2, WM])
    st = C.sb([128, 4, WM])
    sg = [C.sb([128, 2, WM]) for _ in range(2)]
    tmu = [C.sb([128, 2, WM]) for _ in range(2)]
    actb = [C.sb([128, 2, WM], BF16) for _ in range(2)]
    rt = C.sb([128, 160])
    P = [C.ps([128, 512]) for _ in range(8)]
    pk = ['ps%d' % i for i in range(8)]

    S.dma('sp', vecs[:, 0:NVB, :], vecs_d.rearrange("p (v k) -> p v k", v=NVB), w=['vecs'])
    S.dma('sp', ident[:, :], ident_d, w=['ident'])
    S.dma('sp', wr[:, :, :], wr_d.rearrange("(k p) n -> p k n", p=128), w=['wr'])
    S.dma('sp', brbc[:, :], brbc_d, w=['brbc'])
    S.dma('pool', sel[:, :], sel_d, w=['sel'])
    S.op('dve', lambda e: e.memset(ones[:, :], 1.0), w=['ones'])
    S.op('dve', lambda e: e.memset(cst[:, 0:1], EPS_LN), w=['cst'])
    ts(S, 'dve', vecs[:, V_G1A, :], vecs[:, V_G1L, :], 1.0 / ALPHA, None, ALU.mult, None, r=['vecs'], w=['vecs'])
    ts(S, 'dve', vecs[:, V_A2, :], vecs[:, V_SC2L, :], 1.0, None, ALU.add, None, r=['vecs'], w=['vecs'])
    ts(S, 'dve', vecs[:, V_G2A, :], vecs[:, V_G2L, :], 1.0 / ALPHA, None, ALU.mult, None, r=['vecs'], w=['vecs'])
    ts(S, 'dve', aux[:, 0, :], vecs[:, V_G1C, :], 1.0 / ALPHA, None, ALU.mult, None, r=['vecs'], w=['vecs'])
    ts(S, 'dve', aux[:, 1, :], vecs[:, V_SC2C, :], 1.0, None, ALU.add, None, r=['vecs'], w=['vecs'])
    ts(S, 'dve', aux[:, 2, :], vecs[:, V_G2C, :], 1.0 / ALPHA, None, ALU.mult, None, r=['vecs'], w=['vecs'])

    def col(v, j):
        return vecs[:, v, j:j + 1]

    for gi, g in enumerate(groups):
        g0 = chunks[g[0]][0]
        allw = [(nm, b_) for nm in ('wg', 'wu', 'wd') for b_ in range(NB)]
        S.dma('pool', wout, wout_d.rearrange("(k p) n -> p k n", p=128), w=['R'] + allw)
        if even:
            S.dma('pool', gluw, glw_d.rearrange("(k p) n -> p k n", p=128), w=['R2'] + allw)
        rk = ['R', 'R2']
        for ci in g:
            c0, w, isctx = chunks[ci]
            l0 = c0 - g0
            g1a = (lambda j: aux[:, 0, j:j + 1]) if isctx else (lambda j: col(V_G1A, j))
            a2 = (lambda j: aux[:, 1, j:j + 1]) if isctx else (lambda j: col(V_A2, j))
            sh2 = (lambda j: col(V_SH2C, j)) if isctx else (lambda j: col(V_SH2L, j))
            S.dma('sp', hch[:, :, :w], hT[:, gc(c0):gc(c0) + w].rearrange("(k p) t -> p k t", p=128), w=[('hch', j) for j in range(KD)])
            if even:
                S.dma('sp', ys[:, :, :w], mixT[0:512, gc(c0):gc(c0) + w].rearrange("(k p) t -> p k t", p=128), w=[('ys', k) for k in range(4)])
                S.dma('pool', mT[:, 4:8, :w], mixT[512:1024, gc(c0):gc(c0) + w].rearrange("(k p) t -> p k t", p=128), w=[('mT', k) for k in range(4, 8)])
                for k in range(4):
                    x = ys[:, k, :w]
                    t1 = gtmp[:, 0, :w]
                    act(S, t1, x, AF.Square, r=[('ys', k)], w=['gt0'])
                    ts(S, 'dve', t1, t1, 0.044715, 1.0, ALU.mult, ALU.add, r=['gt0'], w=['gt0'])
                    tt(S, 'dve', t1, t1, x, ALU.mult, r=['gt0', ('ys', k)], w=['gt0'])
                    act(S, t1, t1, AF.Sigmoid, r=['gt0'], w=['gt0'], scale=2.0 * math.sqrt(2.0 / math.pi))
                    tt(S, 'pool', zT[:, k, :w], x, t1, ALU.mult, r=['gt0', ('ys', k)], w=[('zT', k)])
                for j in range(4):
                    pv, pg = P[2 * (j % 2)], P[2 * (j % 2) + 1]
                    kv, kg = pk[2 * (j % 2)], pk[2 * (j % 2) + 1]
                    for k in range(4):
                        mm(S, pv[:, :w], gluw[:, k, j * 128:(j + 1) * 128], zT[:, k, :w], k == 0, k == 3, r=[('zT', k)] + rk, w=[kv])
                    for k in range(4):
                        mm(S, pg[:, :w], gluw[:, k, 512 + j * 128:512 + (j + 1) * 128], zT[:, k, :w], k == 0, k == 3, r=[('zT', k)] + rk, w=[kg])
                    t2 = gtmp[:, 1, :w]
                    act(S, t2, pg[:, :w], AF.Sigmoid, r=[kg, 'vecs'], w=['gt1'], bias=col(V_GLUBG, j))
                    stt(S, mT[:, j, :w], pv[:, :w], col(V_GLUBV, j), t2, ALU.add, ALU.mult, r=[kv, 'gt1', 'vecs'], w=[('mT', j)])
            else:
                S.dma('pool', mT[:, :, :w], mixT[:, gc(c0):gc(c0) + w].rearrange("(k p) t -> p k t", p=128), w=[('mT', k) for k in range(KD)])
            for j in range(KD):
                py, ky = P[j % 4], pk[j % 4]
                for k in range(KD):
                    mm(S, py[:, :w], wout[:, k, j * 128:(j + 1) * 128], mT[:, k, :w], k == 0, k == KD - 1, r=[('mT', k)] + rk, w=[ky])
                stt(S, tq[:, j, :w], py[:, :w], g1a(j), hch[:, j, :w], ALU.mult, ALU.add, r=[ky, ('hch', j), 'vecs'], w=[('tq', j)])
            ln_feature_major(C, tq, ('tq',), w, P[4], P[5], ones, sqt, ('sqt',), st, ('st',), cst)
            for j in range(KD):
                a_j = acc[:, j, l0:l0 + w]
                act(S, a_j, tq[:, j, :w], AF.Identity, r=[('tq', j), 'vecs'], w=[('acc', ci, j)], scale=col(V_LNG1, j), bias=col(V_LNB1, j))
                ts(S, 'dve', sqt[:, j, :w], a_j, a2(j), sh2(j), ALU.mult, ALU.add, r=[('acc', ci, j), 'vecs'], w=[('sqt', j)])
                S.op('pool', lambda e, j=j: e.tensor_copy(out=x2b[:, j, l0:l0 + w], in_=sqt[:, j, :w]), r=[('sqt', j)], w=[('x2b', ci, j)])
            for t0 in range(0, w, 128):
                pr, kr = P[6], pk[6]
                for k in range(KD):
                    mm(S, pr[:, 0:36], sqt[:, k, t0:t0 + 128], wr[:, k, :], k == 0, k == KD - 1, r=[('sqt', k), 'wr'], w=[kr])
                lg = rt[:, 0:36]
                tt(S, 'dve', lg, pr[:, 0:36], brbc[:, :], ALU.add, r=[kr, 'brbc'], w=['rt'])
                RK = dict(r=['rt'], w=['rt'])
                gm, ngm, oh, eg, gs, pgp = rt[:, 36:37], rt[:, 37:38], rt[:, 40:44], rt[:, 44:48], rt[:, 38:39], rt[:, 39:40]
                S.op('dve', lambda e: e.tensor_reduce(out=gm, in_=rt[:, 0:4], axis=AX.X, op=ALU.max), **RK)
                ts(S, 'dve', oh, rt[:, 0:4], gm, None, ALU.is_equal, None, **RK)
                ts(S, 'dve', ngm, gm, -1.0, None, ALU.mult, None, **RK)
                act(S, eg, rt[:, 0:4], AF.Exp, bias=ngm, accum_out=gs, **RK)
                S.op('dve', lambda e: e.reciprocal(out=pgp, in_=gs), **RK)
                lein, mk1, le2, mk2, c8 = rt[:, 48:56], rt[:, 56:64], rt[:, 64:72], rt[:, 72:80], rt[:, 80:88]
                ts(S, 'dve', lein, rt[:, 4:12], oh[:, 0:1], None, ALU.mult, None, **RK)
                for gq in range(1, 4):
                    stt(S, lein, rt[:, 4 + 8 * gq:12 + 8 * gq], oh[:, gq:gq + 1], lein, ALU.mult, ALU.add, **RK)
                m1, m2, dd, ed, w1, w2 = (rt[:, 88 + i:89 + i] for i in range(6))
                S.op('dve', lambda e: e.tensor_reduce(out=m1, in_=lein, axis=AX.X, op=ALU.max), **RK)
                ts(S, 'dve', mk1, lein, m1, None, ALU.is_equal, None, **RK)
                stt(S, le2, mk1, -1e30, lein, ALU.mult, ALU.add, **RK)
                S.op('dve', lambda e: e.tensor_reduce(out=m2, in_=le2, axis=AX.X, op=ALU.max), **RK)
                ts(S, 'dve', mk2, le2, m2, None, ALU.is_equal, None, **RK)
                tt(S, 'dve', dd, m2, m1, ALU.subtract, **RK)
                act(S, ed, dd, AF.Exp, **RK)
                ts(S, 'dve', w1, ed, 1.0, None, ALU.add, None, **RK)
                S.op('dve', lambda e: e.reciprocal(out=w1, in_=w1), **RK)
                tt(S, 'dve', w1, w1, pgp, ALU.mult, **RK)
                tt(S, 'dve', w2, ed, w1, ALU.mult, **RK)
                ts(S, 'dve', c8, mk1, w1, None, ALU.mult, None, **RK)
                stt(S, c8, mk2, w2, c8, ALU.mult, ALU.add, **RK)
                comb = rt[:, 96:128]
                for gq in range(4):
                    ts(S, 'dve', comb[:, 8 * gq:8 * gq + 8], c8, oh[:, gq:gq + 1], None, ALU.mult, None, **RK)
                pT, kT = P[7], pk[7]
                S.op('pe', lambda e: e.transpose(out=pT[0:32, 0:128], in_=comb, identity=ident[:, :]), r=['rt', 'ident'], w=[kT])
                act(S, combT[:, l0 + t0:l0 + t0 + 128], pT[0:32, 0:128], AF.Copy, r=[kT], w=[('combT', ci)])
        def load_exp(e):
            b = e % NB
            S.dma('pool', wg_v(b), wg_d[e].rearrange("(k p) f -> p k f", p=128), w=[('wg', b)] + rk)
            S.dma('pool', wu_v(b), wu_d[e].rearrange("(k p) f -> p k f", p=128), w=[('wu', b)] + rk)
            S.dma('pool', wd_v(b), wd_d[e].rearrange("(c p) d -> p c d", p=128), w=[('wd', b)] + rk)
        load_exp(0)
        load_exp(1)
        yrot = [0]
        items = [(e, ci) for e in range(NEXP) for ci in g]

        def stage_a(n, e, ci):
            b = e % NB
            wg, wu = wg_v(b), wu_v(b)
            c0, w, isctx = chunks[ci]
            l0 = c0 - g0
            q = n % 2
            pb, kb = P[4], pk[4]
            mm(S, pb[:, :w], sel[:, e * 128:(e + 1) * 128], combT[:, l0:l0 + w], True, True, r=['sel', ('combT', ci)], w=[kb])
            for fc in range(2):
                phg, phu = P[2 * fc], P[2 * fc + 1]
                kg_, ku_ = pk[2 * fc], pk[2 * fc + 1]
                for k in range(KD):
                    mm(S, phg[:, :w], wg[:, k, fc * 128:(fc + 1) * 128], x2b[:, k, l0:l0 + w], k == 0, k == KD - 1, r=[('wg', b), ('x2b', ci, k)], w=[kg_])
                for k in range(KD):
                    mm(S, phu[:, :w], wu[:, k, fc * 128:(fc + 1) * 128], x2b[:, k, l0:l0 + w], k == 0, k == KD - 1, r=[('wu', b), ('x2b', ci, k)], w=[ku_])
                act(S, sg[q][:, fc, :w], phg[:, :w], AF.Silu, r=[kg_], w=[('sg', q, fc)])
                tt(S, 'dve', tmu[q][:, fc, :w], phu[:, :w], sg[q][:, fc, :w], ALU.mult, r=[ku_, ('sg', q, fc)], w=[('tmu', q, fc)])
                tt(S, 'dve', actb[q][:, fc, :w], tmu[q][:, fc, :w], pb[:, :w], ALU.mult, r=[kb, ('tmu', q, fc)], w=[('actb', q, fc)])

        def stage_b(n, e, ci):
            b = e % NB
            wd = wd_v(b)
            c0, w, isctx = chunks[ci]
            l0 = c0 - g0
            q = n % 2
            g2a = (lambda j: aux[:, 2, j:j + 1]) if isctx else (lambda j: col(V_G2A, j))
            for j in range(KD):
                yi = 5 + (yrot[0] % 3)
                yrot[0] += 1
                pyj, kyj = P[yi], pk[yi]
                for fc in range(2):
                    mm(S, pyj[:, :w], wd[:, fc, j * 128:(j + 1) * 128], actb[q][:, fc, :w], fc == 0, fc == 1, r=[('wd', b), ('actb', q, fc)], w=[kyj])
                a_j = acc[:, j, l0:l0 + w]
                stt(S, a_j, pyj[:, :w], g2a(j), a_j, ALU.mult, ALU.add, r=[kyj, ('acc', ci, j), 'vecs'], w=[('acc', ci, j)])

        for n in range(len(items) + 1):
            if n < len(items):
                stage_a(n, *items[n])
            if n >= 1:
                stage_b(n - 1, *items[n - 1])
            if n < len(items) and items[n][1] == g[0] and items[n][0] + 2 < NEXP:
                load_exp(items[n][0] + 2)
        for ci in g:
            c0, w, isctx = chunks[ci]
            l0 = c0 - g0
            av = acc[:, :, l0:l0 + w]
            ln_feature_major(C, av, ('acc', ci), w, P[4], P[5], ones, sqt, ('sqt',), st, ('st',), cst)
            for j in range(KD):
                act(S, hch[:, j, :w], av[:, j, :], AF.Identity, r=[('acc', ci, j), 'vecs'], w=[('hch', j)], scale=col(V_LNG2, j), bias=col(V_LNB2, j))
            S.dma('sp', outT[:, gc(c0):gc(c0) + w].rearrange("(k p) t -> p k t", p=128), hch[:, :, :w], r=[('hch', j) for j in range(KD)])
    return C.close()
def fm(v):
    return np.ascontiguousarray(np.asarray(v, np.float32).reshape(KD, 128).T)


def b_consts():
    sel = np.zeros((32, NEXP * 128), np.float32)
    for e in range(NEXP):
        sel[e, e * 128:(e + 1) * 128] = 1.0
    return dict(ident=np.eye(128, dtype=np.float32), sel=sel)


def b_vecs(mod_lat, mod_ctx, ln_g, ln_b, glu_b):
    z = np.zeros(1024, np.float32)
    gb = np.asarray(glu_b, np.float32) if glu_b is not None else np.zeros(1024, np.float32)
    bv = z.copy(); bv[:512] = gb[:512]
    bg = z.copy(); bg[:512] = gb[512:]
    lst = [mod_lat[2], mod_ctx[2], mod_lat[4], mod_ctx[4], mod_lat[3], mod_ctx[3], mod_lat[5], mod_ctx[5],
           ln_g[0], ln_b[0], ln_g[1], ln_b[1], bv, bg]
    return np.ascontiguousarray(np.concatenate([fm(v) for v in lst], axis=1))
GLA_SC = 0.125
NCOL_ODD = 2080
(C_GQ, C_GK, C_GV, C_GR, C_GZ, C_DQ, C_DQP, C_DK, C_DKP, C_DV) = (0, 128, 256, 512, 768, 800, 1056, 1312, 1568, 1824)


def barrier(S):
    return phase_barrier(S)


def _old_barrier(S):
    for e in S.eng:
        for e2 in S.eng:
            if e2 != e and S.cnt[e2] > 0:
                S._wait(e, (e2, S.cnt[e2]))
        for q in S.lanes:
            for lane in S.lanes[q]:
                if lane[1] > 0:
                    S._wait(e, (lane[0], 16 * lane[1]))
    S.lastw = {}
    S.readers = {}


def rev_ap(ap2d, n):
    a = ap2d
    return AP(a.tensor, a.offset + (n - 1), [list(a.ap[0]), [-1, n]])


def modulate_aT(C, hT, vecs, aT, blocks, hb, hbk):
    S = C.S
    for (b0, bw, isctx) in blocks:
        S.dma('sp', hb[:, :, :bw], hT[:, b0:b0 + bw].rearrange("(k p) t -> p k t", p=128), w=[(hbk, j) for j in range(KD)])
        vsh, vsc = (2, 3) if isctx else (0, 1)
        for j in range(KD):
            if j % 2 == 0:
                act(S, aT[:, j, b0:b0 + bw], hb[:, j, :bw], AF.Identity, r=[(hbk, j), 'vecs'], w=[('aT', j, b0)],
                    scale=vecs[:, vsc, j:j + 1], bias=vecs[:, vsh, j:j + 1])
            else:
                ts(S, 'dve', aT[:, j, b0:b0 + bw], hb[:, j, :bw], vecs[:, vsc, j:j + 1], vecs[:, vsh, j:j + 1], ALU.mult, ALU.add,
                   r=[(hbk, j), 'vecs'], w=[('aT', j, b0)])


def build_A_odd(TC, TL, C=None, rowmap=None):
    T = TC + TL
    C = C or Ctx()
    rm = rowmap or (lambda r0: r0)
    S = C.S
    hT = C.din("hT", [D, T])
    vecs_d = C.din("vecsA", [128, 4 * KD])
    W_d = C.din("W", [D, NCOL_ODD])
    gw_d = C.din("gate_w_aug", [17, 256])
    gn_d = C.din("gla_g", [128, 2])
    dn_d = C.din("diff_g", [128, 1])
    lam_d = C.din("lam_rows", [128, 256])
    rope_d = C.din("rope", [64, 128])
    maskf_d = C.din("maskf", [64, 64])
    maskb_d = C.din("maskb", [64, 64])
    scanm_d = C.din("scanmask", [64, 512])
    ident_d = C.din("ident", [128, 128])
    selq_d = C.din("selq", [64, 65])
    li_d = C.din("lam_init", [128, 2])
    out = C.dout("out", [512, T])

    blocks = [(0, TC, True)] + [(TC + 512 * i, 512, False) for i in range(TL // 512)]
    NCH = T // 64
    NKC = T // 128

    vecs = C.sb([128, 4, KD])
    aT = C.sb([128, KD, T], BF16)
    W = C.sb([128, KD, NCOL_ODD], BF16)
    gw = C.sb([17, 256], BF16)
    gn = C.sb([128, 2])
    dn = C.sb([128, 1])
    lamr = C.sb([128, 256])
    lamc = C.sb([128, 8])
    li = C.sb([128, 2])
    rope = C.sb([64, 128])
    maskf = C.sb([64, 64])
    maskb = C.sb([64, 64])
    scanm = C.sb([64, 512])
    identb = C.sb([128, 128], BF16)
    selq = C.sb([64, 65])
    ones = C.sb([128, 128])
    onesb = C.sb([128, 128], BF16)
    cst = C.sb([128, 4])
    USZ = int(3.5 * T + T // 64) + 5800
    U = C.sb([128, USZ])
    P = [C.ps([128, 512]) for _ in range(8)]
    pk = ['ps%d' % i for i in range(8)]

    S.dma('sp', vecs[:, :, :], vecs_d.rearrange("p (v k) -> p v k", v=4), w=['vecs'])
    S.dma('pool', W[:, :, :], W_d.rearrange("(k p) n -> p k n", p=128), w=['W'])
    S.dma('pool', gw[:, :], gw_d, w=['gw'])
    S.dma('sp', gn[:, :], gn_d, w=['gn'])
    S.dma('sp', dn[:, :], dn_d, w=['dn'])
    S.dma('sp', lamr[:, :], lam_d, w=['lamr'])
    S.dma('sp', li[:, :], li_d, w=['li'])
    S.dma('sp', rope[:, :], rope_d, w=['rope'])
    S.dma('sp', maskf[:, :], maskf_d, w=['maskf'])
    S.dma('sp', maskb[:, :], maskb_d, w=['maskb'])
    S.dma('sp', scanm[:, :], scanm_d, w=['scanm'])
    S.dma('pool', identb[:, :], ident_d, w=['identb'])
    S.dma('sp', selq[:, :], selq_d, w=['selq'])
    S.op('dve', lambda e: e.memset(ones[:, :], 1.0), w=['ones'])
    S.op('dve', lambda e: e.memset(onesb[:, :], 1.0), w=['onesb'])
    S.op('dve', lambda e: e.memset(cst[:, 0:1], EPS), w=['cst'])
    S.op('dve', lambda e: e.memset(cst[:, 1:2], 1.0), w=['cst'])
    ts(S, 'dve', vecs[:, 1, :], vecs[:, 1, :], 1.0, None, ALU.add, None, r=['vecs'], w=['vecs'])
    ts(S, 'dve', vecs[:, 3, :], vecs[:, 3, :], 1.0, None, ALU.add, None, r=['vecs'], w=['vecs'])
    tt(S, 'dve', lamr[:, 0:64], lamr[:, 0:64], lamr[:, 64:128], ALU.mult, r=['lamr'], w=['lamr'])
    tt(S, 'dve', lamr[:, 128:192], lamr[:, 128:192], lamr[:, 192:256], ALU.mult, r=['lamr'], w=['lamr'])
    S.op('dve', lambda e: e.tensor_reduce(out=lamc[:, 0:1], in_=lamr[:, 0:64], axis=AX.X, op=ALU.add), r=['lamr'], w=['lamc'])
    S.op('dve', lambda e: e.tensor_reduce(out=lamc[:, 1:2], in_=lamr[:, 128:192], axis=AX.X, op=ALU.add), r=['lamr'], w=['lamc'])
    act(S, lamc[:, 2:4], lamc[:, 0:2], AF.Exp, r=['lamc'], w=['lamc'])
    tt(S, 'dve', lamc[:, 4:5], lamc[:, 2:3], lamc[:, 3:4], ALU.subtract, r=['lamc'], w=['lamc'])
    ts(S, 'dve', lamc[:, 5:6], lamc[:, 4:5], li[:, 0:1], -1.0, ALU.add, ALU.mult, r=['lamc', 'li'], w=['lamc'])
    tt(S, 'dve', dn[:, :], dn[:, :], li[:, 1:2], ALU.mult, r=['dn', 'li'], w=['dn'])

    off = [0]

    def carve(shape, dt=F32):
        n = int(np.prod(shape[1:]))
        words = n if dt == F32 else (n + 1) // 2
        o = off[0]
        off[0] += words
        v = U[0:shape[0], o:o + words]
        if dt != F32:
            v = v.bitcast(dt)[:, 0:n]
        if len(shape) == 3:
            v = v.rearrange("p (a b) -> p a b", a=shape[1])
        return v

    hb = carve([128, KD, 512])
    modulate_aT(C, hT, vecs, aT, blocks, hb, 'hb')

    WK = ['W'] + [('aT', j, b[0]) for j in range(KD) for b in blocks]

    def inproj(ps_out, kps, col0, ncol, t0, tw):
        for k in range(KD):
            mm(S, ps_out, W[:, k, col0:col0 + ncol], aT[:, k, t0:t0 + tw], k == 0, k == KD - 1, r=WK, w=[kps])

    barrier(S)
    off[0] = 0
    vtok = carve([64, NCH, 128], BF16)
    osum = carve([128, T])
    qdec = carve([64, T], BF16)
    kinc = carve([64, T], BF16)
    kdtok = carve([64, NCH, 64], BF16)
    decay = carve([64, NCH])
    zaug = carve([17, 512], BF16)
    xs = carve([64, 512])
    cs = carve([64, 512])
    eg = carve([64, 512])
    egi = carve([64, 512])
    ekd = carve([64, 512])
    kdT = carve([64, 512], BF16)
    attm = carve([64, 8, 64], BF16)
    Sf = carve([64, 128])
    Sb = [carve([64, 128], BF16), carve([64, 128], BF16)]
    fin = carve([128, 4, 512])
    assert off[0] <= USZ, (off[0], USZ)
    PTb = P[6][0:64, 0:256].bitcast(BF16).rearrange("p (a b) -> p a b", b=64)

    S.op('dve', lambda e: e.memset(zaug[:, :], 1.0), w=['zaug'])
    for hg in range(2):
        for n0 in range(0, NCH, 4):
            nn = min(4, NCH - n0)
            pv, kv = P[(n0 // 4) % 2], pk[(n0 // 4) % 2]
            pvv = pv[0:64, :].rearrange("p (a b) -> p a b", a=4)
            for n in range(nn):
                for k in range(KD):
                    mm(S, pvv[:, n, :], aT[:, k, (n0 + n) * 64:(n0 + n + 1) * 64], W[:, k, C_GV + hg * 128:C_GV + (hg + 1) * 128],
                       k == 0, k == KD - 1, r=WK, w=[kv])
            act(S, vtok[:, n0:n0 + nn, :], pvv[:, 0:nn, :], AF.Copy, r=[kv], w=[('vtok', n0)])
        VK = [('vtok', n0) for n0 in range(0, NCH, 4)]
        for d in range(2):
            for (b0, bw, isctx) in blocks:
                nch = bw // 64
                c0 = b0 // 64
                pq, pkk, pz, pg = P[2], P[3], P[4], P[5]
                inproj(pq[0:64, :bw], pk[2], C_GQ + hg * 64, 64, b0, bw)
                inproj(pkk[0:64, :bw], pk[3], C_GK + hg * 64, 64, b0, bw)
                inproj(pz[0:16, :bw], pk[4], C_GZ + d * 16, 16, b0, bw)
                act(S, zaug[0:16, :bw], pz[0:16, :bw], AF.Copy, r=[pk[4]], w=['zaug'])
                mm(S, pg[0:64, :bw], gw[0:17, d * 128 + hg * 64:d * 128 + (hg + 1) * 64], zaug[0:17, :bw], True, True, r=['gw', 'zaug'], w=[pk[5]])
                ts(S, 'dve', xs[:, :bw], pg[0:64, :bw], -80.0, None, ALU.max, None, r=[pk[5]], w=['xs'])
                act(S, xs[:, :bw], xs[:, :bw], AF.Exp, r=['xs'], w=['xs'], scale=-1.0)
                act(S, xs[:, :bw], xs[:, :bw], AF.Ln, r=['xs', 'cst'], w=['xs'], bias=cst[0:64, 1:2])
                if d == 0:
                    S.op('dve', lambda e: e.tensor_tensor_scan(out=cs[:, :bw], data0=scanm[:, :bw], data1=xs[:, :bw], initial=0.0,
                                                               op0=ALU.mult, op1=ALU.add), r=['xs', 'scanm'], w=['cs'])
                else:
                    S.op('dve', lambda e: e.tensor_tensor_scan(out=rev_ap(cs[:, :bw], bw), data0=scanm[:, :bw], data1=rev_ap(xs[:, :bw], bw),
                                                               initial=0.0, op0=ALU.mult, op1=ALU.add), r=['xs', 'scanm'], w=['cs'])
                cs3 = cs[:, :bw].rearrange("p (a b) -> p a b", b=64)
                gi = 63 if d == 0 else 0
                act(S, eg[:, :bw], cs[:, :bw], AF.Exp, r=['cs'], w=['eg'], scale=-1.0 / 16)
                act(S, egi[:, :bw], cs[:, :bw], AF.Exp, r=['cs'], w=['egi'], scale=1.0 / 16)
                act(S, decay[:, c0:c0 + nch], cs3[:, :, gi], AF.Exp, r=['cs'], w=[('decay', c0)], scale=-1.0 / 16)
                csb = cs[:, :]
                Gb = AP(csb.tensor, csb.offset + gi, [list(csb.ap[0]), [64, nch], [0, 64]])
                tt(S, 'dve', ekd[:, :bw].rearrange("p (a b) -> p a b", b=64), cs3, Gb, ALU.subtract, r=['cs'], w=['ekd'])
                act(S, ekd[:, :bw], ekd[:, :bw], AF.Exp, r=['ekd'], w=['ekd'], scale=1.0 / 16)
                stt(S, qdec[:, b0:b0 + bw], pq[0:64, :bw], GLA_SC, eg[:, :bw], ALU.mult, ALU.mult, r=[pk[2], 'eg'], w=[('qdec', b0)])
                tt(S, 'dve', kinc[:, b0:b0 + bw], pkk[0:64, :bw], egi[:, :bw], ALU.mult, r=[pk[3], 'egi'], w=[('kinc', b0)])
                tt(S, 'dve', kdT[:, :bw], pkk[0:64, :bw], ekd[:, :bw], ALU.mult, r=[pk[3], 'ekd'], w=['kdT'])
                for n in range(nch):
                    S.op('pe', lambda e: e.transpose(out=PTb[:, n, :], in_=kdT[:, n * 64:(n + 1) * 64], identity=identb[0:64, 0:64]),
                         r=['kdT', 'identb'], w=[pk[6]])
                act(S, kdtok[:, c0:c0 + nch, :], PTb[:, 0:nch, :], AF.Copy, r=[pk[6]], w=[('kdtok', c0)])
            S.op('dve', lambda e: e.memset(Sf[:, :], 0.0), w=['Sf'])
            S.op('dve', lambda e: e.memset(Sb[0][:, :], 0.0), w=[('Sb', 0)])
            cur = 0
            blk_order = [blocks[0]] + (blocks[1:] if d == 0 else blocks[1:][::-1])
            mask = maskf if d == 0 else maskb
            for (b0, bw, isctx) in blk_order:
                nch = bw // 64
                c0 = b0 // 64
                pa, pu0, pu1, po = P[0], P[1], P[6], P[7]
                pa3 = pa[0:64, :].rearrange("p (a b) -> p a b", b=64)
                for n in range(nch):
                    t0 = b0 + n * 64
                    mm(S, pa3[:, n, :], kinc[:, t0:t0 + 64], qdec[:, t0:t0 + 64], True, True, r=[('kinc', b0), ('qdec', b0)], w=[pk[0]])
                tt(S, 'dve', attm[:, 0:nch, :], pa3[:, 0:nch, :], AP(mask[:, :].tensor, mask[:, :].offset, [list(mask[:, :].ap[0]), [0, nch], [1, 64]]), ALU.mult,
                   r=[pk[0], 'maskf', 'maskb'], w=['attm'])
                pus = [pu0[0:64, :].rearrange("p (a b) -> p a b", b=128), pu1[0:64, :].rearrange("p (a b) -> p a b", b=128)]
                pukey = [pk[1], pk[6]]
                for n in range(nch):
                    mm(S, pus[n // 4][:, n % 4, :], kdtok[:, c0 + n, :], vtok[:, c0 + n, :], True, True, r=[('kdtok', c0)] + VK, w=[pukey[n // 4]])
                order = list(range(nch)) if d == 0 else list(range(nch))[::-1]
                for n in order:
                    t0 = b0 + n * 64
                    mm(S, po[:, n * 64:(n + 1) * 64], vtok[:, c0 + n, :], attm[:, n, :], True, False, r=VK + ['attm'], w=[pk[7]])
                    mm(S, po[:, n * 64:(n + 1) * 64], Sb[cur][:, :], qdec[:, t0:t0 + 64], False, True, r=[('Sb', cur), ('qdec', b0)], w=[pk[7]])
                    stt(S, Sf[:, :], Sf[:, :], decay[:, c0 + n:c0 + n + 1], pus[n // 4][:, n % 4, :], ALU.mult, ALU.add,
                        r=['Sf', ('decay', c0), pukey[n // 4]], w=['Sf'])
                    act(S, Sb[1 - cur][:, :], Sf[:, :], AF.Copy, r=['Sf'], w=[('Sb', 1 - cur)])
                    cur = 1 - cur
                if d == 0:
                    act(S, osum[:, b0:b0 + bw], po[:, :bw], AF.Copy, r=[pk[7]], w=[('osum', b0)])
                else:
                    tt(S, 'dve', osum[:, b0:b0 + bw], osum[:, b0:b0 + bw], po[:, :bw], ALU.add, r=[pk[7], ('osum', b0)], w=[('osum', b0)])
        for (b0, bw, isctx) in blocks:
            sq, rs, y, sr = fin[:, 0, :bw], fin[:, 1, :bw], fin[:, 2, :bw], fin[:, 3, :bw]
            act(S, sq, osum[:, b0:b0 + bw], AF.Square, r=[('osum', b0)], w=['fin0'])
            mm(S, P[2][:, :bw], ones[:, :], sq, True, True, r=['fin0', 'ones'], w=[pk[2]])
            act(S, rs, P[2][:, :bw], AF.Sqrt, r=[pk[2], 'cst'], w=['fin1'], scale=1.0 / 128, bias=cst[:, 0:1])
            S.op('dve', lambda e: e.reciprocal(out=rs, in_=rs), r=['fin1'], w=['fin1'])
            tt(S, 'dve', y, osum[:, b0:b0 + bw], rs, ALU.mult, r=[('osum', b0), 'fin1'], w=['fin2'])
            inproj(P[3][:, :bw], pk[3], C_GR + hg * 128, 128, b0, bw)
            act(S, sr, P[3][:, :bw], AF.Silu, r=[pk[3]], w=['fin3'])
            stt(S, y, y, gn[:, hg:hg + 1], sr, ALU.mult, ALU.mult, r=['fin2', 'fin3', 'gn'], w=['fin2'])
            S.dma('sp', out[rm(hg * 128):rm(hg * 128) + 128, b0:b0 + bw], y, r=['fin2'])

    barrier(S)
    off[0] = 0
    Qa = [carve([65, T], BF16), carve([65, T], BF16)]
    Ka = [carve([65, T], BF16), carve([65, T], BF16)]
    Vt = carve([128, NKC, 128], BF16)
    nrm = carve([65, T])
    kmx = carve([65, 4])
    t1 = carve([64, 512])
    t2 = carve([64, 512])
    kr = carve([64, 512])
    sqn = carve([64, 512])
    E = [carve([128, 512], BF16) for _ in range(3)]
    o1 = carve([128, 512])
    o2 = carve([128, 512])
    rl = carve([128, 512])
    assert off[0] <= USZ, (off[0], USZ)

    def rope_tab(which, b0, bw, isctx):
        r0 = (b0 - TC) // 64
        nr = bw // 64
        base = rope[:, :]
        pitch = base.ap[0][0]
        lo = AP(base.tensor, base.offset + which * 64 + r0, [[pitch, 32], [1, nr], [0, 64]])
        hi = AP(base.tensor, base.offset + 32 * pitch + which * 64, [[pitch, 32], [0, nr], [1, 64]])
        return lo, hi

    def qk_block(dst, col, colp, b0, bw, isctx, scale, m, key):
        p1, p2, pn = P[0], P[1], P[2]
        inproj(p1[0:64, :bw], pk[0], col, 64, b0, bw)
        if isctx:
            act(S, kr[:, :bw], p1[0:64, :bw], AF.Copy, r=[pk[0]], w=['kr'], scale=scale)
        else:
            inproj(p2[0:64, :bw], pk[1], colp, 64, b0, bw)
            clo, chi = rope_tab(0, b0, bw, isctx)
            slo, shi = rope_tab(1, b0, bw, isctx)
            v3 = lambda a, lo_: a[(0 if lo_ else 32):(32 if lo_ else 64), :bw].rearrange("p (a b) -> p a b", b=64)
            for lo_, ct, stb in ((True, clo, slo), (False, chi, shi)):
                stt(S, v3(t1, lo_), v3(p1, lo_), scale, ct, ALU.mult, ALU.mult, r=[pk[0], 'rope'], w=['t1'])
                stt(S, v3(t2, lo_), v3(p2, lo_), scale, stb, ALU.mult, ALU.mult, r=[pk[1], 'rope'], w=['t2'])
            tt(S, 'pool', kr[:, :bw], t1[:, :bw], t2[:, :bw], ALU.add, r=['t1', 't2'], w=['kr'])
        act(S, dst[0:64, b0:b0 + bw], kr[:, :bw], AF.Copy, r=['kr'], w=[(key, m, b0)])
        act(S, sqn[:, :bw], kr[:, :bw], AF.Square, r=['kr'], w=['sqn'])
        mm(S, pn[0:65, :bw], selq[:, :], sqn[:, :bw], True, True, r=['sqn', 'selq'], w=[pk[2]])
        act(S, nrm[64:65, b0:b0 + bw], pn[64:65, :bw], AF.Copy, r=[pk[2]], w=[('nrm', m, b0)])

    for hd in range(2):
        for m in range(2):
            for (b0, bw, isctx) in blocks:
                qk_block(Ka[m], C_DK + hd * 128 + m * 64, C_DKP + hd * 128 + m * 64, b0, bw, isctx, 1.0, m, 'Ka')
            S.op('dve', lambda e: e.tensor_reduce(out=kmx[64:65, m:m + 1], in_=nrm[64:65, :], axis=AX.X, op=ALU.max),
                 r=[('nrm', m, b[0]) for b in blocks], w=[('kmx', m)])
            act(S, kmx[64:65, m:m + 1], kmx[64:65, m:m + 1], AF.Sqrt, r=[('kmx', m)], w=[('kmx', m)])
            S.op('dve', lambda e: e.memset(Ka[m][64:65, :], 1.0), w=[('Ka1', m)])
            for (b0, bw, isctx) in blocks:
                qk_block(Qa[m], C_DQ + hd * 128 + m * 64, C_DQP + hd * 128 + m * 64, b0, bw, isctx, 0.125, m, 'Qa')
                act(S, nrm[64:65, b0:b0 + bw], nrm[64:65, b0:b0 + bw], AF.Sqrt, r=[('nrm', m, b0)], w=[('nrm', m, b0)])
                ts(S, 'dve', Qa[m][64:65, b0:b0 + bw], nrm[64:65, b0:b0 + bw], kmx[64:65, m:m + 1], -1.0, ALU.mult, ALU.mult,
                   r=[('nrm', m, b0), ('kmx', m)], w=[('Qa', m, b0)])
        for kc in range(NKC):
            pv, kv = P[3 + kc % 2], pk[3 + kc % 2]
            for k in range(KD):
                mm(S, pv[:, 0:128], aT[:, k, kc * 128:(kc + 1) * 128], W[:, k, C_DV + hd * 128:C_DV + (hd + 1) * 128], k == 0, k == KD - 1, r=WK, w=[kv])
            act(S, Vt[:, kc, :], pv[:, 0:128], AF.Copy, r=[kv], w=[('Vt', kc)])
        KQ = [('Ka', m, b[0]) for m in range(2) for b in blocks] + [('Ka1', m) for m in range(2)] + [('Qa', m, b[0]) for m in range(2) for b in blocks]
        items = []
        for (b0, bw, isctx) in blocks:
            kcs = list(range(TC // 128)) if isctx else list(range(NKC))
            for m in range(2):
                for ii, kc in enumerate(kcs):
                    items.append((b0, bw, m, ii, kc, len(kcs)))
        LA = 2

        def stage1(si, it):
            b0, bw, m, ii, kc, nk = it
            ps_, ks_ = P[si % 3], pk[si % 3]
            Et, ke = E[si % 3], ('E', si % 3)
            mm(S, ps_[:, :bw], Ka[m][0:65, kc * 128:(kc + 1) * 128], Qa[m][0:65, b0:b0 + bw], True, True, r=KQ, w=[ks_])
            act(S, Et[:, :bw], ps_[:, :bw], AF.Exp, r=[ks_], w=[ke])

        def stage2(si, it):
            b0, bw, m, ii, kc, nk = it
            Et, ke = E[si % 3], ('E', si % 3)
            pO, pL = P[3 + 2 * m], P[4 + 2 * m]
            kO, kL = pk[3 + 2 * m], pk[4 + 2 * m]
            mm(S, pO[:, :bw], Vt[:, kc, :], Et[:, :bw], ii == 0, ii == nk - 1, r=[('Vt', kc), ke], w=[kO])
            mm(S, pL[:, :bw], onesb[:, :], Et[:, :bw], ii == 0, ii == nk - 1, r=['onesb', ke], w=[kL])
            if ii != nk - 1:
                return
            S.op('dve', lambda e: e.reciprocal(out=rl[:, :bw], in_=pL[:, :bw]), r=[kL], w=['rl'])
            tt(S, 'dve', (o1 if m == 0 else o2)[:, :bw], pO[:, :bw], rl[:, :bw], ALU.mult, r=[kO, 'rl'], w=['o%d' % (m + 1)])
            if m == 0:
                return
            stt(S, o1[:, :bw], o2[:, :bw], lamc[:, 5:6], o1[:, :bw], ALU.mult, ALU.add, r=['o1', 'o2', 'lamc'], w=['o1'])
            act(S, o2[:, :bw], o1[:, :bw], AF.Square, r=['o1'], w=['o2'])
            mm(S, P[7][:, :bw], ones[:, :], o2[:, :bw], True, True, r=['o2', 'ones'], w=[pk[7]])
            act(S, rl[:, :bw], P[7][:, :bw], AF.Sqrt, r=[pk[7], 'cst'], w=['rl'], scale=1.0 / 128, bias=cst[:, 0:1])
            S.op('dve', lambda e: e.reciprocal(out=rl[:, :bw], in_=rl[:, :bw]), r=['rl'], w=['rl'])
            stt(S, o2[:, :bw], o1[:, :bw], dn[:, 0:1], rl[:, :bw], ALU.mult, ALU.mult, r=['o1', 'rl', 'dn'], w=['o2'])
            S.dma('sp', out[rm(256 + hd * 128):rm(256 + hd * 128) + 128, b0:b0 + bw], o2[:, :bw], r=['o2'])

        for idx in range(len(items) + LA):
            if idx < len(items):
                stage1(idx, items[idx])
            if idx - LA >= 0:
                stage2(idx - LA, items[idx - LA])
    return C.close()
ODD_SPLITS_ = (256, 256, 512, 512, 32, 512, 512, 512)


def perm64():
    Pm = np.arange(64)
    Pm[0:16] = np.arange(16, 32)
    Pm[16:32] = np.arange(0, 16)
    Pm[32:48] = np.arange(48, 64)
    Pm[48:64] = np.arange(32, 48)
    return Pm


def rope_table():
    inv = (10000.0 ** (-np.arange(16, dtype=np.float32) / 16)).astype(np.float32)
    pos = np.arange(64, dtype=np.float32)
    tab = np.zeros((64, 128), np.float32)
    for p in range(64):
        i = p % 16
        ang = (pos * inv[i]).astype(np.float32)
        tab[p, 0:64] = np.cos(ang)
        sgn = -1.0 if (p % 32) < 16 else 1.0
        tab[p, 64:128] = sgn * np.sin(ang)
    return tab


def a_odd_consts():
    j = np.arange(64)
    selq = np.zeros((64, 65), np.float32)
    selq[:, 64] = 1.0
    sm = np.ones((64, 512), np.float32)
    sm[:, ::64] = 0.0
    return dict(rope=rope_table(), maskf=(j[:, None] <= j[None, :]).astype(np.float32),
                maskb=(j[:, None] >= j[None, :]).astype(np.float32), scanmask=sm,
                ident=np.eye(128, dtype=np.float32), selq=selq)


def a_odd_params(half, w_in, gate_w, gate_b, gla_g, lq1, lk1, lq2, lk2, diff_g):
    offs = np.concatenate([[0], np.cumsum(ODD_SPLITS_)])
    heads = [2 * half, 2 * half + 1]
    Pm = perm64()
    cols = []
    for gh in heads:
        cols += list(offs[0] + gh * 64 + np.arange(64))
    for gh in heads:
        cols += list(offs[1] + gh * 64 + np.arange(64))
    for gh in heads:
        cols += list(offs[2] + gh * 128 + np.arange(128))
    for gh in heads:
        cols += list(offs[3] + gh * 128 + np.arange(128))
    cols += list(offs[4] + np.arange(32))
    for base, pm in ((offs[5], None), (offs[5], Pm), (offs[6], None), (offs[6], Pm)):
        for gh in heads:
            for m in range(2):
                idx = np.arange(64) if pm is None else pm
                cols += list(base + gh * 128 + m * 64 + idx)
    for gh in heads:
        cols += list(offs[7] + gh * 128 + np.arange(128))
    W = np.ascontiguousarray(np.asarray(w_in)[:, np.array(cols)])
    gcols = np.concatenate([gh * 64 + np.arange(64) for gh in heads])
    gwa = np.zeros((17, 256), np.float32)
    for d in range(2):
        gwa[0:16, d * 128:(d + 1) * 128] = np.asarray(gate_w)[d][:, gcols]
        gwa[16, d * 128:(d + 1) * 128] = np.asarray(gate_b)[d][gcols]
    gg = np.ascontiguousarray(np.asarray(gla_g).reshape(4, 128)[heads].T)
    lam = np.tile(np.concatenate([lq1, lk1, lq2, lk2])[None].astype(np.float32), (128, 1))
    return dict(W=W, gate_w_aug=gwa, gla_g=gg, diff_g=np.asarray(diff_g, np.float32).reshape(128, 1), lam_rows=lam)


def a_vecs(mod_lat, mod_ctx):
    return np.ascontiguousarray(np.concatenate([fm(mod_lat[0]), fm(mod_lat[1]), fm(mod_ctx[0]), fm(mod_ctx[1])], axis=1))


def a_even_consts():
    selq = np.zeros((64, 65), np.float32)
    selq[:, 64] = 1.0
    io = np.arange(1, S5L + 1, dtype=np.float32)
    iota = np.tile(np.concatenate([io, io[::-1]])[None], (128, 1))
    return dict(ident=np.eye(128, dtype=np.float32), selq=selq, iota=np.ascontiguousarray(iota))


def natten_bm(rpb, half):
    rpb = np.asarray(rpb, np.float32)
    bm = np.full((4, 128, 32 * 64), -30000.0, np.float32)
    w = np.arange(64)
    cs = np.clip(w - 8, 0, 48)
    for hh in range(4):
        h = 4 * half + hh
        for dl in range(8):
            for i in range(4):
                for jj in range(2):
                    dr = 2 * i + jj - dl + 7
                    for kcol in range(64):
                        ok = (kcol >= cs) & (kcol < cs + 16)
                        dc = kcol - w + 15
                        wi = w[ok]
                        bm[hh, jj * 64 + kcol, (dl * 4 + i) * 64 + wi] = rpb[h, dr, dc[ok]]
    return bm


def a_even_params(half, w_in, lam_re, lam_im, log_dt, b_re, b_im, c_re, c_im, d_skip, rpb):
    w_in = np.asarray(w_in)
    cols = list(256 * half + np.arange(256))
    for base in (512, 1024, 1536):
        cols += list(base + 256 * half + np.arange(256))
    W = np.ascontiguousarray(w_in[:, np.array(cols)])
    lam = np.zeros((128, 48), np.float32)
    brb = np.zeros((128, 16, 128), np.float32)
    bib = np.zeros((128, 16, 128), np.float32)
    cre = np.zeros((128, 16, 128), np.float32)
    cim = np.zeros((128, 16, 128), np.float32)
    for d in range(2):
        for j in range(8):
            s = d * 8 + j
            for gl in range(2):
                g = 16 * half + 2 * j + gl
                rows = slice(gl * 64, (gl + 1) * 64)
                lam[rows, s] = lam_re[d, g]
                lam[rows, 16 + s] = lam_im[d, g]
                lam[rows, 32 + s] = log_dt[d, g]
                c0 = 32 * (j % 4) + gl * 16
                brb[rows, s, c0:c0 + 16] = b_re[d, g]
                bib[rows, s, c0:c0 + 16] = b_im[d, g]
                cc = 32 * (j % 4) + gl * 16
                cre[rows, s, cc:cc + 16] = np.asarray(c_re[d, g]).T
                cim[rows, s, cc:cc + 16] = np.asarray(c_im[d, g]).T
    dsk = np.ascontiguousarray(np.asarray(d_skip, np.float32)[256 * half:256 * half + 256].reshape(2, 128).T)
    return dict(W=W, s5_lam=lam, s5_brb=brb.reshape(128, -1), s5_bib=bib.reshape(128, -1), s5_cre=cre.reshape(128, -1),
                s5_cim=cim.reshape(128, -1), s5_d=dsk, bm=natten_bm(rpb, half))
C_U, C_NQ, C_NK, C_NV = 0, 256, 512, 768
NCOL_EVEN = 1024
TWO_PI = 2.0 * math.pi
CW1 = 6.28125
CW2 = TWO_PI - CW1
S5L = 256


def reduce_angle(S, dst, src, kf, ki, key_src, key_dst, key_tmp):
    ts(S, 'dve', kf, src, 1.0 / TWO_PI, None, ALU.mult, None, r=[key_src], w=[key_tmp])
    S.op('dve', lambda e: e.tensor_copy(out=ki, in_=kf), r=[key_tmp], w=[key_tmp + 'i'])
    S.op('dve', lambda e: e.tensor_copy(out=kf, in_=ki), r=[key_tmp + 'i'], w=[key_tmp])
    stt(S, dst, kf, -CW1, src, ALU.mult, ALU.add, r=[key_tmp, key_src], w=[key_dst])
    stt(S, dst, kf, -CW2, dst, ALU.mult, ALU.add, r=[key_tmp, key_dst], w=[key_dst])
    ts(S, 'dve', kf, dst, math.pi, TWO_PI, ALU.is_gt, ALU.mult, r=[key_dst], w=[key_tmp])
    tt(S, 'dve', dst, dst, kf, ALU.subtract, r=[key_dst, key_tmp], w=[key_dst])
    ts(S, 'dve', kf, dst, -math.pi, TWO_PI, ALU.is_lt, ALU.mult, r=[key_dst], w=[key_tmp])
    tt(S, 'dve', dst, dst, kf, ALU.add, r=[key_dst, key_tmp], w=[key_dst])
    ts(S, 'dve', dst, dst, math.pi, -math.pi, ALU.min, ALU.max, r=[key_dst], w=[key_dst])


def build_A_even(TC, TL, stage=9, nst=16, C=None, rowmap=None):
    T = TC + TL
    ROWS = TL // 64
    C = C or Ctx()
    rm = rowmap or (lambda r0: r0)
    S = C.S
    hT = C.din("hT", [D, T])
    vecs_d = C.din("vecsA", [128, 4 * KD])
    W_d = C.din("W", [D, NCOL_EVEN])
    bm_d = C.din("bm", [4, 128, 32 * 64])
    ident_d = C.din("ident", [128, 128])
    selq_d = C.din("selq", [64, 65])
    lam_d = C.din("s5_lam", [128, 48])
    brb_d = C.din("s5_brb", [128, 16 * 128])
    bib_d = C.din("s5_bib", [128, 16 * 128])
    cre_d = C.din("s5_cre", [128, 16 * 128])
    cim_d = C.din("s5_cim", [128, 16 * 128])
    dsk_d = C.din("s5_d", [128, 2])
    iota_d = C.din("iota", [128, 2 * S5L])
    out = C.dout("out", [512, T])

    blocks = [(0, TC, True)] + [(TC + 512 * i, 512, False) for i in range(TL // 512)]
    NKC_C = TC // 128
    NKE = TL // 128
    NKO = TL // 128 - 1

    vecs = C.sb([128, 4, KD])
    uT = C.sb([128, 2, T], BF16)
    identb = C.sb([128, 128], BF16)
    identf = C.sb([128, 128])
    selq = C.sb([64, 65])
    onesb = C.sb([128, 128], BF16)
    cst = C.sb([128, 4])
    USZ = max(int(2.0 * T) + 4096 + 4096 + 12000, 41500)
    U = C.sb([128, USZ])
    P = [C.ps([128, 512]) for _ in range(8)]
    pk = ['ps%d' % i for i in range(8)]

    S.dma('sp', vecs[:, :, :], vecs_d.rearrange("p (v k) -> p v k", v=4), w=['vecs'])
    S.dma('pool', identb[:, :], ident_d, w=['identb'])
    S.dma('sp', identf[:, :], ident_d, w=['identf'])
    S.dma('sp', selq[:, :], selq_d, w=['selq'])
    S.op('dve', lambda e: e.memset(onesb[:, :], 1.0), w=['onesb'])
    S.op('dve', lambda e: e.memset(cst[:, 0:1], EPS), w=['cst'])
    ts(S, 'dve', vecs[:, 1, :], vecs[:, 1, :], 1.0, None, ALU.add, None, r=['vecs'], w=['vecs'])
    ts(S, 'dve', vecs[:, 3, :], vecs[:, 3, :], 1.0, None, ALU.add, None, r=['vecs'], w=['vecs'])

    off = [0]

    def carve(shape, dt=F32):
        n = int(np.prod(shape[1:]))
        words = n if dt == F32 else (n + 1) // 2
        o = off[0]
        off[0] += words
        assert off[0] <= USZ, (off[0], USZ)
        v = U[0:shape[0], o:o + words]
        if dt != F32:
            v = v.bitcast(dt)[:, 0:n]
        if len(shape) == 3:
            v = v.rearrange("p (a b) -> p a b", a=shape[1])
        return v

    aT = carve([128, KD, T], BF16)
    W = carve([128, KD, NCOL_EVEN], BF16)
    hb = carve([128, KD, 512])
    S.dma('pool', W[:, :, :], W_d.rearrange("(k p) n -> p k n", p=128), w=['W'])
    modulate_aT(C, hT, vecs, aT, blocks, hb, 'hb')
    barrier(S)
    off[0] -= KD * 512
    WK = []

    def inproj(ps_out, kps, col0, ncol, t0, tw):
        for k in range(KD):
            mm(S, ps_out, W[:, k, col0:col0 + ncol], aT[:, k, t0:t0 + tw], k == 0, k == KD - 1, r=WK, w=[kps])

    for (b0, bw, isctx) in blocks:
        for a in range(2):
            pu, ku = P[a], pk[a]
            inproj(pu[:, :bw], ku, C_U + a * 128, 128, b0, bw)
            act(S, uT[:, a, b0:b0 + bw], pu[:, :bw], AF.Copy, r=[ku], w=[('uT', a, b0)])

    if stage <= 1:
        return C.close()
    base_n = off[0]
    Qa = carve([65, T], BF16)
    Ka = carve([65, T], BF16)
    Vc = carve([128, NKC_C, 64], BF16)
    Ve = carve([128, NKE, 64], BF16)
    Vo = carve([128, max(NKO, 1), 64], BF16)
    BMt = carve([128, 32, 64], BF16)
    nrm = carve([65, 512])
    kmx = carve([65, 4])
    kr = carve([64, 512])
    sqn = carve([64, 512])
    E = [carve([128, 6, 64], BF16) for _ in range(3)]
    Ec = [carve([128, 512], BF16) for _ in range(2)]
    rl = carve([64, 512])
    ob = carve([64, 512])

    def qk_block(dst, col, b0, bw, scale, key):
        p1, pn = P[0], P[2]
        inproj(p1[0:64, :bw], pk[0], col, 64, b0, bw)
        act(S, kr[:, :bw], p1[0:64, :bw], AF.Copy, r=[pk[0]], w=['kr'], scale=scale)
        act(S, dst[0:64, b0:b0 + bw], kr[:, :bw], AF.Copy, r=['kr'], w=[(key, b0)])
        act(S, sqn[:, :bw], kr[:, :bw], AF.Square, r=['kr'], w=['sqn'])
        mm(S, pn[0:65, :bw], selq[:, :], sqn[:, :bw], True, True, r=['sqn', 'selq'], w=[pk[2]])
        act(S, nrm[64:65, :bw], pn[64:65, :bw], AF.Copy, r=[pk[2]], w=['nrm'])

    for hh in range(4):
        S.dma('pool', BMt[:, :, :], bm_d[hh].rearrange("p (a b) -> p a b", b=64), w=['BMt'])
        S.op('dve', lambda e: e.memset(kmx[64:65, 0:1], 0.0), w=['kmx'])
        for (b0, bw, isctx) in blocks:
            qk_block(Ka, C_NK + hh * 64, b0, bw, 1.0, 'Ka')
            S.op('dve', lambda e: e.tensor_reduce(out=kmx[64:65, 1:2], in_=nrm[64:65, :bw], axis=AX.X, op=ALU.max), r=['nrm'], w=['kmx1'])
            tt(S, 'dve', kmx[64:65, 0:1], kmx[64:65, 0:1], kmx[64:65, 1:2], ALU.max, r=['kmx', 'kmx1'], w=['kmx'])
        act(S, kmx[64:65, 0:1], kmx[64:65, 0:1], AF.Sqrt, r=['kmx'], w=['kmx'])
        S.op('dve', lambda e: e.memset(Ka[64:65, :], 1.0), w=['Ka1'])
        for (b0, bw, isctx) in blocks:
            qk_block(Qa, C_NQ + hh * 64, b0, bw, 0.125, 'Qa')
            act(S, nrm[64:65, :bw], nrm[64:65, :bw], AF.Sqrt, r=['nrm'], w=['nrm'])
            ts(S, 'dve', Qa[64:65, b0:b0 + bw], nrm[64:65, :bw], kmx[64:65, 0:1], -1.0, ALU.mult, ALU.mult, r=['nrm', 'kmx'], w=[('Qa', b0)])
        vi = 0
        for (dstV, vn, n, tbase) in ((Vc, 'Vc', NKC_C, 0), (Ve, 'Ve', NKE, TC), (Vo, 'Vo', NKO, TC + 64)):
            for kc in range(n):
                pv, kv = P[3 + vi % 2], pk[3 + vi % 2]
                vi += 1
                t0 = tbase + kc * 128
                for k in range(KD):
                    mm(S, pv[:, 0:64], aT[:, k, t0:t0 + 128], W[:, k, C_NV + hh * 64:C_NV + (hh + 1) * 64], k == 0, k == KD - 1, r=WK, w=[kv])
                act(S, dstV[:, kc, :], pv[:, 0:64], AF.Copy, r=[kv], w=[('V', vn, kc)])
        VK = [('V', vn, kc) for (vn, n) in (('Vc', NKC_C), ('Ve', NKE), ('Vo', NKO)) for kc in range(n)]
        KQ = [('Ka', b[0]) for b in blocks] + ['Ka1'] + [('Qa', b[0]) for b in blocks]
        pO, pL = P[5], P[6]
        for kc in range(NKC_C):
            ps_, ks_ = P[kc % 2], pk[kc % 2]
            mm(S, ps_[:, :TC], Ka[0:65, kc * 128:(kc + 1) * 128], Qa[0:65, 0:TC], True, True, r=KQ, w=[ks_])
            act(S, Ec[kc % 2][:, :TC], ps_[:, :TC], AF.Exp, r=[ks_], w=[('Ec', kc % 2)])
            mm(S, pO[0:64, :TC], Vc[:, kc, :], Ec[kc % 2][:, :TC], kc == 0, kc == NKC_C - 1, r=VK + [('Ec', kc % 2)], w=[pk[5]])
            mm(S, pL[0:64, :TC], onesb[:, 0:64], Ec[kc % 2][:, :TC], kc == 0, kc == NKC_C - 1, r=['onesb', ('Ec', kc % 2)], w=[pk[6]])
        S.op('dve', lambda e: e.reciprocal(out=rl[:, :TC], in_=pL[0:64, :TC]), r=[pk[6]], w=['rl'])
        tt(S, 'dve', ob[:, :TC], pO[0:64, :TC], rl[:, :TC], ALU.mult, r=[pk[5], 'rl'], w=['ob'])
        S.dma('sp', out[rm(256 + hh * 64):rm(256 + hh * 64) + 64, 0:TC], ob[:, :TC], r=['ob'])
        LA = 2
        rowinfo = {}

        def n_stage1(r):
            rs = min(max(r - 4, 0), ROWS - 8)
            dl = r - rs
            q0 = TC + 64 * r
            ps_, ks_ = P[r % 3], pk[r % 3]
            Et, ke = E[r % 3], ('E', r % 3)
            ps3 = ps_[:, 0:384].rearrange("p (a b) -> p a b", b=64)
            chunks = []
            for i in range(4):
                k0 = TC + 64 * rs + 128 * i
                mm(S, ps3[:, i, :], Ka[0:65, k0:k0 + 128], Qa[0:65, q0:q0 + 64], True, False, r=KQ, w=[ks_])
                mm(S, ps3[:, i, :], identb[:, :], BMt[:, dl * 4 + i, :], False, True, r=['identb', 'BMt'], w=[ks_])
                if rs % 2 == 0:
                    chunks.append(Ve[:, rs // 2 + i, :])
                else:
                    chunks.append(Vo[:, (rs - 1) // 2 + i, :])
            for kc in range(NKC_C):
                mm(S, ps3[:, 4 + kc, :], Ka[0:65, kc * 128:(kc + 1) * 128], Qa[0:65, q0:q0 + 64], True, True, r=KQ, w=[ks_])
                chunks.append(Vc[:, kc, :])
            nck = 4 + NKC_C
            act(S, Et[:, 0:nck, :], ps3[:, 0:nck, :], AF.Exp, r=[ks_], w=[ke])
            rowinfo[r] = chunks

        def n_stage2(r):
            r0 = r - (r % 4)
            rr = r % 4
            pOL, kOL = P[5 + (r0 // 4) % 2], pk[5 + (r0 // 4) % 2]
            pol4 = pOL[0:64, :].rearrange("p (r a b) -> p r a b", r=4, a=2)
            Et, ke = E[r % 3], ('E', r % 3)
            chunks = rowinfo.pop(r)
            nck = 4 + NKC_C
            for i in range(nck):
                mm(S, pol4[:, rr, 0, :], chunks[i], Et[:, i, :], i == 0, i == nck - 1, r=VK + [ke], w=[kOL])
            for i in range(nck):
                mm(S, pol4[:, rr, 1, :], onesb[:, 0:64], Et[:, i, :], i == 0, i == nck - 1, r=['onesb', ke], w=[kOL])
            if rr != 3:
                return
            c4 = (r0 % 8) * 64
            rl4 = rl[:, c4:c4 + 256].rearrange("p (r b) -> p r b", b=64)
            ob4 = ob[:, c4:c4 + 256].rearrange("p (r b) -> p r b", b=64)
            S.op('dve', lambda e: e.reciprocal(out=rl4, in_=pol4[:, :, 1, :]), r=[kOL], w=['rl'])
            tt(S, 'dve', ob4, pol4[:, :, 0, :], rl4, ALU.mult, r=[kOL, 'rl'], w=['ob'])
            if r0 % 8 == 4 or r0 + 4 >= ROWS:
                t0 = TC + 64 * (r0 - (r0 % 8))
                nw = 64 * ((r0 % 8) + 4)
                S.dma('sp', out[rm(256 + hh * 64):rm(256 + hh * 64) + 64, t0:t0 + nw], ob[:, 0:nw], r=['ob'])

        for r in range(ROWS + LA):
            if r < ROWS:
                n_stage1(r)
            if r - LA >= 0:
                n_stage2(r - LA)

    if stage <= 2:
        return C.close()
    barrier(S)
    off[0] = 0
    L = S5L
    ysum = carve([128, 2, T])
    cosT = carve([128, 16, L])
    sinT = carve([128, 16, L])
    BT = [carve([128, 16, 128], BF16), carve([128, 16, 128], BF16)]
    CT = [carve([128, 16, 128], BF16), carve([128, 16, 128], BF16), carve([128, 16, 128], BF16)]
    negI = carve([128, 128])
    cry = carve([128, 2, 4])
    lam = carve([128, 3, 16])
    sc = carve([128, 16, 16])
    hprev = carve([128, 16, 2])
    dsk = carve([128, 2])
    iota = carve([128, 2, L])
    wk = carve([128, 16, L])
    wkB = carve([128, 16, L])
    hbf = carve([128, 8, L], BF16)
    stg0 = off[0]
    brb = carve([128, 16, 128])
    bib = carve([128, 16, 128])
    cst32 = carve([128, 2, 16 * 128])
    kI = U[:, off[0]:off[0] + L].bitcast(I32)
    off[0] += L
    assert off[0] <= USZ, (off[0], USZ)

    S.dma('sp', lam[:, :, :], lam_d.rearrange("p (a b) -> p a b", a=3), w=['lam'])
    S.dma('sp', brb[:, :, :], brb_d.rearrange("p (a b) -> p a b", a=16), w=['brb'])
    S.dma('sp', bib[:, :, :], bib_d.rearrange("p (a b) -> p a b", a=16), w=['bib'])
    S.dma('sp', cst32[:, 0, :], cre_d, w=['c32'])
    S.dma('sp', cst32[:, 1, :], cim_d, w=['c32'])
    S.dma('sp', dsk[:, :], dsk_d, w=['dsk'])
    S.dma('sp', iota[:, :, :], iota_d.rearrange("p (a b) -> p a b", a=2), w=['iota'])
    S.op('dve', lambda e: e.tensor_copy(out=CT[0][:, :, :], in_=cst32[:, 0, :].rearrange("p (a b) -> p a b", b=128)), r=['c32'], w=['CT'])
    ts(S, 'dve', CT[1][:, :, :], cst32[:, 1, :].rearrange("p (a b) -> p a b", b=128), -1.0, None, ALU.mult, None, r=['c32'], w=['CT'])
    ts(S, 'dve', CT[2][:, :, :], cst32[:, 0, :].rearrange("p (a b) -> p a b", b=128), -1.0, None, ALU.mult, None, r=['c32'], w=['CT'])
    ts(S, 'dve', negI[:, :], identf[:, :], -1.0, None, ALU.mult, None, r=['identf'], w=['negI'])
    if stage <= 2.2:
        return C.close()
    (Q_DT, Q_LR, Q_TH, Q_R, Q_THR, Q_SIN, Q_COS, Q_ARE, Q_AIM, Q_DEN, Q_FRE, Q_FIM, Q_T1, Q_T2, Q_NFIM, Q_KF) = range(16)
    q = lambda i: sc[:, i, :]
    SK = dict(r=['sc', 'lam'], w=['sc'])
    act(S, q(Q_DT), lam[:, 2, :], AF.Exp, **SK)
    tt(S, 'dve', q(Q_LR), lam[:, 0, :], q(Q_DT), ALU.mult, **SK)
    tt(S, 'dve', q(Q_TH), lam[:, 1, :], q(Q_DT), ALU.mult, **SK)
    act(S, q(Q_R), q(Q_LR), AF.Exp, **SK)
    kI16 = kI[:, 0:16]
    reduce_angle(S, q(Q_THR), q(Q_TH), q(Q_KF), kI16, 'sc', 'sc', 'sc')
    act(S, q(Q_SIN), q(Q_THR), AF.Sin, **SK)
    ts(S, 'dve', q(Q_T1), q(Q_THR), math.pi / 2, None, ALU.add, None, **SK)
    reduce_angle(S, q(Q_T2), q(Q_T1), q(Q_KF), kI16, 'sc', 'sc', 'sc')
    act(S, q(Q_COS), q(Q_T2), AF.Sin, **SK)
    tt(S, 'dve', q(Q_ARE), q(Q_R), q(Q_COS), ALU.mult, **SK)
    tt(S, 'dve', q(Q_AIM), q(Q_R), q(Q_SIN), ALU.mult, **SK)
    tt(S, 'dve', q(Q_T1), lam[:, 0, :], lam[:, 0, :], ALU.mult, **SK)
    tt(S, 'dve', q(Q_T2), lam[:, 1, :], lam[:, 1, :], ALU.mult, **SK)
    tt(S, 'dve', q(Q_DEN), q(Q_T1), q(Q_T2), ALU.add, **SK)
    S.op('dve', lambda e: e.reciprocal(out=q(Q_DEN), in_=q(Q_DEN)), **SK)
    ts(S, 'dve', q(Q_ARE), q(Q_ARE), -1.0, None, ALU.add, None, **SK)
    tt(S, 'dve', q(Q_T1), q(Q_ARE), lam[:, 0, :], ALU.mult, **SK)
    tt(S, 'dve', q(Q_T2), q(Q_AIM), lam[:, 1, :], ALU.mult, **SK)
    tt(S, 'dve', q(Q_FRE), q(Q_T1), q(Q_T2), ALU.add, **SK)
    tt(S, 'dve', q(Q_FRE), q(Q_FRE), q(Q_DEN), ALU.mult, **SK)
    tt(S, 'dve', q(Q_T1), q(Q_AIM), lam[:, 0, :], ALU.mult, **SK)
    tt(S, 'dve', q(Q_T2), q(Q_ARE), lam[:, 1, :], ALU.mult, **SK)
    tt(S, 'dve', q(Q_FIM), q(Q_T1), q(Q_T2), ALU.subtract, **SK)
    tt(S, 'dve', q(Q_FIM), q(Q_FIM), q(Q_DEN), ALU.mult, **SK)
    ts(S, 'dve', q(Q_NFIM), q(Q_FIM), -1.0, None, ALU.mult, None, **SK)
    if stage <= 2.5:
        return C.close()
    for s in range(nst):
        d, j = s // 8, s % 8
        pb = 64 * ((j % 4) // 2)
        col = lambda i: sc[:, i, s:s + 1]
        for part, (x0, f0, x1, f1) in enumerate(((brb, Q_FRE, bib, Q_NFIM), (bib, Q_FRE, brb, Q_FIM))):
            tmpb = wk[:, part, 0:128]
            ts(S, 'dve', tmpb, x0[:, s, :], col(f0), None, ALU.mult, None, r=['sc', 'brb', 'bib'], w=[('wk', part)])
            stt(S, tmpb, x1[:, s, :], col(f1), tmpb, ALU.mult, ALU.add, r=['sc', 'brb', 'bib', ('wk', part)], w=[('wk', part)])
            S.op('pe', lambda e: e.transpose(out=P[part][:, 0:128], in_=tmpb, identity=identf[:, :]), r=[('wk', part), 'identf'], w=[pk[part]])
            act(S, BT[part][:, s, :], P[part][:, 0:128], AF.Copy, r=[pk[part]], w=['BT'])
        ang, ang2, kf = wk[:, 2, :], wk[:, 3, :], wk[:, 4, :]
        ts(S, 'dve', ang, iota[:, d, :], col(Q_THR), None, ALU.mult, None, r=['sc', 'iota'], w=[('wk', 2)])
        reduce_angle(S, ang2, ang, kf, kI[:, 0:L], ('wk', 2), ('wk', 3), 'kf')
        act(S, sinT[:, s, :], ang2, AF.Sin, r=[('wk', 3)], w=[('tab', s)])
        ts(S, 'dve', ang, ang, math.pi / 2, None, ALU.add, None, r=[('wk', 2)], w=[('wk', 2)])
        reduce_angle(S, ang2, ang, kf, kI[:, 0:L], ('wk', 2), ('wk', 3), 'kf')
        act(S, cosT[:, s, :], ang2, AF.Sin, r=[('wk', 3)], w=[('tab', s)])
        if stage <= 2.8 and s == nst - 1:
            return C.close()
    off[0] = stg0
    barrier(S)

    if stage <= 3:
        return C.close()
    chunks_f = [(0, TC)] if TC <= L else [(i, L) for i in range(0, TC, L)]
    lat_ch = [(TC + i, L) for i in range(0, TL, L)]
    items = []
    for d in range(2):
        order = (chunks_f + lat_ch) if d == 0 else (chunks_f[::-1] + lat_ch[::-1])
        for ci_, (t0, Lc) in enumerate(order):
            for a in range(2):
                for jj in range(4):
                    items.append((d, t0, Lc, a, jj, ci_ == 0))

    def geom(n):
        d, t0, Lc, a, jj, first = items[n]
        j = 4 * a + jj
        s = d * 8 + j
        wsel = n % 2
        wkc = wk if wsel == 0 else wkB
        if d == 0:
            cs_, sn_ = cosT[:, s, 0:Lc], sinT[:, s, 0:Lc]
        else:
            cs_, sn_ = cosT[:, s, L - Lc:L], sinT[:, s, L - Lc:L]
        return d, t0, Lc, a, jj, first, s, wsel, wkc, cs_, sn_

    def front(n):
        d, t0, Lc, a, jj, first, s, wsel, wkc, cs_, sn_ = geom(n)
        pb = 64 * (jj // 2)
        X, kX = P[n % 2], pk[n % 2]
        V, kV = P[4 + n % 2], pk[4 + n % 2]
        A_, B_ = X[:, 0:Lc], X[:, 256:256 + Lc]
        mm(S, A_, BT[0][:, s, :], uT[:, a, t0:t0 + Lc], True, True, r=['BT', ('uT', a)], w=[kX])
        mm(S, B_, BT[1][:, s, :], uT[:, a, t0:t0 + Lc], True, True, r=['BT', ('uT', a)], w=[kX])
        w_ = lambda i: wkc[:, i, 0:Lc]
        tt(S, 'dve', w_(0), A_, cs_, ALU.mult, r=[kX], w=[('wk', wsel, 0)])
        tt(S, 'dve', w_(1), B_, sn_, ALU.mult, r=[kX], w=[('wk', wsel, 1)])
        tt(S, 'dve', w_(2), B_, cs_, ALU.mult, r=[kX], w=[('wk', wsel, 2)])
        tt(S, 'dve', w_(3), A_, sn_, ALU.mult, r=[kX], w=[('wk', wsel, 3)])
        mm(S, V[:, 0:Lc], identf[:, :], w_(0), True, False, r=['identf', ('wk', wsel, 0)], w=[kV])
        mm(S, V[:, 0:Lc], identf[:, :], w_(1), False, True, r=['identf', ('wk', wsel, 1)], w=[kV])
        mm(S, V[:, 256:256 + Lc], identf[:, :], w_(2), True, False, r=['identf', ('wk', wsel, 2)], w=[kV])
        mm(S, V[:, 256:256 + Lc], negI[:, :], w_(3), False, True, r=['negI', ('wk', wsel, 3)], w=[kV])

    def back(n):
        d, t0, Lc, a, jj, first, s, wsel, wkc, cs_, sn_ = geom(n)
        pb = 64 * (jj // 2)
        py, kpy = P[2 + a], pk[2 + a]
        V, kV = P[4 + n % 2], pk[4 + n % 2]
        w_ = lambda i: wkc[:, i, 0:Lc]
        rcol = sc[:, Q_R, s:s + 1]
        rbc = AP(rcol.tensor, rcol.offset, [list(rcol.ap[0]), [0, Lc]])
        for part in range(2):
            src, dst = V[:, 256 * part:256 * part + Lc], w_(6 + part)
            ini = 0.0 if first else hprev[:, s, part:part + 1]
            if d == 0:
                S.op('dve', lambda e: e.tensor_tensor_scan(out=dst, data0=rbc, data1=src, initial=ini, op0=ALU.mult, op1=ALU.add),
                     r=[kV, ('hprev', s)], w=[('wk', wsel, 6 + part)])
            else:
                S.op('dve', lambda e: e.tensor_tensor_scan(out=rev_ap(dst, Lc), data0=rbc, data1=rev_ap(src, Lc), initial=ini,
                                                           op0=ALU.mult, op1=ALU.add), r=[kV, ('hprev', s)], w=[('wk', wsel, 6 + part)])
        gre, gim = w_(6), w_(7)
        q = 4 * (n % 2)
        u_ = lambda i: hbf[:, q + i, 0:Lc]
        tt(S, 'pool', u_(0), gre, cs_, ALU.mult, r=[('wk', wsel, 6)], w=[('hbf', q + 0)])
        tt(S, 'pool', u_(1), gim, sn_, ALU.mult, r=[('wk', wsel, 7)], w=[('hbf', q + 1)])
        tt(S, 'dve', u_(2), gre, sn_, ALU.mult, r=[('wk', wsel, 6)], w=[('hbf', q + 2)])
        tt(S, 'dve', u_(3), gim, cs_, ALU.mult, r=[('wk', wsel, 7)], w=[('hbf', q + 3)])
        lastc = Lc - 1 if d == 0 else 0
        gl = lambda i: wkc[:, i, lastc:lastc + 1]
        cc, sn1 = cs_[:, lastc:lastc + 1], sn_[:, lastc:lastc + 1]
        ck = ('cry', n % 2)
        cr = lambda i: cry[:, n % 2, i:i + 1]
        act(S, cr(0), gl(6), AF.Identity, r=[('wk', wsel, 6)], w=[ck], scale=cc)
        act(S, cr(1), gl(7), AF.Identity, r=[('wk', wsel, 7)], w=[ck], scale=sn1)
        act(S, hprev[:, s, 0:1], cr(1), AF.Identity, r=[ck], w=[('hprev', s)], scale=-1.0, bias=cr(0))
        act(S, cr(2), gl(6), AF.Identity, r=[('wk', wsel, 6)], w=[ck], scale=sn1)
        act(S, hprev[:, s, 1:2], gl(7), AF.Identity, r=[ck, ('wk', wsel, 7)], w=[('hprev', s)], scale=cc, bias=cr(2))
        mm(S, py[:, 0:Lc], CT[0][:, s, :], u_(0), jj == 0, False, r=['CT', ('hbf', q + 0)], w=[kpy])
        mm(S, py[:, 0:Lc], CT[2][:, s, :], u_(1), False, False, r=['CT', ('hbf', q + 1)], w=[kpy])
        mm(S, py[:, 0:Lc], CT[1][:, s, :], u_(2), False, False, r=['CT', ('hbf', q + 2)], w=[kpy])
        mm(S, py[:, 0:Lc], CT[1][:, s, :], u_(3), False, jj == 3, r=['CT', ('hbf', q + 3)], w=[kpy])
        if jj != 3:
            return
        ys_ = ysum[:, a, t0:t0 + Lc]
        if d == 0:
            stt(S, ys_, uT[:, a, t0:t0 + Lc], dsk[:, a:a + 1], py[:, 0:Lc], ALU.mult, ALU.add, r=[kpy, ('uT', a), 'dsk'], w=[('ys', a, t0)])
        else:
            tt(S, 'dve', ys_, ys_, py[:, 0:Lc], ALU.add, r=[kpy, ('ys', a, t0)], w=[('ys', a, t0)])
            S.dma('sp', out[rm(a * 128):rm(a * 128) + 128, t0:t0 + Lc], ys_, r=[('ys', a, t0)])

    for n in range(len(items) + 1):
        if n < len(items):
            front(n)
        if n >= 1:
            back(n - 1)
    return C.close()
def build_M(ncols):
    C = Ctx()
    S = C.S
    cT_d = C.din("cT", [128, KD * 5])
    w_d = C.din("w", [D, ncols])
    b_d = C.din("b", [5, ncols])
    out = C.dout("out", [5, ncols])
    cT = C.sb([128, KD, 5])
    sg = C.sb([128, KD, 5])
    wt = [C.sb([128, KD, 512]) for _ in range(2)]
    bt = C.sb([5, ncols])
    ot = C.sb([5, ncols])
    P = [C.ps([128, 512]) for _ in range(2)]
    S.dma('sp', cT[:, :, :], cT_d.rearrange("p (k c) -> p k c", k=KD), w=['cT'])
    S.dma('sp', bt[:, :], b_d, w=['bt'])
    act(S, sg[:, :, :], cT[:, :, :], AF.Sigmoid, r=['cT'], w=['sg'])
    tt(S, 'dve', cT[:, :, :], cT[:, :, :], sg[:, :, :], ALU.mult, r=['cT', 'sg'], w=['cT'])
    for i in range(ncols // 512):
        w_ = wt[i % 2]
        S.dma('sp' if i % 2 == 0 else 'act', w_[:, :, :], w_d[:, i * 512:(i + 1) * 512].rearrange("(k p) n -> p k n", p=128), w=[('w', i % 2)])
        for k in range(KD):
            mm(S, P[i % 2][0:5, :], cT[:, k, :], w_[:, k, :], k == 0, k == KD - 1, r=['cT', ('w', i % 2)], w=[('p', i % 2)])
        tt(S, 'dve', ot[:, i * 512:(i + 1) * 512], P[i % 2][0:5, :], bt[:, i * 512:(i + 1) * 512], ALU.add, r=[('p', i % 2), 'bt'], w=['ot'])
    S.dma('sp', out, ot[:, :], r=['ot'])
    return C.close()
TC_FULL, TL_FULL, NB = 256, 4096, 4
_PROG = {}


def _prog(name, fn):
    if name not in _PROG:
        _PROG[name] = fn()
    return _PROG[name]


def _run(nc, in_maps):
    in_maps = [{k: np.ascontiguousarray(v, dtype=np.float32) for k, v in m.items()} for m in in_maps]
    return run_bass_kernel_spmd(nc, in_maps, core_ids=list(range(8))).results


def kernel_unfused(**I):
    I = {k: np.asarray(v) for k, v in I.items()}
    T = TC_FULL + TL_FULL
    c_all = np.concatenate([I['c'], I['c_ctx'][None]], 0).astype(np.float32)
    cT = np.ascontiguousarray(c_all.reshape(5, KD, 128).transpose(2, 1, 0).reshape(128, KD * 5))
    maps = []
    for c in range(8):
        li, hf = c // 2, c % 2
        maps.append(dict(cT=cT, w=I['mod_w'][li][:, hf * 3072:(hf + 1) * 3072],
                         b=np.tile(I['mod_b'][li][None, hf * 3072:(hf + 1) * 3072], (5, 1))))
    r = _run(_prog('M', lambda: build_M(3072)), maps)
    mods = [np.concatenate([r[2 * li]['out'], r[2 * li + 1]['out']], 1).reshape(5, 6, D) for li in range(DEPTH)]
    hT = [np.ascontiguousarray(np.concatenate([I['ctx'][b], I['x'][b]], 0).T) for b in range(NB)]
    chunksB = [(0, 128, True)] + [(128 + 512 * i, 512, False) for i in range(4)]
    groupsB = [[0, 1, 2], [3, 4]]
    idxB = [np.concatenate([np.arange(128) + 128 * th, TC_FULL + 2048 * th + np.arange(2048)]) for th in range(2)]
    bc = b_consts()
    for li in range(DEPTH):
        j = li // 2
        even = li % 2 == 0
        maps = []
        for c in range(8):
            b, hf = c // 2, c % 2
            m = dict(hT=hT[b], vecsA=a_vecs(mods[li][b], mods[li][4]))
            if even:
                m.update(a_even_consts())
                m.update(a_even_params(hf, I['even_w_in'][j], I['s5_lam_re'][j], I['s5_lam_im'][j], I['s5_log_dt'][j], I['s5_b_re'][j],
                                       I['s5_b_im'][j], I['s5_c_re'][j], I['s5_c_im'][j], I['s5_d'][j], I['na_rpb'][j]))
            else:
                lam0 = 0.8 - 0.6 * math.exp(-0.3 * li)
                m.update(a_odd_consts())
                m.update(a_odd_params(hf, I['odd_w_in'][j], I['gla_gate_w'][j], I['gla_gate_b'][j], I['gla_norm_g'][j], I['diff_lq1'][j],
                                      I['diff_lk1'][j], I['diff_lq2'][j], I['diff_lk2'][j], I['diff_norm_g'][j]))
                m['lam_init'] = np.tile(np.array([[lam0, 1.0 - lam0]], np.float32), (128, 1))
            maps.append(m)
        if even:
            r = _run(_prog('Ae', lambda: build_A_even(TC_FULL, TL_FULL)), maps)
        else:
            r = _run(_prog('Ao', lambda: build_A_odd(TC_FULL, TL_FULL)), maps)
        mix = []
        for b in range(NB):
            o0, o1 = r[2 * b]['out'], r[2 * b + 1]['out']
            mix.append(np.concatenate([o0[0:256], o1[0:256], o0[256:512], o1[256:512]], 0))
        w_r = np.concatenate([I['moe_w_grp'][li], I['moe_w_exp'][li]], 1)
        b_r = np.tile(np.concatenate([I['moe_b_grp'][li], I['moe_b_exp'][li]])[None], (128, 1))
        w_out = I['even_w_out'][j] if even else I['odd_w_out'][j]
        glu_w = I['s5_glu_w'][j] if even else np.zeros((512, D), np.float32)
        glu_b = I['s5_glu_b'][j] if even else None
        maps = []
        for c in range(8):
            b, th = c // 2, c % 2
            maps.append(dict(hT=hT[b][:, idxB[th]], mixT=mix[b][:, idxB[th]],
                             vecs=b_vecs(mods[li][b], mods[li][4], I['ln_g'][li], I['ln_b'][li], glu_b),
                             w_out=w_out, glu_w=glu_w, w_r=w_r, b_r_bc=b_r, w_gate=I['moe_w_gate'][li], w_up=I['moe_w_up'][li],
                             w_down=I['moe_w_down'][li], **bc))
        nm = 'Be' if even else 'Bo'
        r = _run(_prog(nm, lambda: build_B(2176, chunksB, groupsB, even)), maps)
        for c in range(8):
            b, th = c // 2, c % 2
            hT[b][:, idxB[th]] = r[c]['outT']
    return np.ascontiguousarray(np.stack([hT[b][:, TC_FULL:].T for b in range(NB)], 0)).astype(np.float32)
def emit_M(C, vecsA_s, vecsB_s):
    S = C.S
    cT_d = C.din("cT", [128, KD * 2])
    w_d = C.din("mod_w", [DEPTH, D, 6 * D])
    bT_d = C.din("mod_bT", [DEPTH, 128, 48])
    lnv_d = C.din("lnv", [DEPTH, 128, 48])
    cT = C.sb([128, KD, 2])
    sg = C.sb([128, KD, 2])
    wt = [C.sb([128, KD, 512]) for _ in range(2)]
    bT = C.sb([128, DEPTH, 48])
    mods = C.sb([128, 48, 2])
    va = C.sb([128, 4, KD])
    vb = C.sb([128, NVB, KD])
    P = [C.ps([128, 512]) for _ in range(2)]
    S.dma('sp', cT[:, :, :], cT_d.rearrange("p (k c) -> p k c", k=KD), w=['cT'])
    S.dma('sp', bT[:, :, :], bT_d.rearrange("l p c -> p l c"), w=['bT'])
    act(S, sg[:, :, :], cT[:, :, :], AF.Sigmoid, r=['cT'], w=['sg'])
    tt(S, 'dve', cT[:, :, :], cT[:, :, :], sg[:, :, :], ALU.mult, r=['cT', 'sg'], w=['cT'])
    wi = 0
    for li in range(DEPTH):
        pm = P[li % 2]
        pm3 = pm[:, 0:96].rearrange("p (c r) -> p c r", r=2)
        for blk in range(12):
            w_ = wt[wi % 2]
            S.dma('sp' if wi % 2 == 0 else 'act', w_[:, :, :], w_d[li, :, blk * 512:(blk + 1) * 512].rearrange("(k p) n -> p k n", p=128), w=[('w', wi % 2)])
            for c4 in range(4):
                ch = blk * 4 + c4
                for k in range(KD):
                    mm(S, pm3[:, ch, :], w_[:, k, c4 * 128:(c4 + 1) * 128], cT[:, k, :], k == 0, k == KD - 1, r=['cT', ('w', wi % 2)], w=[('pm', li % 2)])
            wi += 1
        bsrc = bT[:, li, :]
        bb = AP(bsrc.tensor, bsrc.offset, [list(bsrc.ap[0]), [1, 48], [0, 2]])
        tt(S, 'dve', mods[:, :, :], pm3, bb, ALU.add, r=[('pm', li % 2), 'bT'], w=['mods'])
        cp = lambda dst, c0, row: S.op('dve', lambda e: e.tensor_copy(out=dst, in_=mods[:, c0:c0 + 8, row]), r=['mods'], w=['vv'])
        cp(va[:, 0, :], 0, 0)
        cp(va[:, 1, :], 8, 0)
        cp(va[:, 2, :], 0, 1)
        cp(va[:, 3, :], 8, 1)
        for v, (c0, row) in enumerate(((16, 0), (16, 1), (32, 0), (32, 1), (24, 0), (24, 1), (40, 0), (40, 1))):
            cp(vb[:, v, :], c0, row)
        S.dma('sp', vb[:, 8:14, :], lnv_d[li].rearrange("p (v k) -> p v k", k=KD), r=['vv'], w=['vv'])
        S.dma('sp', vecsA_s[li], va[:, :, :].rearrange("p v k -> p (v k)"), r=['vv', 'mods'], w=[('vAs', li)])
        S.dma('sp', vecsB_s[li], vb[:, :, :].rearrange("p v k -> p (v k)"), r=['vv', 'mods'], w=[('vBs', li)])


def build_fused(TC=256, TL=4096):
    T = TC + TL
    C = Ctx()
    C.fused = True
    nc = C.nc
    h0 = nc.dram_tensor("hT0", [D, T], F32, kind="ExternalInput").ap()
    hS = C.scratch("hS", [D, T])
    mixS = C.scratch("mixS", [D, T])
    hOut = nc.dram_tensor("hOut", [D, T], F32, kind="ExternalOutput").ap()
    vecsA_s = [C.scratch("vecsA_s%d" % li, [128, 4 * KD]) for li in range(DEPTH)]
    vecsB_s = [C.scratch("vecsB_s%d" % li, [128, NVB * KD]) for li in range(DEPTH)]
    C.begin("M_", {})
    emit_M(C, vecsA_s, vecsB_s)
    C.end()
    nB = (T // 2)
    chunksB = [(0, TC // 2, True)] + [(TC // 2 + 512 * i, 512, False) for i in range(TL // 2 // 512)]
    groupsB = [[0, 1, 2], [3, 4]] if len(chunksB) == 5 else [list(range(len(chunksB)))]
    for li in range(DEPTH):
        even = li % 2 == 0
        hin = h0 if li == 0 else hS
        hout = hOut if li == DEPTH - 1 else hS
        for half in range(2):
            C.begin("L%dA%d_" % (li, half), dict(hT=hin, vecsA=vecsA_s[li], out=mixS))
            rowmap = (lambda r0, half=half: 256 * half + r0 if r0 < 256 else 512 + 256 * half + (r0 - 256))
            if even:
                build_A_even(TC, TL, C=C, rowmap=rowmap)
            else:
                build_A_odd(TC, TL, C=C, rowmap=rowmap)
            C.end()
        shared = {}
        for nm, shp in (("w_out", [D, D]), ("glu_w", [512, D]), ("w_r", [D, 36]), ("b_r_bc", [128, 36]), ("w_gate", [NEXP, D, FE]),
                        ("w_up", [NEXP, D, FE]), ("w_down", [NEXP, FE, D])):
            shared[nm] = nc.dram_tensor("L%dB_%s" % (li, nm), shp, F32, kind="ExternalInput").ap()
        if li == 0:
            cshared = {"ident": nc.dram_tensor("B_ident", [128, 128], F32, kind="ExternalInput").ap(),
                       "sel": nc.dram_tensor("B_sel", [32, NEXP * 128], F32, kind="ExternalInput").ap()}
        for th in range(2):
            al = dict(hT=hin, mixT=mixS, outT=hout, vecs=vecsB_s[li])
            al.update(shared)
            al.update(cshared)
            colmap = (lambda c0, th=th: (TC // 2) * th + c0 if c0 < TC // 2 else TC + (TL // 2) * th + (c0 - TC // 2))
            C.begin("L%dB%d_" % (li, th), al)
            build_B(nB, chunksB, groupsB, even, C=C, colmap=colmap)
            C.end()
    return C.close_all()
_FUSED = {}


def kernel(**I):
    return _kernel_fused(I, TC_FULL, TL_FULL, NB)


def _kernel_fused(I, TC, TL, nb):
    I = {k: np.asarray(v) for k, v in I.items()}
    if (TC, TL) not in _FUSED:
        _FUSED[(TC, TL)] = build_fused(TC, TL)
    nc = _FUSED[(TC, TL)]
    common = {}
    common["M_mod_w"] = I['mod_w']
    common["M_mod_bT"] = np.stack([I['mod_b'][li].reshape(48, 128).T for li in range(DEPTH)], 0)
    lnv = []
    for li in range(DEPTH):
        j = li // 2
        z = np.zeros(D, np.float32)
        bv, bg = z.copy(), z.copy()
        if li % 2 == 0:
            bv[:512] = I['s5_glu_b'][j][:512]
            bg[:512] = I['s5_glu_b'][j][512:]
        lnv.append(np.concatenate([fm(v) for v in (I['ln_g'][li, 0], I['ln_b'][li, 0], I['ln_g'][li, 1], I['ln_b'][li, 1], bv, bg)], 1))
    common["M_lnv"] = np.stack(lnv, 0)
    bc = b_consts()
    common["B_ident"] = bc['ident']
    common["B_sel"] = bc['sel']
    for li in range(DEPTH):
        j = li // 2
        even = li % 2 == 0
        for hf in range(2):
            pre = "L%dA%d_" % (li, hf)
            if even:
                m = dict(a_even_consts())
                m.update(a_even_params(hf, I['even_w_in'][j], I['s5_lam_re'][j], I['s5_lam_im'][j], I['s5_log_dt'][j], I['s5_b_re'][j],
                                       I['s5_b_im'][j], I['s5_c_re'][j], I['s5_c_im'][j], I['s5_d'][j], I['na_rpb'][j]))
            else:
                lam0 = 0.8 - 0.6 * math.exp(-0.3 * li)
                m = dict(a_odd_consts())
                m.update(a_odd_params(hf, I['odd_w_in'][j], I['gla_gate_w'][j], I['gla_gate_b'][j], I['gla_norm_g'][j], I['diff_lq1'][j],
                                      I['diff_lk1'][j], I['diff_lq2'][j], I['diff_lk2'][j], I['diff_norm_g'][j]))
                m['lam_init'] = np.tile(np.array([[lam0, 1.0 - lam0]], np.float32), (128, 1))
            for k, v in m.items():
                common[pre + k] = v
        pre = "L%dB_" % li
        common[pre + "w_out"] = I['even_w_out'][j] if even else I['odd_w_out'][j]
        common[pre + "glu_w"] = I['s5_glu_w'][j] if even else np.zeros((512, D), np.float32)
        common[pre + "w_r"] = np.concatenate([I['moe_w_grp'][li], I['moe_w_exp'][li]], 1)
        common[pre + "b_r_bc"] = np.tile(np.concatenate([I['moe_b_grp'][li], I['moe_b_exp'][li]])[None], (128, 1))
        common[pre + "w_gate"] = I['moe_w_gate'][li]
        common[pre + "w_up"] = I['moe_w_up'][li]
        common[pre + "w_down"] = I['moe_w_down'][li]
    common = {k: np.ascontiguousarray(v, dtype=np.float32) for k, v in common.items()}
    maps = []
    for c in range(8):
        b = c % nb
        m = dict(common)
        m["hT0"] = np.ascontiguousarray(np.concatenate([I['ctx'][b], I['x'][b]], 0).T.astype(np.float32))
        cc = np.stack([I['c'][b], I['c_ctx']], 0).astype(np.float32)
        m["M_cT"] = np.ascontiguousarray(cc.reshape(2, KD, 128).transpose(2, 1, 0).reshape(128, KD * 2))
        maps.append(m)
    r = run_bass_kernel_spmd(nc, maps, core_ids=list(range(8))).results
    return np.ascontiguousarray(np.stack([r[b]["hOut"][:, TC:].T for b in range(nb)], 0)).astype(np.float32)

kernel = kernel_unfused
```
